# Optimizing a Trainium2 kernel written in Bass

```python
import math
import jax
import jax.numpy as jnp
from jax import lax
import numpy as np

D_MODEL = 1024
BATCH = 2
SEQ = 8192
DEPTH = 1

GRID_W = 64
CTX_LEN = 256
EPS = 1e-6
DA_HEADS = 4
DA_D = 64
DA_DV = 2 * DA_D
ROPE_AXIS_DIM = DA_D // 2
ROPE_THETA = 10000.0
Q_BLOCK = 128
GDN_HEADS = 4
GDN_DK = 128
GDN_DV = 128
CONV_K = 3
CHUNK = 64
N_EXPERTS = 16
EXPERT_FF = 1024
CAP_FACTOR = 2
DA_QK = DA_HEADS * 2 * DA_D
DA_V = DA_HEADS * DA_DV
GDN_QK = GDN_HEADS * GDN_DK
GDN_V = GDN_HEADS * GDN_DV
GDN_QKV = 2 * GDN_QK + GDN_V
GDN_AB = 2 * 2 * GDN_HEADS
D_MIX = DA_V + GDN_V
IN_SPLIT_POINTS = (DA_QK, 2 * DA_QK, 2 * DA_QK + DA_V, 2 * DA_QK + DA_V + GDN_QKV, 2 * DA_QK + DA_V + GDN_QKV + GDN_V)
IN_COLS = 2 * DA_QK + DA_V + GDN_QKV + GDN_V + GDN_AB

kernel_name = 'hybrid_diffattn_gdn_ecmoe_dit_layer'


def rms_norm(x, g):
    xf = x.astype(jnp.float32)
    y = xf * lax.rsqrt(jnp.mean(xf * xf, axis=-1, keepdims=True) + EPS)
    return (y * g.astype(jnp.float32)).astype(x.dtype)


def modulate(x, g, shift, scale):
    return rms_norm(x, g) * (1.0 + scale) + shift


def l2_normalize(t):
    tf = t.astype(jnp.float32)
    return (tf * lax.rsqrt(jnp.sum(tf * tf, axis=-1, keepdims=True) + EPS)).astype(t.dtype)


def rope_axis(x, ang):
    cos = jnp.cos(ang).astype(x.dtype)[:, None, None, :]
    sin = jnp.sin(ang).astype(x.dtype)[:, None, None, :]
    x1, x2 = jnp.split(x, 2, axis=-1)
    return jnp.concatenate([x1 * cos - x2 * sin, x2 * cos + x1 * sin], axis=-1)


def rope_2d(x, ang_row, ang_col):
    xr, xc = jnp.split(x, 2, axis=-1)
    return jnp.concatenate([rope_axis(xr, ang_row), rope_axis(xc, ang_col)], axis=-1)


def diff_attn(q, k, v, lam):
    B, L = q.shape[:2]
    nblk = L // Q_BLOCK
    qb = jnp.moveaxis((q * DA_D ** -0.5).reshape(B, nblk, Q_BLOCK, DA_HEADS, 2, DA_D), 1, 0)

    def block(qi):
        s = jnp.einsum('bqhid,bkhid->bhiqk', qi, k).astype(jnp.float32)
        p = jax.nn.softmax(s, axis=-1)
        a = (p[:, :, 0] - lam * p[:, :, 1]).astype(v.dtype)
        return jnp.einsum('bhqk,bkhe->bqhe', a, v)

    o = lax.map(block, qb)
    return jnp.moveaxis(o, 0, 1).reshape(B, L, DA_HEADS, DA_DV)


def short_conv(x, w):
    L = x.shape[1]
    p = CONV_K // 2
    xp = jnp.pad(x, ((0, 0), (p, p), (0, 0)))
    return jax.nn.silu(sum(xp[:, i:i + L] * w[i] for i in range(CONV_K)))


def gdn_inputs(qkv, ab, conv_w, a_log, dt_bias):
    B, L = qkv.shape[:2]
    q, k, v = jnp.split(short_conv(qkv, conv_w), [GDN_QK, 2 * GDN_QK], axis=-1)
    q = l2_normalize(q.reshape(B, L, GDN_HEADS, GDN_DK)) * GDN_DK ** -0.5
    k = l2_normalize(k.reshape(B, L, GDN_HEADS, GDN_DK))
    v = v.reshape(B, L, GDN_HEADS, GDN_DV)
    ab = ab.reshape(B, L, 2, 2, GDN_HEADS).astype(jnp.float32)
    g = -jnp.exp(a_log.astype(jnp.float32)) * jax.nn.softplus(ab[:, :, 0] + dt_bias.astype(jnp.float32))
    beta = jax.nn.sigmoid(ab[:, :, 1])
    return q, k, v, g, beta


def gdn_chunked(q, k, v, g, beta, s0, need_out):
    B, L, H, dk = k.shape
    dv = v.shape[-1]
    n = L // CHUNK
    f32 = jnp.float32

    def chunks(t):
        t = t.astype(f32).reshape(B, n, CHUNK, *t.shape[2:])
        return jnp.moveaxis(t, (1, 3), (0, 2))

    kc, vc, gc, bc = chunks(k), chunks(v), chunks(g), chunks(beta)
    Gc = jnp.cumsum(gc, axis=-1)
    diff = Gc[..., :, None] - Gc[..., None, :]
    idx = jnp.arange(CHUNK)
    strict = idx[:, None] > idx[None, :]
    dec_strict = jnp.where(strict, jnp.exp(jnp.where(strict, diff, 0.0)), 0.0)
    a_mat = jnp.eye(CHUNK, dtype=f32) + bc[..., None] * jnp.einsum('nbhid,nbhjd->nbhij', kc, kc) * dec_strict
    rhs = jnp.concatenate([(bc * jnp.exp(Gc))[..., None] * kc, bc[..., None] * vc], axis=-1)
    sol = lax.linalg.triangular_solve(a_mat, rhs, left_side=True, lower=True, unit_diagonal=True)
    w, ub = sol[..., :dk], sol[..., dk:]
    kd = kc * jnp.exp(Gc[..., -1:] - Gc)[..., None]
    g_last = jnp.exp(Gc[..., -1])[..., None, None]

    if need_out:
        qc = chunks(q)
        incl = idx[:, None] >= idx[None, :]
        dec_incl = jnp.where(incl, jnp.exp(jnp.where(incl, diff, 0.0)), 0.0)
        qd = qc * jnp.exp(Gc)[..., None]
        aqk = jnp.einsum('nbhid,nbhjd->nbhij', qc, kc) * dec_incl

        def step(s, xs):
            w_n, ub_n, kd_n, gl_n, qd_n, aqk_n = xs
            u = ub_n - jnp.einsum('bhcd,bhde->bhce', w_n, s)
            o = jnp.einsum('bhcd,bhde->bhce', qd_n, s) + jnp.einsum('bhij,bhje->bhie', aqk_n, u)
            return gl_n * s + jnp.einsum('bhcd,bhce->bhde', kd_n, u), o

        s_fin, o = lax.scan(step, s0, (w, ub, kd, g_last, qd, aqk))
        o = jnp.moveaxis(o, (0, 2), (1, 3)).reshape(B, L, H, dv).astype(v.dtype)
        return o, s_fin

    def step_state(s, xs):
        w_n, ub_n, kd_n, gl_n = xs
        u = ub_n - jnp.einsum('bhcd,bhde->bhce', w_n, s)
        return gl_n * s + jnp.einsum('bhcd,bhce->bhde', kd_n, u), None

    s_fin, _ = lax.scan(step_state, s0, (w, ub, kd, g_last))
    return None, s_fin


def bidirectional_gdn(lat, ctx_in, need_ctx_out):
    ql, kl, vl, gl, bl = lat
    qc, kc, vc, gc, bc = ctx_in
    B = ql.shape[0]
    o_lat, o_ctx = None, None
    for d in range(2):
        rev = (lambda t: jnp.flip(t, axis=1)) if d == 1 else (lambda t: t)
        s0 = jnp.zeros((B, GDN_HEADS, GDN_DK, GDN_DV), jnp.float32)
        oc, s_ctx = gdn_chunked(rev(qc), rev(kc), rev(vc), rev(gc[:, :, d]), rev(bc[:, :, d]), s0, need_ctx_out)
        ol, _ = gdn_chunked(rev(ql), rev(kl), rev(vl), rev(gl[:, :, d]), rev(bl[:, :, d]), s_ctx, True)
        o_lat = rev(ol) if o_lat is None else o_lat + rev(ol)
        if need_ctx_out:
            o_ctx = rev(oc) if o_ctx is None else o_ctx + rev(oc)
    return o_lat, o_ctx


def gdn_output(o, gate, gdn_norm_g):
    B, L = o.shape[:2]
    y = rms_norm(o, gdn_norm_g) * jax.nn.silu(gate.reshape(B, L, GDN_HEADS, GDN_DV))
    return y.reshape(B, L, GDN_V)


def expert_choice_ffn(h, w_router, w_gate, w_up, w_down):
    def route_set(t):
        n = t.shape[0]
        cap = CAP_FACTOR * n // N_EXPERTS
        aff = jax.nn.softmax((t @ w_router).astype(jnp.float32), axis=-1)
        gate, idx = lax.top_k(aff.T, cap)
        xe = t[idx]
        hid = jax.nn.silu(jnp.einsum('ecd,edf->ecf', xe, w_gate)) * jnp.einsum('ecd,edf->ecf', xe, w_up)
        ye = jnp.einsum('ecf,efd->ecd', hid, w_down) * gate[..., None].astype(t.dtype)
        return jnp.zeros_like(t).at[idx.reshape(-1)].add(ye.reshape(-1, t.shape[-1]))

    return jax.vmap(route_set)(h)


def hybrid_layer(x, xc, c, c_ctx, ang_row, ang_col, lam_init, last,
                 w_mod, b_mod, norm1_g, w_in, conv_w, a_log, dt_bias, gdn_norm_g,
                 lam_q1, lam_k1, lam_q2, lam_k2, da_subln_g, w_out, norm2_g,
                 w_router, w_gate, w_up, w_down):
    B, L, _ = x.shape
    Lc = xc.shape[1]
    sh1, sc1, gt1, sh2, sc2, gt2 = jnp.split((jax.nn.silu(c) @ w_mod + b_mod)[:, None, :], 6, axis=-1)
    sh1c, sc1c, gt1c, sh2c, sc2c, gt2c = jnp.split(jax.nn.silu(c_ctx) @ w_mod + b_mod, 6, axis=-1)

    p = modulate(x, norm1_g, sh1, sc1) @ w_in
    pc = modulate(xc, norm1_g, sh1c, sc1c) @ w_in
    q, k, v, qkv, gate, ab = jnp.split(p, IN_SPLIT_POINTS, axis=-1)
    qc, kc, vc, qkvc, gatec, abc = jnp.split(pc, IN_SPLIT_POINTS, axis=-1)

    lam = (jnp.exp(jnp.sum(lam_q1 * lam_k1, dtype=jnp.float32))
           - jnp.exp(jnp.sum(lam_q2 * lam_k2, dtype=jnp.float32)) + lam_init)
    k_c = kc.reshape(B, Lc, DA_HEADS, 2, DA_D)
    v_c = vc.reshape(B, Lc, DA_HEADS, DA_DV)
    q_l = rope_2d(q.reshape(B, L, DA_HEADS, 2, DA_D), ang_row, ang_col)
    k_l = rope_2d(k.reshape(B, L, DA_HEADS, 2, DA_D), ang_row, ang_col)
    o_da = diff_attn(q_l, jnp.concatenate([k_c, k_l], axis=1),
                     jnp.concatenate([v_c, v.reshape(B, L, DA_HEADS, DA_DV)], axis=1), lam)
    o_da = (rms_norm(o_da, da_subln_g) * (1.0 - lam_init)).reshape(B, L, DA_V)

    o_gdn_l, o_gdn_c = bidirectional_gdn(gdn_inputs(qkv, ab, conv_w, a_log, dt_bias),
                                         gdn_inputs(qkvc, abc, conv_w, a_log, dt_bias), not last)
    o_gdn = gdn_output(o_gdn_l, gate, gdn_norm_g)

    x_new = x + gt1 * (jnp.concatenate([o_da, o_gdn], axis=-1) @ w_out)
    x_new = x_new + gt2 * expert_choice_ffn(modulate(x_new, norm2_g, sh2, sc2), w_router, w_gate, w_up, w_down)

    if not last:
        o_da_c = diff_attn(qc.reshape(B, Lc, DA_HEADS, 2, DA_D), k_c, v_c, lam)
        o_da_c = (rms_norm(o_da_c, da_subln_g) * (1.0 - lam_init)).reshape(B, Lc, DA_V)
        xc = xc + gt1c * (jnp.concatenate([o_da_c, gdn_output(o_gdn_c, gatec, gdn_norm_g)], axis=-1) @ w_out)
        xc = xc + gt2c * expert_choice_ffn(modulate(xc, norm2_g, sh2c, sc2c), w_router, w_gate, w_up, w_down)
    return x_new, xc


def setup_inputs(seed: int = 0) -> dict:
    key = jax.random.key(seed)
    ks = jax.random.split(key, 24)
    D = D_MODEL
    f32 = jnp.float32

    def nrm(k, shape, s):
        return jax.random.normal(k, shape, f32) * s

    dt = jnp.exp(jax.random.uniform(ks[10], (DEPTH, 2, GDN_HEADS), f32, math.log(1e-3), math.log(1e-1)))
    return {
        'x': nrm(ks[0], (BATCH, SEQ, D), 1.0),
        'c': nrm(ks[1], (BATCH, D), 1.0),
        'ctx': nrm(ks[2], (BATCH, CTX_LEN, D), 1.0),
        'c_ctx': nrm(ks[3], (D,), 1.0),
        'w_mod': nrm(ks[4], (DEPTH, D, 6 * D), 0.5 * D ** -0.5),
        'b_mod': nrm(ks[5], (DEPTH, 6 * D), 0.01),
        'norm1_g': 1.0 + nrm(ks[6], (DEPTH, D), 0.05),
        'w_in': nrm(ks[7], (DEPTH, D, IN_COLS), D ** -0.5),
        'conv_w': nrm(ks[8], (DEPTH, CONV_K, GDN_QKV), CONV_K ** -0.5),
        'a_log': jnp.log(jax.random.uniform(ks[9], (DEPTH, 2, GDN_HEADS), f32, 1.0, 16.0)),
        'dt_bias': dt + jnp.log(-jnp.expm1(-dt)),
        'gdn_norm_g': 1.0 + nrm(ks[11], (DEPTH, GDN_DV), 0.05),
        'lam_q1': nrm(ks[12], (DEPTH, DA_D), 0.1),
        'lam_k1': nrm(ks[13], (DEPTH, DA_D), 0.1),
        'lam_q2': nrm(ks[14], (DEPTH, DA_D), 0.1),
        'lam_k2': nrm(ks[15], (DEPTH, DA_D), 0.1),
        'da_subln_g': 1.0 + nrm(ks[16], (DEPTH, DA_DV), 0.05),
        'w_out': nrm(ks[17], (DEPTH, D_MIX, D), D_MIX ** -0.5),
        'norm2_g': 1.0 + nrm(ks[18], (DEPTH, D), 0.05),
        'w_router': nrm(ks[19], (DEPTH, D, N_EXPERTS), D ** -0.5),
        'w_gate': nrm(ks[20], (DEPTH, N_EXPERTS, D, EXPERT_FF), D ** -0.5),
        'w_up': nrm(ks[21], (DEPTH, N_EXPERTS, D, EXPERT_FF), D ** -0.5),
        'w_down': nrm(ks[22], (DEPTH, N_EXPERTS, EXPERT_FF, D), EXPERT_FF ** -0.5),
        'final_g': 1.0 + nrm(ks[23], (D,), 0.05),
    }


def reference(x, c, ctx, c_ctx, w_mod, b_mod, norm1_g, w_in, conv_w, a_log, dt_bias, gdn_norm_g,
              lam_q1, lam_k1, lam_q2, lam_k2, da_subln_g, w_out, norm2_g,
              w_router, w_gate, w_up, w_down, final_g):
    f32 = jnp.float32
    L = x.shape[1]
    ROWS = L // GRID_W
    rows = jnp.repeat(jnp.arange(ROWS, dtype=f32), GRID_W)
    cols = jnp.tile(jnp.arange(GRID_W, dtype=f32), ROWS)
    inv_freq = jnp.power(ROPE_THETA, -jnp.arange(0, ROPE_AXIS_DIM, 2, dtype=f32) / ROPE_AXIS_DIM)
    ang_row = rows[:, None] * inv_freq[None, :]
    ang_col = cols[:, None] * inv_freq[None, :]

    xc = ctx
    for layer in range(DEPTH):
        lam_init = 0.8 - 0.6 * math.exp(-0.3 * layer)
        x, xc = hybrid_layer(x, xc, c, c_ctx, ang_row, ang_col, lam_init, layer == DEPTH - 1,
                             w_mod[layer], b_mod[layer], norm1_g[layer], w_in[layer], conv_w[layer],
                             a_log[layer], dt_bias[layer], gdn_norm_g[layer],
                             lam_q1[layer], lam_k1[layer], lam_q2[layer], lam_k2[layer], da_subln_g[layer],
                             w_out[layer], norm2_g[layer], w_router[layer], w_gate[layer], w_up[layer], w_down[layer])
    return rms_norm(x, final_g)
```

```python
import math
import numpy as np
import concourse.bass as bass
import concourse.mybir as mybir
from concourse.bass_utils import run_bass_kernel_spmd

F32 = mybir.dt.float32
BF16 = mybir.dt.bfloat16
AF = mybir.ActivationFunctionType
ALU = mybir.AluOpType
AX = mybir.AxisListType

ENGS = ("pe", "act", "dve", "pool", "sp")
EPOCH = 30000
EPS = 1e-6
L = 8192
LC = 256
D = 1024
NSC = 66
LAM_INIT = 0.8 - 0.6 * math.exp(-0.3 * 0)

STAGE = [99]
MAPS_ONLY = [False]
SEQ = [False]
RUN_KW = {}
DEBUG = []
LAST = {}


class Prog:
    def __init__(self, nc, sync_same_engine=True):
        self.nc = nc
        self.ops = {e: [] for e in ENGS}
        self.count = {e: 0 for e in ENGS}
        self.sems = {}
        self.chan_count = {}
        self.chan_inc = {}
        self.waited = {e: {} for e in ENGS}
        self.bufs = {}
        self.sync_same = sync_same_engine

    def sem(self, name):
        if name not in self.sems:
            ctx = self.nc.semaphore(name)
            self.sems[name] = ctx.__enter__()
        return self.sems[name]

    def _need(self, eng, tok, waits):
        if tok is None:
            return
        sname, val, teng = tok
        if teng == eng and (eng == "pe" or not self.sync_same):
            return
        if val <= self.waited[eng].get(sname, 0):
            return
        self.waited[eng][sname] = val
        waits.append((sname, val))

    def _deps(self, eng, reads, writes):
        waits = []
        for k in reads:
            b = self.bufs.get(k)
            if b is not None:
                self._need(eng, b["w"], waits)
                if k[0] == "B":
                    for t in b["r"]:
                        if t[2] != eng:
                            self._need(eng, t, waits)
        for k in writes:
            b = self.bufs.get(k)
            if b is not None:
                self._need(eng, b["w"], waits)
                for t in b["r"]:
                    self._need(eng, t, waits)
        return waits

    def _commit(self, tok, reads, writes):
        for k in reads:
            b = self.bufs.setdefault(k, {"w": None, "r": []})
            b["r"].append(tok)
            if len(b["r"]) > 64:
                b["r"] = b["r"][-48:]
        for k in writes:
            self.bufs[k] = {"w": tok, "r": []}

    def op(self, eng, fn, reads=(), writes=()):
        waits = self._deps(eng, reads, writes)
        idx = self.count[eng]
        self.count[eng] += 1
        ep, k = divmod(idx, EPOCH)
        sname = f"p_{eng}_{ep}"
        self.sem(sname)
        tok = (sname, k + 1, eng)
        self.ops[eng].append((waits, fn, (sname, 1)))
        self._commit(tok, reads, writes)
        return tok

    def dma(self, eng, chan, fn, reads=(), writes=(), inc=16):
        waits = self._deps(eng, reads, writes)
        n = self.chan_count.get(chan, 0)
        sname = f"d_{chan}"
        self.sem(sname)
        self.chan_inc[chan] = inc
        if n > 0:
            self._need(eng, (sname, inc * n, "dma"), waits)
        self.chan_count[chan] = n + 1
        tok = (sname, inc * (n + 1), "dma")
        self.ops[eng].append((waits, fn, (sname, inc)))
        self._commit(tok, reads, writes)
        return tok

    def barrier(self):
        toks = []
        for e in ENGS:
            n = self.count[e]
            if n:
                ep, k = divmod(n - 1, EPOCH)
                toks.append((f"p_{e}_{ep}", k + 1, e))
        for c, n in self.chan_count.items():
            if c.startswith("ccrs"):
                continue
            toks.append((f"d_{c}", self.chan_inc[c] * n, "dma"))
        for e in ENGS:
            waits = []
            for t in toks:
                if t[2] == e and t[2] != "dma":
                    pass
                sname, val, _ = t
                if val > self.waited[e].get(sname, 0):
                    self.waited[e][sname] = val
                    waits.append((sname, val))
            self.ops[e].append((waits, None, None))

    def emit(self):
        nc = self.nc
        sems = self.sems
        ops = self.ops
        with nc.Block() as block:
            def run(e, engobj):
                for waits, fn, inc in ops[e]:
                    for sname, val in waits:
                        engobj.wait_ge(sems[sname], val)
                    if fn is not None:
                        fn(engobj).then_inc(sems[inc[0]], inc[1])

            @block.tensor
            def _(eng):
                run("pe", eng)

            @block.scalar
            def _(eng):
                run("act", eng)

            @block.vector
            def _(eng):
                run("dve", eng)

            @block.gpsimd
            def _(eng):
                run("pool", eng)

            @block.sync
            def _(eng):
                run("sp", eng)


def _isz(dt):
    return 2 if dt == BF16 else 4


class Arena:
    def __init__(self, nc, limit=229376 - 1024):
        self.nc = nc
        self.off = 16384 + 1024
        self.n = 0
        self.limit = limit

    def alloc(self, name, shape, dt):
        sz = _isz(dt)
        for s in shape[1:]:
            sz *= s
        sz = (sz + 63) // 64 * 64
        t = self.nc.alloc_sbuf_tensor_at(f"{name}_{self.n}", list(shape), dt, offset=self.off)
        self.off += sz
        self.n += 1
        assert self.off <= self.limit, (name, self.off)
        return t


NCONST = 13


def make_consts():
    idx = np.arange(128)
    blk = (idx[:, None] // 64) == (idx[None, :] // 64)
    k = idx[:, None]
    m = idx[None, :]
    c = np.zeros((NCONST, 128, 128), np.float32)
    c[0] = np.eye(128)
    c[1] = 1.0
    c[2] = blk & (k <= m)
    c[3] = blk & (k >= m)
    c[4] = blk
    c[5] = (k < 64) & (m >= 0)
    c[6] = (k >= 64) & (m >= 0)
    c[7] = blk & (m > k)
    c[8] = blk & (m >= k)
    c[9] = blk & (m < k)
    c[10] = blk & (m <= k)
    pm = np.zeros((128, 128), np.float32)
    for mm_ in range(128):
        i = mm_ % 64
        r = i % 32
        partner = mm_ + 16 if r < 16 else mm_ - 16
        pm[partner, mm_] = 1.0
    c[11] = pm
    g = np.zeros((128, 128), np.float32)
    g[:64, :64] = (idx[:64, None] % 16) == (idx[None, :64] % 16)
    c[12] = g
    return np.ascontiguousarray(c.transpose(1, 0, 2).reshape(128, NCONST * 128))


def make_rope():
    f32 = np.float32
    t = np.arange(L)
    rows = (t // 64).astype(f32)
    cols = (t % 64).astype(f32)
    inv_freq = np.power(f32(10000.0), -np.arange(0, 32, 2, dtype=f32) / f32(32)).astype(f32)
    ang_row = (rows[:, None] * inv_freq[None, :]).astype(f32)
    ang_col = (cols[:, None] * inv_freq[None, :]).astype(f32)
    cosT = np.zeros((128, L), f32)
    sinT = np.zeros((128, L), f32)
    for p in range(128):
        i = p % 64
        ang = ang_row if i < 32 else ang_col
        r = i % 32
        f = r % 16
        sign = -1.0 if r < 16 else 1.0
        cosT[p] = np.cos(ang[:, f]).astype(f32)
        sinT[p] = (sign * np.sin(ang[:, f])).astype(f32)
    return np.stack([cosT, sinT], 0)


def build(debug=(), stage=99):
    nc = bass.Bass("TRN2", target_bir_lowering=False)
    P = Prog(nc)

    def din(name, shape, dt=F32):
        return nc.dram_tensor(name, list(shape), dt, kind="ExternalInput").ap()

    xb = din("xb", [L, D])
    ctxb = din("ctxb", [LC, D])
    xo = din("xo", [2048, D])
    cT_d = din("cT", [128, 16])
    wmod = din("wmod", [D, 6 * D])
    bm1T_d = din("bm1T", [128, 16])
    bm2_d = din("bm2", [1, 4096])
    g1T_d = din("g1T", [128, 8])
    g2T_d = din("g2T", [128, 8])
    fgbc_d = din("fgbc", [128, D])
    wslab_d = din("wslab", [D, 896])
    wab_d = din("wab", [D, 4])
    cwT_d = din("cwT", [128, 9])
    gsc_d = din("gsc", [128, 4])
    gng_d = din("gng", [128, 128])
    sgg_d = din("sgg", [128, 128])
    lamv_d = din("lamv", [128, 256])
    wout_d = din("wout", [2, 128, D])
    rope_d = din("rope", [2, 128, L])
    consts_d = din("consts", [128, NCONST * 128])
    wr_d = din("wr", [128, 128])
    wshape = [16, D, D] if stage > 4 else [1, 8, 8]
    wg_d = din("wg", wshape)
    wu_d = din("wu", wshape)
    wd_d = din("wd", wshape)
    out_d = nc.dram_tensor("out", [2048, D], F32, kind="ExternalOutput").ap()

    kt_s = nc.dram_tensor("kt_s", [128, LC + L], BF16).ap()
    qt_s = nc.dram_tensor("qt_s", [128, L], BF16).ap()
    v_s = nc.dram_tensor("v_s", [NSC, 128, 130], BF16).ap()
    rs_in = [nc.dram_tensor(f"rs_in{i}", [2048, D], F32) for i in range(4)]
    rs_out = [nc.dram_tensor(f"rs_out{i}", [512, D], F32) for i in range(4)]
    ag_in = nc.dram_tensor("ag_in", [16, 2048], F32)
    ag_out = nc.dram_tensor("ag_out", [64, 2048], F32)
    xnew_s = nc.dram_tensor("xnew_s", [2048, D], F32).ap()

    dbg_outs = {}

    def finish():
        waits = []
        for k in ["out0", "out1"] + ["dbg_" + n for n in dbg_outs]:
            b = P.bufs.get(k)
            if b is not None:
                P._need("pool", b["w"], waits)
        P.ops["pool"].append((waits, None, None))
        P.emit()
        return nc, dbg_outs

    def dbg(name, src_ap, shape, key):
        if name in debug:
            o = nc.dram_tensor("dbg_" + name, list(shape), src_ap.tensor.dtype, kind="ExternalOutput").ap()
            dbg_outs[name] = o
            P.dma("pool", "dbg_" + name, lambda e: e.dma_start(out=o, in_=src_ap), reads=[key], writes=["dbg_" + name])

    psum = nc.alloc_psum_tensor("ps", [128, 8, 512], F32)

    def bank(i):
        return psum[:, i, :]

    def mm(out, lhsT, rhs, r, w, start=True, stop=True):
        P.op("pe", lambda e: e.matmul(out, lhsT=lhsT, rhs=rhs, start=start, stop=stop), reads=r, writes=w)

    def tr(out, in_, idt, r, w):
        P.op("pe", lambda e: e.transpose(out, in_, idt), reads=r, writes=w)

    def act(out, in_, func, r, w, bias=None, scale=None, accum=None):
        kw = {}
        if bias is not None:
            kw["bias"] = bias
        if scale is not None:
            kw["scale"] = scale
        if accum is not None:
            kw["accum_out"] = accum
        P.op("act", lambda e: e.activation(out=out, in_=in_, func=func, **kw), reads=r, writes=w)

    def ts(eng, out, in0, s1, s2, op0, op1, r, w, accum=None):
        if op1 is None:
            P.op(eng, lambda e: e.tensor_scalar(out=out, in0=in0, scalar1=s1, scalar2=None, op0=op0), reads=r, writes=w)
        elif accum is None:
            P.op(eng, lambda e: e.tensor_scalar(out=out, in0=in0, scalar1=s1, scalar2=s2, op0=op0, op1=op1), reads=r, writes=w)
        else:
            P.op(eng, lambda e: e.tensor_scalar(out=out, in0=in0, scalar1=s1, scalar2=s2, op0=op0, op1=op1, accum_out=accum),
                 reads=r, writes=w)

    def tt(eng, out, in0, in1, op, r, w):
        P.op(eng, lambda e: e.tensor_tensor(out=out, in0=in0, in1=in1, op=op), reads=r, writes=w)

    def stt(out, in0, scalar, in1, op0, op1, r, w):
        P.op("dve", lambda e: e.scalar_tensor_tensor(out=out, in0=in0, scalar=scalar, in1=in1, op0=op0, op1=op1), reads=r, writes=w)

    def cp(eng, out, in_, r, w):
        if eng == "act":
            P.op("act", lambda e: e.copy(out=out, in_=in_), reads=r, writes=w)
        else:
            P.op(eng, lambda e: e.tensor_copy(out=out, in_=in_), reads=r, writes=w)

    def recip(out, in_, r, w):
        P.op("dve", lambda e: e.reciprocal(out=out, in_=in_), reads=r, writes=w)

    def mset(eng, ap, val, w):
        P.op(eng, lambda e: e.memset(ap, val), reads=(), writes=w)

    def dma(eng, chan, out, in_, r, w):
        P.dma(eng, chan, lambda e: e.dma_start(out=out, in_=in_), reads=r, writes=w)

    A = Arena(nc)
    cst = A.alloc("cst", [128, NCONST, 128], F32)
    dma("sp", "cst", cst[:, :, :].rearrange("p a b -> p (a b)"), consts_d, (), ["cst"])

    def C(i):
        return cst[:, i, :]
    ident, ones, triF, triB, blkm, H0, H1 = C(0), C(1), C(2), C(3), C(4), C(5), C(6)
    mS = [C(7), C(9)]
    mI = [C(8), C(10)]
    tri = [triF, triB]
    identb = A.alloc("identb", [128, 128], BF16)
    pmb = A.alloc("pmb", [128, 128], BF16)
    cp("dve", identb[:, :], ident, ["cst"], ["identb"])
    cp("dve", pmb[:, :], C(11), ["cst"], ["pmb"])

    small = A.alloc("small", [128, 512], F32)
    _sc = [0]

    def col(n=1):
        c0 = _sc[0]
        _sc[0] += n
        assert _sc[0] <= 512
        return small[:, c0:c0 + n]

    cT = col(16)
    bm1T = col(16)
    g1T = col(8)
    g2T = col(8)
    cwT = col(9)
    gsc = col(4)
    dma("sp", "sm0", cT, cT_d, (), ["cT"])
    dma("sp", "sm1", bm1T, bm1T_d, (), ["bm1T"])
    dma("sp", "sm2", g1T, g1T_d, (), ["g1T"])
    dma("sp", "sm3", g2T, g2T_d, (), ["g2T"])
    dma("sp", "sm4", cwT, cwT_d, (), ["cwT"])
    dma("sp", "sm5", gsc, gsc_d, (), ["gsc"])
    gng = A.alloc("gng", [128, 128], F32)
    sgg = A.alloc("sgg", [128, 128], F32)
    dma("sp", "sm6", gng[:, :], gng_d, (), ["gng"])
    dma("sp", "sm7", sgg[:, :], sgg_d, (), ["sgg"])
    lamv = A.alloc("lamv", [128, 256], F32)
    dma("sp", "sm8", lamv[:, :], lamv_d, (), ["lamv"])

    lamt = col(8)
    lsc = A.alloc("lsc", [128, 64], F32)
    P.op("dve", lambda e: e.tensor_tensor(out=lsc[:, :], in0=lamv[:, 0:64], in1=lamv[:, 64:128], op=ALU.mult), reads=["lamv"], writes=["lsc"])
    P.op("dve", lambda e: e.tensor_reduce(out=lamt[:, 0:1], in_=lsc[:, :], axis=AX.X, op=ALU.add), reads=["lsc"], writes=["lam0"])
    P.op("dve", lambda e: e.tensor_tensor(out=lsc[:, :], in0=lamv[:, 128:192], in1=lamv[:, 192:256], op=ALU.mult), reads=["lamv", "lam0"], writes=["lsc"])
    P.op("dve", lambda e: e.tensor_reduce(out=lamt[:, 1:2], in_=lsc[:, :], axis=AX.X, op=ALU.add), reads=["lsc"], writes=["lam1"])
    act(lamt[:, 2:4], lamt[:, 0:2], AF.Exp, ["lam0", "lam1"], ["lam2"])
    tt("dve", lamt[:, 4:5], lamt[:, 2:3], lamt[:, 3:4], ALU.subtract, ["lam2"], ["lam3"])
    ts("dve", lamt[:, 5:6], lamt[:, 4:5], -1.0, -LAM_INIT, ALU.mult, ALU.add, ["lam3"], ["nlam"])
    nlam = lamt[:, 5:6]

    scb = A.alloc("scb", [128, 8, 2], BF16)
    persist_mark = A.off

    KnT = A.alloc("KnT", [128, NSC * 128], BF16)
    QnT = A.alloc("QnT", [128, NSC * 128], BF16)
    Vg = A.alloc("Vg", [128, NSC, 128], BF16)
    gate_s = A.alloc("gate_s", [128, 64, 128], BF16)
    abt = A.alloc("abt", [128, NSC, 4], F32)
    gdn_mark = A.off

    wslab = A.alloc("wslab", [128, 8, 896], BF16)
    wab = A.alloc("wab", [128, 8, 4], BF16)
    for kc in range(8):
        dma("pool", f"wsl{kc % 2}", wslab[:, kc, :], wslab_d[kc * 128:(kc + 1) * 128, :], (), ["wslab"])
    dma("pool", "wab", wab[:, :, :], wab_d.rearrange("(kc p) n -> p kc n", p=128), (), ["wab"])

    m1 = A.alloc("m1", [128, 16, 2], F32)
    a1 = col(8)
    a1c = col(8)
    ipw_mark = A.off
    wm1 = A.alloc("wm1", [128, 8, 2048], BF16)
    for kc in range(8):
        dma("pool", f"wm{kc % 2}", wm1[:, kc, :], wmod[kc * 128:(kc + 1) * 128, 0:2048], (), [f"wm1_{kc}"])
    act(scb[:, :, :].rearrange("p a b -> p (a b)"), cT, AF.Silu, ["cT"], ["scb"])
    pmod = bank(0)[:, 0:32]
    for c_ in range(16):
        for kc in range(8):
            mm(pmod[:, c_ * 2:(c_ + 1) * 2], wm1[:, kc, c_ * 128:(c_ + 1) * 128], scb[:, kc, :], [f"wm1_{kc}", "scb"], ["B0"],
               start=(kc == 0), stop=(kc == 7))
    pmod3 = pmod.rearrange("p (c v) -> p c v", v=2)
    for v in range(2):
        tt("dve", m1[:, :, v], pmod3[:, :, v], bm1T, ALU.add, ["B0", "bm1T"], ["m1"])
    stt(a1, m1[:, 8:16, 0], 1.0, g1T, ALU.add, ALU.mult, ["m1", "g1T"], ["a1"])
    stt(a1c, m1[:, 8:16, 1], 1.0, g1T, ALU.add, ALU.mult, ["m1", "g1T"], ["a1c"])
    if stage == 0:
        return finish()
    P.barrier()
    A.off = ipw_mark

    NT = 33
    xt = [A.alloc(f"xt{i}", [128, 2, D], F32) for i in range(2)]
    hT = [A.alloc(f"hT{i}", [128, 8, 256], BF16) for i in range(2)]
    pre = [A.alloc(f"pre{i}", [128, 3, 258], F32) for i in range(4)]
    rtab = [A.alloc(f"rtab{i}", [128, 2, 256], F32) for i in range(2)]
    sqj = A.alloc("sqj", [128, D], BF16)
    nrm = A.alloc("nrm", [128, 8], F32)
    qb = [A.alloc(f"qb{i}", [128, 256], BF16) for i in range(2)]
    rt1 = [A.alloc(f"rt1_{i}", [128, 256], F32) for i in range(2)]
    rt2 = [A.alloc(f"rt2_{i}", [128, 256], F32) for i in range(2)]
    qkst = [A.alloc(f"qkst{i}", [128, 256], BF16) for i in range(4)]
    vst = [A.alloc(f"vst{i}", [128, 2, 130], BF16) for i in range(2)]
    cacc = A.alloc("cacc", [128, 3, 256], F32)
    csil = A.alloc("csil", [128, 3, 256], F32)
    csq = A.alloc("csq", [128, 2, 256], F32)
    rinv = A.alloc("rinv", [128, 2, 256], F32)
    for i in range(2):
        mset("pool", vst[i][:, :, 128:130], 1.0, [f"vst{i}"])

    def tile_info(t):
        if t == 0:
            return ctxb, 0, True
        return xb[(t - 1) * 256:t * 256, :], LC + (t - 1) * 256, False

    fm_cnt = [0]

    def front(t):
        src, koff, is_ctx = tile_info(t)
        xs = xt[t % 2]
        hs = hT[t % 2]
        kx, kh = f"xt{t % 2}", f"hT{t % 2}"
        dma("sp", f"x{t % 2}", xs[:, :, :], src.rearrange("(s p) d -> p s d", p=128), (), [kx])
        yield
        if not is_ctx:
            rs_ = rtab[t % 2]
            dma("sp", f"rt{t % 2}", rs_[:, :, :], rope_d[:, :, (t - 1) * 256:t * 256].rearrange("a p n -> p a n"), (), [f"rtab{t % 2}"])
            yield
        sh = m1[:, 0:8, 1] if is_ctx else m1[:, 0:8, 0]
        aa = a1c if is_ctx else a1
        for s in range(2):
            act(sqj[:, :], xs[:, s, :], AF.Square, [kx], ["sqj", f"nrm{s}"], accum=nrm[:, s:s + 1])
            yield
        act(nrm[:, 2:4], nrm[:, 0:2], AF.Sqrt, ["nrm0", "nrm1"], ["nrmB"], bias=EPS, scale=1.0 / D)
        yield
        recip(nrm[:, 4:6], nrm[:, 2:4], ["nrmB"], ["nrmC"])
        yield
        for s in range(2):
            ts("pool", xs[:, s, :], xs[:, s, :], nrm[:, 4 + s:5 + s], 1.0, ALU.mult, ALU.mult, [kx, "nrmC"], [kx])
            yield
        for kc in range(8):
            pb = kc % 2
            pT = bank(pb)[:, 0:256]
            for s in range(2):
                tr(pT[:, s * 128:(s + 1) * 128], xs[:, s, kc * 128:(kc + 1) * 128], ident, [kx, "cst"], [f"B{pb}"])
                yield
            if kc % 2 == 0:
                act(hs[:, kc, :], pT, AF.Identity, [f"B{pb}", "a1", "a1c", "m1"], [f"{kh}_{kc}"], bias=sh[:, kc:kc + 1], scale=aa[:, kc:kc + 1])
                yield
            else:
                ts("dve", hs[:, kc, :], pT, aa[:, kc:kc + 1], sh[:, kc:kc + 1], ALU.mult, ALU.add, [f"B{pb}", "a1", "a1c", "m1"], [f"{kh}_{kc}"])
                yield

    def back(t):
        src, koff, is_ctx = tile_info(t)
        xs = xt[t % 2]
        hs = hT[t % 2]
        kx, kh = f"xt{t % 2}", f"hT{t % 2}"
        sh = m1[:, 0:8, 1] if is_ctx else m1[:, 0:8, 0]
        aa = a1c if is_ctx else a1
        pr = pre[t % 4]
        kp = f"pre{t % 4}"
        for blk_ in range(5):
            if is_ctx and blk_ == 0:
                continue
            fb = 2 + (fm_cnt[0] % 2)
            fm_cnt[0] += 1
            pf = bank(fb)[:, 0:256]
            for kc in range(8):
                mm(pf, wslab[:, kc, blk_ * 128:(blk_ + 1) * 128], hs[:, kc, :], ["wslab", f"{kh}_{kc}"], [f"B{fb}"], start=(kc == 0), stop=(kc == 7))
                yield
            if blk_ < 2:
                st = qkst[(t % 2) * 2 + blk_]
                kst = f"qkst{(t % 2) * 2 + blk_}"
                dst = (qt_s[:, (t - 1) * 256:t * 256] if blk_ == 0 else kt_s[:, koff:koff + 256]) if not is_ctx else kt_s[:, 0:256]
                if is_ctx:
                    cp("act", st[:, :], pf, [f"B{fb}"], [kst])
                    yield
                else:
                    q_b = qb[blk_]
                    rs_ = rtab[t % 2]
                    cp("act", q_b[:, :], pf, [f"B{fb}"], [f"qb{blk_}"])
                    yield
                    pp = bank(6)[:, blk_ * 256:(blk_ + 1) * 256]
                    mm(pp, pmb[:, :], q_b[:, :], ["pmb", f"qb{blk_}"], ["B6"])
                    yield
                    tt("dve", rt1[blk_][:, :], pf, rs_[:, 0, :], ALU.mult, [f"B{fb}", f"rtab{t % 2}"], [f"rt1_{blk_}"])
                    yield
                    tt("dve", rt2[blk_][:, :], pp, rs_[:, 1, :], ALU.mult, ["B6", f"rtab{t % 2}"], [f"rt2_{blk_}"])
                    yield
                    tt("pool", st[:, :], rt1[blk_][:, :], rt2[blk_][:, :], ALU.add, [f"rt1_{blk_}", f"rt2_{blk_}"], [kst])
                    yield
                if stage != 0.325:
                    dma("pool", f"qk{(t % 2) * 2 + blk_}", dst, st[:, :], [kst], ["ktqt_s"])
                    yield
            else:
                c_ = blk_ - 2
                if c_ % 2 == 0:
                    cp("act", pr[:, c_, 1:257], pf, [f"B{fb}"], [kp])
                    yield
                else:
                    cp("dve", pr[:, c_, 1:257], pf, [f"B{fb}"], [kp])
                    yield
        vs_ = vst[t % 2]
        for s in range(2):
            sc = (koff // 128) + s
            pv = bank(4 + s)
            kb = f"B{4 + s}"
            for kc in range(8):
                mm(pv[:, 0:128], hs[:, kc, s * 128:(s + 1) * 128], wslab[:, kc, 640:768], [f"{kh}_{kc}", "wslab"], [kb], start=(kc == 0), stop=(kc == 7))
                yield
            if not is_ctx:
                for kc in range(8):
                    mm(pv[:, 128:256], hs[:, kc, s * 128:(s + 1) * 128], wslab[:, kc, 768:896], [f"{kh}_{kc}", "wslab"], [kb], start=(kc == 0), stop=(kc == 7))
                    yield
            if stage != 0.331:
                for kc in range(8):
                    mm(pv[:, 256:260], hs[:, kc, s * 128:(s + 1) * 128], wab[:, kc, :], [f"{kh}_{kc}", "wab"], [kb], start=(kc == 0), stop=(kc == 7))
                    yield
            if stage != 0.333:
                cp("act", vs_[:, s, 0:128], pv[:, 0:128], [kb], [f"vst{t % 2}"])
                yield
            if not is_ctx:
                act(gate_s[:, sc - 2, :], pv[:, 128:256], AF.Silu, [kb], ["gate_s"])
                yield
            if stage not in (0.331, 0.332):
                cp("dve", abt[:, sc, :], pv[:, 256:260], [kb], ["abt"])
                yield
        dma("pool", f"v{t % 2}", v_s[(koff // 128):(koff // 128) + 2, :, :].rearrange("s p n -> p s n"), vs_[:, :, :], [f"vst{t % 2}"], ["v_s"])
        yield

    def conv_stage(t, left, right):
        src, koff, is_ctx = tile_info(t)
        pr = pre[t % 4]
        kp = f"pre{t % 4}"
        if left is None:
            mset("pool", pr[:, :, 0:1], 0.0, [kp])
            yield
        else:
            cp("pool", pr[:, :, 0:1], pre[left % 4][:, :, 256:257], [f"pre{left % 4}", kp], [kp])
            yield
        if right is None:
            mset("pool", pr[:, :, 257:258], 0.0, [kp])
            yield
        else:
            cp("pool", pr[:, :, 257:258], pre[right % 4][:, :, 1:2], [f"pre{right % 4}", kp], [kp])
            yield
        for c_ in range(3):
            ts("dve", cacc[:, c_, :], pr[:, c_, 0:256], cwT[:, c_ * 3:c_ * 3 + 1], None, ALU.mult, None, [kp, "cwT"], ["cacc"])
            yield
            stt(cacc[:, c_, :], pr[:, c_, 1:257], cwT[:, c_ * 3 + 1:c_ * 3 + 2], cacc[:, c_, :], ALU.mult, ALU.add, [kp, "cacc"], ["cacc"])
            yield
            stt(cacc[:, c_, :], pr[:, c_, 2:258], cwT[:, c_ * 3 + 2:c_ * 3 + 3], cacc[:, c_, :], ALU.mult, ALU.add, [kp, "cacc"], ["cacc"])
            yield
        act(csil[:, :, :], cacc[:, :, :], AF.Silu, ["cacc"], ["csil"])
        yield
        tt("pool", csq[:, :, :], csil[:, 0:2, :], csil[:, 0:2, :], ALU.mult, ["csil"], ["csq"])
        yield
        pss = bank(7)
        mm(pss, ones, csq[:, :, :].rearrange("p a b -> p (a b)"), ["cst", "csq"], ["B7"])
        yield
        act(rinv[:, :, :].rearrange("p a b -> p (a b)"), pss, AF.Sqrt, ["B7"], ["rinvA"], bias=EPS, scale=1.0)
        yield
        recip(rinv[:, :, :].rearrange("p a b -> p (a b)"), rinv[:, :, :].rearrange("p a b -> p (a b)"), ["rinvA"], ["rinv"])
        yield
        stt(QnT[:, koff:koff + 256], csil[:, 0, :], 128.0 ** -0.5, rinv[:, 0, :], ALU.mult, ALU.mult, ["csil", "rinv"], ["QnT"])
        yield
        tt("dve", KnT[:, koff:koff + 256], csil[:, 1, :], rinv[:, 1, :], ALU.mult, ["csil", "rinv"], ["KnT"])
        yield
        pvt = bank(7)
        for s in range(2):
            tr(pvt[:, 256 + s * 128:256 + (s + 1) * 128], csil[:, 2, s * 128:(s + 1) * 128], ident, ["csil", "cst"], ["B7"])
            yield
        cp("act", Vg[:, koff // 128:koff // 128 + 2, :], pvt[:, 256:512].rearrange("p (s n) -> p s n", s=2), ["B7"], ["Vg"])
        yield

    def interleave0(*gens):
        gens = list(gens)
        if SEQ[0] == 1:
            for g in gens:
                for _ in g:
                    pass
            return
        while gens:
            for g in list(gens):
                try:
                    next(g)
                except StopIteration:
                    gens.remove(g)

    def conv_for(k):
        left = None if k in (0, 1) else k - 1
        right = None if k in (0, NT - 1) else k + 1
        return conv_stage(k, left, right)

    interleave0(front(0))
    for t in range(NT):
        gl = [back(t)]
        if t + 1 < NT:
            gl.append(front(t + 1))
        if t - 2 >= 0:
            if SEQ[0] == 2:
                interleave0(*gl)
                gl = []
            gl.append(conv_for(t - 2))
        interleave0(*gl)
    interleave0(conv_for(NT - 2))
    interleave0(conv_for(NT - 1))

    dbg("KnT", KnT[:, :], [128, NSC * 128], "KnT")
    dbg("QnT", QnT[:, :], [128, NSC * 128], "QnT")
    dbg("Vg", Vg[:, :, :], [128, NSC, 128], "Vg")
    dbg("abt", abt[:, :, :], [128, NSC, 4], "abt")

    if stage == 1:
        return finish()
    P.barrier()
    A.off = gdn_mark

    og = A.alloc("og", [128, 64, 128], F32)
    mset("pool", og[:, :, :], 0.0, [f"og{i}" for i in range(64)])
    gg = A.alloc("gg", [128, 2, NSC], F32)
    bet = A.alloc("bet", [128, 2, NSC], F32)
    nbet = A.alloc("nbet", [128, 2, NSC], F32)
    Gc = A.alloc("Gc", [128, 2, NSC], F32)
    eG = A.alloc("eG", [128, 2, NSC], F32)
    eGl = A.alloc("eGl", [128, 2, NSC], F32)
    egl = A.alloc("egl", [128, 2, 2, NSC], F32)
    gtmp = A.alloc("gtmp", [128, 2, NSC], F32)
    nea = col(2)
    act(nea, gsc[:, 0:2], AF.Exp, ["gsc"], ["neaA"])
    ts("dve", nea, nea, -1.0, None, ALU.mult, None, ["neaA"], ["nea"])
    for d in range(2):
        act(gtmp[:, d, :], abt[:, :, d], AF.Exp, ["abt", "gsc"], ["gtmpA"], bias=gsc[:, 2 + d:3 + d], scale=1.0)
        act(gtmp[:, d, :], gtmp[:, d, :], AF.Ln, ["gtmpA"], ["gtmpB"], bias=1.0, scale=1.0)
        ts("dve", gg[:, d, :], gtmp[:, d, :], nea[:, d:d + 1], None, ALU.mult, None, ["gtmpB", "nea"], ["gg"])
        act(bet[:, d, :], abt[:, :, 2 + d], AF.Sigmoid, ["abt"], ["bet"])
        ts("dve", nbet[:, d, :], bet[:, d, :], -1.0, None, ALU.mult, None, ["bet"], ["nbet"])
        pg_ = bank(0)
        mm(pg_[:, 0:NSC], tri[d], gg[:, d, :], ["cst", "gg"], ["B0"])
        mm(pg_[:, 128:128 + NSC], blkm, gg[:, d, :], ["cst", "gg"], ["B0"])
        mm(pg_[:, 256:256 + NSC], H0, gg[:, d, :], ["cst", "gg"], ["B0"])
        mm(pg_[:, 384:384 + NSC], H1, gg[:, d, :], ["cst", "gg"], ["B0"])
        cp("dve", Gc[:, d, :], pg_[:, 0:NSC], ["B0"], ["Gc"])
        act(eG[:, d, :], pg_[:, 0:NSC], AF.Exp, ["B0"], ["eG"])
        tt("dve", gtmp[:, d, :], pg_[:, 128:128 + NSC], Gc[:, d, :], ALU.subtract, ["B0", "Gc", "gtmpB"], ["gtmpC"])
        act(eGl[:, d, :], gtmp[:, d, :], AF.Exp, ["gtmpC"], ["eGl"])
        act(egl[:, d, 0, :], pg_[:, 256:256 + NSC], AF.Exp, ["B0"], ["egl"])
        act(egl[:, d, 1, :], pg_[:, 384:384 + NSC], AF.Exp, ["B0"], ["egl"])

    dbg("gg", gg[:, :, :], [128, 2, NSC], "gg")
    dbg("Gc", Gc[:, :, :], [128, 2, NSC], "Gc")

    def pcbuf(name, dt, n=128):
        return [A.alloc(f"{name}{d}", [128, n], dt) for d in range(2)]
    gram = pcbuf("gram", F32, 256)
    Rw = pcbuf("Rw", BF16)
    kdp = [[A.alloc(f"kd{d}_{q}", [128, 128], BF16) for d in range(2)] for q in range(2)]
    qd = pcbuf("qd", BF16)
    qdTp = [[A.alloc(f"qdT{d}_{q}", [128, 128], BF16) for d in range(2)] for q in range(2)]
    dg = pcbuf("dg", F32)
    tE = pcbuf("tE", F32)
    Es = pcbuf("Es", F32)
    Ei = pcbuf("Ei", F32)
    aqkTp = [[A.alloc(f"aqkT{d}_{q}", [128, 128], BF16) for d in range(2)] for q in range(2)]
    Xb = [pcbuf("Xa", BF16), pcbuf("Xb", BF16)]
    Yb = [pcbuf("Ya", BF16), pcbuf("Yb", BF16)]
    Rb = [pcbuf("Ra", BF16), pcbuf("Rb", BF16)]
    ATb = pcbuf("AT", BF16)
    WTp = [[A.alloc(f"WT{d}_{q}", [128, 128], BF16) for d in range(2)] for q in range(2)]
    UBbp = [[A.alloc(f"UBb{d}_{q}", [128, 128], F32) for d in range(2)] for q in range(2)]
    S32 = pcbuf("S32", F32)
    Sbf = pcbuf("Sbf", BF16)
    ub = pcbuf("ub", BF16)
    for d in range(2):
        mset("pool", S32[d][:, :], 0.0, [f"S32{d}"])
        mset("pool", Sbf[d][:, :], 0.0, [f"Sbf{d}"])

    slot = [0]

    def pslot():
        s = slot[0] % 4
        slot[0] += 1
        return bank(s)[:, 0:128], f"B{s}"

    def pslot_bf():
        ap_, k_ = pslot()
        return ap_[:, 0:64].bitcast(BF16), k_

    def precompute(sc, d, par):
        WT, UBb, kd, qdT, aqkT = WTp[par], UBbp[par], kdp[par], qdTp[par], aqkTp[par]
        pq = str(par)
        lat = sc >= 2
        c0 = sc * 128
        sd = str(d)
        p1, k1 = pslot()
        mm(p1, KnT[:, c0:c0 + 128], KnT[:, c0:c0 + 128], ["KnT"], [k1])
        yield
        cp("act", gram[d][:, 0:128], p1, [k1], ["gramA" + sd])
        yield
        if lat:
            p2, k2 = pslot()
            mm(p2, KnT[:, c0:c0 + 128], QnT[:, c0:c0 + 128], ["KnT", "QnT"], [k2])
            yield
            cp("act", gram[d][:, 128:256], p2, [k2], ["gramB" + sd])
            yield
        p3, k3 = pslot_bf()
        tr(p3, KnT[:, c0:c0 + 128], identb[:, :], ["KnT", "identb"], [k3])
        yield
        act(Rw[d][:, :], p3, AF.Identity, [k3, "eG"], ["Rw" + sd], scale=eG[:, d, sc:sc + 1])
        yield
        ts("dve", kd[d][:, :], p3, eGl[:, d, sc:sc + 1], None, ALU.mult, None, [k3, "eGl"], ["kd" + sd + pq])
        yield
        if lat:
            p4, k4 = pslot_bf()
            tr(p4, QnT[:, c0:c0 + 128], identb[:, :], ["QnT", "identb"], [k4])
            yield
            act(qd[d][:, :], p4, AF.Identity, [k4, "eG"], ["qd" + sd], scale=eG[:, d, sc:sc + 1])
            yield
            p5, k5 = pslot_bf()
            tr(p5, qd[d][:, :], identb[:, :], ["qd" + sd, "identb"], [k5])
            yield
            cp("act", qdT[d][:, :], p5, [k5], ["qdT" + sd + pq])
            yield
        ts("pool", dg[d][:, :], ident, Gc[:, d, sc:sc + 1], 1.0, ALU.mult, ALU.mult, ["cst", "Gc"], ["dg" + sd])
        yield
        p6, k6 = pslot()
        mm(p6, ones, dg[d][:, :], ["cst", "dg" + sd], [k6])
        yield
        ts("dve", tE[d][:, :], p6, Gc[:, d, sc:sc + 1], 0.0, ALU.subtract, ALU.min, [k6, "Gc"], ["tEA" + sd])
        yield
        act(tE[d][:, :], tE[d][:, :], AF.Exp, ["tEA" + sd], ["tE" + sd])
        yield
        tt("pool", Es[d][:, :], tE[d][:, :], mS[d], ALU.mult, ["tE" + sd, "cst"], ["Es" + sd])
        yield
        X0 = Xb[0][d]
        stt(X0[:, :], gram[d][:, 0:128], nbet[:, d, sc:sc + 1], Es[d][:, :], ALU.mult, ALU.mult, ["gramA" + sd, "nbet", "Es" + sd], ["X0" + sd])
        yield
        if lat:
            tt("pool", Ei[d][:, :], tE[d][:, :], mI[d], ALU.mult, ["tE" + sd, "cst"], ["Ei" + sd])
            yield
            tt("dve", aqkT[d][:, :], gram[d][:, 128:256], Ei[d][:, :], ALU.mult, ["gramB" + sd, "Ei" + sd], ["aqkT" + sd + pq])
            yield
        p7, k7 = pslot_bf()
        tr(p7, X0[:, :], identb[:, :], ["X0" + sd, "identb"], [k7])
        yield
        Y0 = Yb[0][d]
        cp("act", Y0[:, :], p7, [k7], ["Y0" + sd])
        yield
        R0 = Rb[0][d]
        tt("pool", R0[:, :], X0[:, :], ident, ALU.add, ["X0" + sd, "cst"], ["R0" + sd])
        yield
        for lv in range(1, 6):
            a_, b_ = (lv - 1) % 2, lv % 2
            Xp, Yp, Rp = Xb[a_][d], Yb[a_][d], Rb[a_][d]
            Xn, Yn, Rn = Xb[b_][d], Yb[b_][d], Rb[b_][d]
            kXp, kYp, kRp = f"X{a_}{sd}", f"Y{a_}{sd}", f"R{a_}{sd}"
            kXn, kYn, kRn = f"X{b_}{sd}", f"Y{b_}{sd}", f"R{b_}{sd}"
            py, ky = pslot()
            mm(py, Xp[:, :], Yp[:, :], [kXp, kYp], [ky])
            yield
            if lv <= 4:
                px, kx_ = pslot()
                mm(px, Yp[:, :], Xp[:, :], [kXp, kYp], [kx_])
                yield
            cp("act", Yn[:, :], py, [ky], [kYn])
            yield
            if lv <= 4:
                cp("dve", Xn[:, :], px, [kx_], [kXn])
                yield
            pr_, kr_ = pslot()
            mm(pr_, Yn[:, :], Rp[:, :], [kYn, kRp], [kr_])
            yield
            if lv < 5:
                tt("dve", Rn[:, :], pr_, Rp[:, :], ALU.add, [kr_, kRp], [kRn])
                yield
            else:
                tt("dve", ATb[d][:, :], pr_, Rp[:, :], ALU.add, [kr_, kRp], ["AT" + sd])
                yield
        p8, k8 = pslot()
        mm(p8, Rw[d][:, :], ATb[d][:, :], ["Rw" + sd, "AT" + sd], [k8])
        yield
        cp("act", WT[d][:, :], p8, [k8], ["WT" + sd + pq])
        yield
        p9, k9 = pslot()
        mm(p9, ATb[d][:, :], Vg[:, sc, :], ["AT" + sd, "Vg"], [k9])
        yield
        act(UBb[d][:, :], p9, AF.Identity, [k9, "bet"], ["UBb" + sd + pq], scale=bet[:, d, sc:sc + 1])
        yield

    def step(sc, hh, d, par):
        WT, UBb, kd, qdT, aqkT = WTp[par], UBbp[par], kdp[par], qdTp[par], aqkTp[par]
        pq = str(par)
        lat = sc >= 2
        sd = str(d)
        r0, r1 = hh * 64, hh * 64 + 64
        bA = bank(4 + 2 * d)
        bO = bank(5 + 2 * d)
        pws = bA[r0:r1, 0:128]
        mm(pws, WT[d][:, r0:r1], Sbf[d][:, :], ["WT" + sd + pq, "Sbf" + sd], [f"B{4 + 2 * d}"])
        yield
        stt(ub[d][r0:r1, :], pws, nbet[r0:r1, d, sc:sc + 1], UBb[d][r0:r1, :], ALU.mult, ALU.add, [f"B{4 + 2 * d}", "nbet", "UBb" + sd + pq], ["ub" + sd])
        yield
        if lat:
            po = bO[r0:r1, 0:128]
            mm(po, qdT[d][:, r0:r1], Sbf[d][:, :], ["qdT" + sd + pq, "Sbf" + sd], [f"B{5 + 2 * d}"], start=True, stop=False)
            yield
            mm(po, aqkT[d][r0:r1, r0:r1], ub[d][r0:r1, :], ["aqkT" + sd + pq, "ub" + sd], [f"B{5 + 2 * d}"], start=False, stop=True)
            yield
            tt("dve", og[r0:r1, sc - 2, :], po, og[r0:r1, sc - 2, :], ALU.add, [f"B{5 + 2 * d}", f"og{sc - 2}"], [f"og{sc - 2}"])
            yield
        pS = bA[:, 128:256]
        mm(pS, kd[d][r0:r1, :], ub[d][r0:r1, :], ["kd" + sd + pq, "ub" + sd], [f"B{4 + 2 * d}"])
        yield
        stt(S32[d][:, :], S32[d][:, :], egl[:, d, hh, sc:sc + 1], pS, ALU.mult, ALU.add, ["S32" + sd, "egl", f"B{4 + 2 * d}"], ["S32" + sd])
        yield
        cp("act", Sbf[d][:, :], S32[d][:, :], ["S32" + sd], ["Sbf" + sd])
        yield

    fwd = list(range(NSC))
    bwd = [1, 0] + list(range(NSC - 1, 1, -1))
    def seq(*gs):
        for g in gs:
            yield from g

    def interleave(*gens):
        gens = list(gens)
        while gens:
            for g in list(gens):
                try:
                    next(g)
                except StopIteration:
                    gens.remove(g)

    interleave(precompute(fwd[0], 0, 0), precompute(bwd[0], 1, 0))
    for i in range(NSC):
        par = i % 2
        gl = [seq(step(fwd[i], 0, 0, par), step(fwd[i], 1, 0, par)), seq(step(bwd[i], 1, 1, par), step(bwd[i], 0, 1, par))]
        if i + 1 < NSC:
            gl += [precompute(fwd[i + 1], 0, 1 - par), precompute(bwd[i + 1], 1, 1 - par)]
        interleave(*gl)

    P.op("pool", lambda e: e.memset(small[:, 500:501], 0.0), reads=[f"og{i}" for i in range(64)], writes=["og_all"])
    dbg("og", og[:, :, :], [128, 64, 128], "og_all")

    ogb = A.alloc("ogb", [128, 64, 128], BF16)
    ogs = A.alloc("ogs", [128, 64], F32)
    ogr = A.alloc("ogr", [128, 64], F32)
    ogt = A.alloc("ogt", [128, 128], F32)
    sqj2 = A.alloc("sqj2", [128, 128], BF16)
    for i in range(64):
        act(sqj2[:, :], og[:, i, :], AF.Square, ["og_all"], ["sqj2", "ogs"], accum=ogs[:, i:i + 1])
    act(ogr[:, :], ogs[:, :], AF.Sqrt, ["ogs"], ["ogrA"], bias=EPS, scale=1.0 / 128)
    recip(ogr[:, :], ogr[:, :], ["ogrA"], ["ogr"])
    for i in range(64):
        stt(ogt[:, :], og[:, i, :], ogr[:, i:i + 1], gng[:, :], ALU.mult, ALU.mult, ["og_all", "ogr", "gng"], ["ogt"])
        tt("dve", ogb[:, i, :], ogt[:, :], gate_s[:, i, :], ALU.mult, ["ogt", "gate_s"], ["ogb"])
    dbg("ogb", ogb[:, :, :], [128, 64, 128], "ogb")
    if stage == 2:
        return finish()

    P.barrier()
    after_gdn = A.off
    A.off = persist_mark
    KT0 = A.alloc("KT0", [128, LC + L], BF16)
    KT1 = A.alloc("KT1", [128, LC + L], BF16)
    QT = A.alloc("QT", [128, L], BF16)
    Vv = A.alloc("Vv", [128, NSC, 130], BF16)
    assert A.off <= gdn_mark, A.off
    lim1 = A.off
    A.off = after_gdn
    wout = A.alloc("wout", [128, 2, D], BF16)
    pbuf = [A.alloc(f"pb{i}", [128, 512], BF16) for i in range(4)]
    oa = A.alloc("oa", [128, 128], F32)
    ob = A.alloc("ob", [128, 128], F32)
    omx = A.alloc("omx", [128, 128], BF16)
    omT = [A.alloc(f"omT{i}", [128, 128], BF16) for i in range(2)]
    rsst = [A.alloc(f"rsst{i}", [128, D], F32) for i in range(2)]
    att = A.alloc("att", [128, 16], F32)
    sqj3 = A.alloc("sqj3", [128, 128], BF16)
    for kc in range(4):
        dma("sp", f"ld{kc % 2}", KT0[0:64, kc * 2112:(kc + 1) * 2112], kt_s[0:64, kc * 2112:(kc + 1) * 2112], ["ktqt_s"], ["KT0a"])
        dma("sp", f"ld{kc % 2}", KT1[64:128, kc * 2112:(kc + 1) * 2112], kt_s[64:128, kc * 2112:(kc + 1) * 2112], ["ktqt_s"], ["KT1a"])
        dma("sp", f"ld{kc % 2}", QT[:, kc * 2048:(kc + 1) * 2048], qt_s[:, kc * 2048:(kc + 1) * 2048], ["ktqt_s"], ["QT"])
    mset("pool", KT0[64:128, :], 0.0, ["KT0b"])
    mset("pool", KT1[0:64, :], 0.0, ["KT1b"])
    for kc in range(6):
        dma("sp", f"ld{kc % 2}", Vv[:, kc * 11:(kc + 1) * 11, :], v_s[kc * 11:(kc + 1) * 11, :, :].rearrange("s p n -> p s n"), ["v_s"], ["Vv"])
    for f_ in range(2):
        dma("pool", f"wo{f_}", wout[:, f_, :], wout_d[f_, :, :], (), ["wout"])

    NKT = NSC
    pcnt = [0]
    if stage == 2.91:
        dbg("KT", KT0[:, :], [128, LC + L], "KT0a")
        dbg("Vv", Vv[:, :, :], [128, NSC, 130], "Vv")
        return finish()
    qorder = [(sblk, rr, qq) for sblk in range(4) for rr in range(4) for qq in range(2)]
    SB = [0, 1, 7]

    def q0_of(ent):
        sblk_, rr_, qq_ = ent
        return rr_ * 2048 + sblk_ * 512 + qq_ * 256

    def s_mm(q0, kt_):
        sb_ = SB[kt_ % 3]
        pS_ = bank(sb_)
        mm(pS_[:, 0:256], KT0[:, kt_ * 128:(kt_ + 1) * 128], QT[:, q0:q0 + 256], ["KT0a", "KT0b", "QT"], [f"B{sb_}"])
        mm(pS_[:, 256:512], KT1[:, kt_ * 128:(kt_ + 1) * 128], QT[:, q0:q0 + 256], ["KT1a", "KT1b", "QT"], [f"B{sb_}"])

    qlist = qorder[:1] if stage in (2.92, 2.93) else qorder
    s_mm(q0_of(qlist[0]), 0)
    s_mm(q0_of(qlist[0]), 1)
    for qi_, (sblk, rr, qq) in enumerate(qlist):
        q0 = q0_of((sblk, rr, qq))
        qt_ = q0 // 256
        for kt_ in range(NKT):
            sb_ = SB[kt_ % 3]
            pi = pcnt[0] % 4
            pcnt[0] += 1
            if kt_ + 2 < NKT:
                s_mm(q0, kt_ + 2)
            act(pbuf[pi][:, :], bank(sb_), AF.Exp, [f"B{sb_}"], [f"pb{pi}"], scale=0.125)
            for sub in range(2):
                for mp in range(2):
                    acc = bank(2 + sub * 2 + mp)[:, 0:129]
                    mm(acc, pbuf[pi][:, mp * 256 + sub * 128:mp * 256 + (sub + 1) * 128], Vv[:, kt_, 0:129], [f"pb{pi}", "Vv"],
                       [f"B{2 + sub * 2 + mp}"], start=(kt_ == 0), stop=(kt_ == NKT - 1))
        if qi_ + 1 < len(qlist):
            s_mm(q0_of(qlist[qi_ + 1]), 0)
            s_mm(q0_of(qlist[qi_ + 1]), 1)
        for sub in range(0 if stage == 2.93 else 2):
            ti = qt_ * 2 + sub
            a0 = bank(2 + sub * 2)
            a1_ = bank(3 + sub * 2)
            recip(att[:, 0:1], a0[:, 128:129], [f"B{2 + sub * 2}"], ["att0"])
            recip(att[:, 1:2], a1_[:, 128:129], [f"B{3 + sub * 2}"], ["att1"])
            tt("dve", att[:, 2:3], att[:, 1:2], nlam, ALU.mult, ["att1", "nlam"], ["att2"])
            ts("dve", oa[:, :], a0[:, 0:128], att[:, 0:1], None, ALU.mult, None, [f"B{2 + sub * 2}", "att0"], ["oa"])
            stt(ob[:, :], a1_[:, 0:128], att[:, 2:3], oa[:, :], ALU.mult, ALU.add, [f"B{3 + sub * 2}", "att2", "oa"], ["ob"])
            act(sqj3[:, :], ob[:, :], AF.Square, ["ob"], ["sqj3", "att3"], accum=att[:, 3:4])
            act(att[:, 4:5], att[:, 3:4], AF.Sqrt, ["att3"], ["att4"], bias=EPS, scale=1.0 / 128)
            recip(att[:, 5:6], att[:, 4:5], ["att4"], ["att5"])
            ts("dve", oa[:, :], ob[:, :], att[:, 5:6], 1.0 - LAM_INIT, ALU.mult, ALU.mult, ["ob", "att5"], ["oa"])
            tt("dve", omx[:, :], oa[:, :], sgg[:, :], ALU.mult, ["oa", "sgg"], ["omx"])
            pt_ = bank(6)
            ptb = pt_[:, 0:128].bitcast(BF16)
            tr(ptb[:, 0:128], omx[:, :], identb[:, :], ["omx", "identb"], ["B6"])
            tr(ptb[:, 128:256], ogb[:, ti, :], identb[:, :], ["ogb", "identb"], ["B6"])
            cp("act", omT[0][:, :], ptb[:, 0:128], ["B6"], ["omT0"])
            cp("act", omT[1][:, :], ptb[:, 128:256], ["B6"], ["omT1"])
            rb = rsst[ti % 2]
            for nh in range(2):
                po_ = bank(6)
                mm(po_, omT[0][:, :], wout[:, 0, nh * 512:(nh + 1) * 512], ["omT0", "wout"], ["B6"], start=True, stop=False)
                mm(po_, omT[1][:, :], wout[:, 1, nh * 512:(nh + 1) * 512], ["omT1", "wout"], ["B6"], start=False, stop=True)
                if nh == 0:
                    cp("act", rb[:, 0:512], po_, ["B6"], [f"rsst{ti % 2}"])
                else:
                    cp("dve", rb[:, 512:1024], po_, ["B6"], [f"rsst{ti % 2}"])
            rrow = rr * 512 + qq * 256 + sub * 128
            dma("pool", f"rs{ti % 2}", rs_in[sblk].ap()[rrow:rrow + 128, :], rb[:, :], [f"rsst{ti % 2}"], [f"rs_in{sblk}"])
        if rr == 3 and qq == 1 and stage >= 3:
            P.dma("pool", f"ccrs{sblk}", lambda e, sblk=sblk: e.collective_compute(
                "ReduceScatter", ALU.add, replica_groups=[[0, 1, 2, 3], [4, 5, 6, 7]],
                ins=[rs_in[sblk].ap().opt()], outs=[rs_out[sblk].ap().opt()]), reads=[f"rs_in{sblk}"], writes=[f"rs_out{sblk}"], inc=1)

    if 2.9 <= stage < 3:
        return finish()
    if stage == 3:
        return finish()
    P.barrier()
    A.off = persist_mark
    h2T = A.alloc("h2T", [128, 8, 2048], BF16)
    aff = A.alloc("aff", [128, 16, 16], F32)
    gw = A.alloc("gw", [128, 16, 16], F32)
    gt1bc = A.alloc("gt1bc", [128, D], F32)
    gt2bc = A.alloc("gt2bc", [128, D], F32)
    fgbc = A.alloc("fgbc", [128, D], F32)
    wr = A.alloc("wr", [128, 8, 16], BF16)
    sh2T = col(8)
    sc2T = col(8)
    a2 = col(8)
    n2 = A.alloc("n2", [128, 16], F32)
    ex = A.alloc("ex", [128, 16], F32)
    bs = A.alloc("bs", [64, 8], F32)
    taud = A.alloc("taud", [16, 16], F32)
    taubc = A.alloc("taubc", [128, 16], F32)
    xot = [A.alloc(f"xot{i}", [128, D], F32) for i in range(2)]
    rst = [A.alloc(f"rst{i}", [128, D], F32) for i in range(2)]
    xnw = [A.alloc(f"xnw{i}", [128, D], F32) for i in range(2)]
    sqj4 = A.alloc("sqj4", [128, D], BF16)
    m3_mark = A.off
    modrow = A.alloc("modrow", [1, 4096], F32)
    bm2 = A.alloc("bm2", [1, 4096], F32)
    p3_mark = A.off
    wm2 = A.alloc("wm2", [128, 8, 4096], BF16)
    for kc in range(8):
        dma("pool", f"wm{kc % 2}", wm2[:, kc, :], wmod[kc * 128:(kc + 1) * 128, 2048:6144], (), [f"wm2_{kc}"])
    dma("sp", "sm0", bm2[:, :], bm2_d, (), ["bm2"])
    dma("sp", "sm1", fgbc[:, :], fgbc_d, (), ["fgbc"])
    dma("pool", "wab", wr[:, :, :].rearrange("p a b -> p (a b)"), wr_d, (), ["wr"])
    for cb in range(8):
        pm_ = bank(cb % 2)[0:1, :]
        for kc in range(8):
            mm(pm_, scb[:, kc, 0:1], wm2[:, kc, cb * 512:(cb + 1) * 512], ["scb", f"wm2_{kc}"], [f"B{cb % 2}"], start=(kc == 0), stop=(kc == 7))
        tt("dve", modrow[0:1, cb * 512:(cb + 1) * 512], pm_, bm2[0:1, cb * 512:(cb + 1) * 512], ALU.add, [f"B{cb % 2}", "bm2"], ["modrow"])
    for nh in range(2):
        pb_ = bank(2)
        mm(pb_, ones[0:1, :], modrow[0:1, nh * 512:(nh + 1) * 512], ["cst", "modrow"], ["B2"])
        cp("act", gt1bc[:, nh * 512:(nh + 1) * 512], pb_, ["B2"], ["gt1bc"])
        mm(pb_, ones[0:1, :], modrow[0:1, 3072 + nh * 512:3072 + (nh + 1) * 512], ["cst", "modrow"], ["B2"])
        cp("act", gt2bc[:, nh * 512:(nh + 1) * 512], pb_, ["B2"], ["gt2bc"])
    pc_ = bank(3)
    for kc in range(8):
        mm(pc_[:, kc:kc + 1], modrow[0:1, 1024 + kc * 128:1024 + (kc + 1) * 128], ones[0:1, 0:1], ["modrow", "cst"], ["B3"])
        mm(pc_[:, 8 + kc:9 + kc], modrow[0:1, 2048 + kc * 128:2048 + (kc + 1) * 128], ones[0:1, 0:1], ["modrow", "cst"], ["B3"])
    cp("dve", sh2T, pc_[:, 0:8], ["B3"], ["sh2T"])
    cp("dve", sc2T, pc_[:, 8:16], ["B3"], ["sc2T"])
    stt(a2, sc2T, 1.0, g2T, ALU.add, ALU.mult, ["sc2T", "g2T"], ["a2"])
    P.barrier()
    A.off = p3_mark
    xn2 = [A.alloc(f"xn2_{i}", [128, D], F32) for i in range(2)]
    exs = [ex, A.alloc("exb", [128, 16], F32)]
    affT = A.alloc("affT", [16, 2048], F32)
    affall = A.alloc("affall", [64, 2048], F32)
    cmpj = A.alloc("cmpj", [64, 2048], BF16)
    def p2_front(i):
        b_ = i % 2
        c0 = b_ * 3
        dma("sp", f"xo{b_}", xot[b_][:, :], xo[i * 128:(i + 1) * 128, :], (), [f"xot{b_}"])
        yield
        dma("sp", f"rsl{b_}", rst[b_][:, :], rs_out[i // 4].ap()[(i % 4) * 128:(i % 4 + 1) * 128, :], [f"rs_out{i // 4}"], [f"rst{b_}"])
        yield
        tt("pool", rst[b_][:, :], rst[b_][:, :], gt1bc[:, :], ALU.mult, [f"rst{b_}", "gt1bc"], [f"rst{b_}"])
        yield
        tt("dve", xnw[b_][:, :], rst[b_][:, :], xot[b_][:, :], ALU.add, [f"rst{b_}", f"xot{b_}"], [f"xnw{b_}"])
        yield
        dma("pool", f"xns{b_}", xnew_s[i * 128:(i + 1) * 128, :], xnw[b_][:, :], [f"xnw{b_}"], ["xnew_s"])
        yield
        act(sqj4[:, :], xnw[b_][:, :], AF.Square, [f"xnw{b_}"], ["sqj4", f"n2a{b_}"], accum=n2[:, c0:c0 + 1])
        yield
        act(n2[:, c0 + 1:c0 + 2], n2[:, c0:c0 + 1], AF.Sqrt, [f"n2a{b_}"], [f"n2b{b_}"], bias=EPS, scale=1.0 / D)
        yield
        recip(n2[:, c0 + 2:c0 + 3], n2[:, c0 + 1:c0 + 2], [f"n2b{b_}"], [f"n2c{b_}"])
        yield
        ts("pool", xn2[b_][:, :], xnw[b_][:, :], n2[:, c0 + 2:c0 + 3], 1.0, ALU.mult, ALU.mult, [f"xnw{b_}", f"n2c{b_}"], [f"xn2{b_}"])
        yield

    def p2_back(i):
        b_ = i % 2
        c0 = 6 + b_ * 3
        for kc in range(8):
            pb2 = kc % 2
            pT = bank(pb2)[:, 0:128]
            tr(pT, xn2[b_][:, kc * 128:(kc + 1) * 128], ident, [f"xn2{b_}", "cst"], [f"B{pb2}"])
            yield
            if kc % 2 == 0:
                act(h2T[:, kc, i * 128:(i + 1) * 128], pT, AF.Identity, [f"B{pb2}", "a2", "sh2T"], [f"h2T_{kc}"], bias=sh2T[:, kc:kc + 1], scale=a2[:, kc:kc + 1])
            else:
                ts("dve", h2T[:, kc, i * 128:(i + 1) * 128], pT, a2[:, kc:kc + 1], sh2T[:, kc:kc + 1], ALU.mult, ALU.add, [f"B{pb2}", "a2", "sh2T"], [f"h2T_{kc}"])
            yield
        pl = bank(2)[:, 0:16]
        for kc in range(8):
            mm(pl, h2T[:, kc, i * 128:(i + 1) * 128], wr[:, kc, :], [f"h2T_{kc}", "wr"], ["B2"], start=(kc == 0), stop=(kc == 7))
        yield
        P.op("dve", lambda e, pl=pl, c0=c0: e.tensor_reduce(out=n2[:, c0:c0 + 1], in_=pl, axis=AX.X, op=ALU.max, negate=True), reads=["B2"], writes=[f"n2d{b_}"])
        yield
        act(exs[b_][:, :], pl, AF.Exp, ["B2", f"n2d{b_}"], [f"ex{b_}", f"n2e{b_}"], bias=n2[:, c0:c0 + 1], scale=1.0, accum=n2[:, c0 + 1:c0 + 2])
        yield
        recip(n2[:, c0 + 2:c0 + 3], n2[:, c0 + 1:c0 + 2], [f"n2e{b_}"], [f"n2f{b_}"])
        yield
        ts("dve", aff[:, i, :], exs[b_][:, :], n2[:, c0 + 2:c0 + 3], None, ALU.mult, None, [f"ex{b_}", f"n2f{b_}"], ["aff"])
        yield
        pa_ = bank(3)[0:16, 0:128]
        tr(pa_, aff[:, i, :], ident, ["aff", "cst"], ["B3"])
        yield
        cp("act", affT[:, i * 128:(i + 1) * 128], pa_, ["B3"], ["affT"])
        yield

    interleave(p2_front(0))
    for i in range(16):
        gl = [p2_back(i)]
        if i + 1 < 16:
            gl.append(p2_front(i + 1))
        interleave(*gl)
    dma("pool", "agi", ag_in.ap()[:, :], affT[:, :], ["affT"], ["ag_in"])
    P.dma("pool", "ccag", lambda e: e.collective_compute(
        "AllGather", ALU.bypass, replica_groups=[[0, 1, 2, 3], [4, 5, 6, 7]],
        ins=[ag_in.ap().opt()], outs=[ag_out.ap().opt()]), reads=["ag_in"], writes=["ag_out"], inc=1)
    dma("sp", "ago", affall[:, :], ag_out.ap()[:, :], ["ag_out"], ["affall"])
    mset("pool", bs[:, 0:1], 0.0, ["lo"])
    G64 = C(12)
    for it in range(26):
        hw = 2.0 ** -(it + 1)
        ts("dve", bs[:, 1:2], bs[:, 0:1], hw, None, ALU.add, None, ["lo"], ["mid"])
        ts("dve", cmpj[:, :], affall[:, :], bs[:, 1:2], 0.0, ALU.is_ge, ALU.add, ["affall", "mid"], ["cmpj", "cnt"], accum=bs[:, 2:3])
        pc2 = bank(4)[0:64, 0:1]
        mm(pc2, G64[0:64, 0:64], bs[:, 2:3], ["cst", "cnt"], ["B4"])
        ts("dve", bs[:, 3:4], pc2, 1023.5, hw, ALU.is_ge, ALU.mult, ["B4"], ["gd"])
        tt("dve", bs[:, 0:1], bs[:, 0:1], bs[:, 3:4], ALU.add, ["lo", "gd"], ["lo"])
    ts("dve", taud[:, :], ident[0:16, 0:16], bs[0:16, 0:1], None, ALU.mult, None, ["cst", "lo"], ["taud"])
    ptau = bank(5)[:, 0:16]
    mm(ptau, ones[0:16, :], taud[:, :], ["cst", "taud"], ["B5"])
    cp("dve", taubc[:, :], ptau, ["B5"], ["taubc"])
    for i in range(16):
        tt("dve", gw[:, i, :], aff[:, i, :], taubc[:, :], ALU.is_ge, ["aff", "taubc"], ["gwA"])
        tt("dve", gw[:, i, :], gw[:, i, :], aff[:, i, :], ALU.mult, ["gwA", "aff"], ["gw"])
    dbg("xnew", xnew_s[:, :], [2048, D], "xnew_s")
    dbg("taubc", taubc[:, :], [128, 16], "taubc")
    dbg("aff", aff[:, :, :], [128, 16, 16], "aff")
    dbg("gw", gw[:, :, :], [128, 16, 16], "gw")

    if stage == 4:
        return finish()
    P.barrier()
    A.off = m3_mark
    yacc = A.alloc("yacc", [128, 16, D], F32)
    mset("pool", yacc[:, :, :], 0.0, [f"yacc{a}_{b}" for a in range(16) for b in range(2)])
    wgs = [A.alloc(f"wgs{i}", [128, 8, 512], BF16) for i in range(2)]
    wus = [A.alloc(f"wus{i}", [128, 8, 512], BF16) for i in range(2)]
    wds = [A.alloc(f"wds{i}", [128, 4, D], BF16) for i in range(2)]
    sg = [A.alloc(f"sg{i}", [128, 512], F32) for i in range(1)]
    hid = [A.alloc(f"hid{i}", [128, 512], BF16) for i in range(8)]
    gcnt = [0]

    def gu(e_, hf, TB, ws):
        for fc in range(4):
            gp = gcnt[0] % 2
            gcnt[0] += 1
            bG, bU = gp * 2, gp * 2 + 1
            pG, pU = bank(bG), bank(bU)
            for kc in range(8):
                mm(pG, wgs[ws][:, kc, fc * 128:(fc + 1) * 128], h2T[:, kc, TB * 512:(TB + 1) * 512], [f"wgs{ws}", f"h2T_{kc}"], [f"B{bG}"],
                   start=(kc == 0), stop=(kc == 7))
            for kc in range(8):
                mm(pU, wus[ws][:, kc, fc * 128:(fc + 1) * 128], h2T[:, kc, TB * 512:(TB + 1) * 512], [f"wus{ws}", f"h2T_{kc}"], [f"B{bU}"],
                   start=(kc == 0), stop=(kc == 7))
            act(sg[0][:, :], pG, AF.Silu, [f"B{bG}"], ["sg0"])
            hi = (TB % 2) * 4 + fc
            tt("dve", hid[hi][:, :], sg[0][:, :], pU, ALU.mult, ["sg0", f"B{bU}"], [f"hid{hi}"])

    def down(e_, hf, TB, ws):
        for half in range(2):
            for sub in range(2):
                for nh in range(2):
                    bY = 4 + sub * 2 + nh
                    py_ = bank(bY)
                    c0 = half * 256 + sub * 128
                    for fc in range(4):
                        hi = (TB % 2) * 4 + fc
                        mm(py_, hid[hi][:, c0:c0 + 128], wds[ws][:, fc, nh * 512:(nh + 1) * 512], [f"hid{hi}", f"wds{ws}"],
                           [f"B{bY}"], start=(fc == 0), stop=(fc == 3))
                    ti = TB * 4 + half * 2 + sub
                    stt(yacc[:, ti, nh * 512:(nh + 1) * 512], py_, gw[:, ti, e_:e_ + 1], yacc[:, ti, nh * 512:(nh + 1) * 512], ALU.mult, ALU.add,
                        [f"B{bY}", "gw", f"yacc{ti}_{nh}"], [f"yacc{ti}_{nh}"])

    prev = None
    for e_ in range(16):
        for hf in range(2):
            ws = (e_ * 2 + hf) % 2
            for kk in range(2):
                dma("pool", f"wg{ws}{kk}", wgs[ws][:, kk * 4:(kk + 1) * 4, :],
                    wg_d[e_, kk * 512:(kk + 1) * 512, hf * 512:(hf + 1) * 512].rearrange("(kc p) f -> p kc f", p=128), (), [f"wgs{ws}"])
                dma("pool", f"wu{ws}{kk}", wus[ws][:, kk * 4:(kk + 1) * 4, :],
                    wu_d[e_, kk * 512:(kk + 1) * 512, hf * 512:(hf + 1) * 512].rearrange("(kc p) f -> p kc f", p=128), (), [f"wus{ws}"])
                dma("pool", f"wd{ws}{kk}", wds[ws][:, kk * 2:(kk + 1) * 2, :],
                    wd_d[e_, hf * 512 + kk * 256:hf * 512 + (kk + 1) * 256, :].rearrange("(fc p) n -> p fc n", p=128), (), [f"wds{ws}"])
            for TB in range(4):
                gu(e_, hf, TB, ws)
                if prev is not None:
                    down(*prev)
                prev = (e_, hf, TB, ws)
    down(*prev)


    for i in range(16):
        b_ = i % 2
        dma("sp", f"xo{b_}", xot[b_][:, :], xnew_s[i * 128:(i + 1) * 128, :], ["xnew_s"], [f"xot{b_}"])
        tt("pool", rst[b_][:, :], yacc[:, i, :], gt2bc[:, :], ALU.mult, [f"yacc{i}_0", f"yacc{i}_1", "gt2bc"], [f"rst{b_}"])
        tt("dve", xnw[b_][:, :], rst[b_][:, :], xot[b_][:, :], ALU.add, [f"rst{b_}", f"xot{b_}"], [f"xnw{b_}"])
        act(sqj4[:, :], xnw[b_][:, :], AF.Square, [f"xnw{b_}"], ["sqj4", "n2a"], accum=n2[:, 0:1])
        act(n2[:, 1:2], n2[:, 0:1], AF.Sqrt, ["n2a"], ["n2b"], bias=EPS, scale=1.0 / D)
        recip(n2[:, 2:3], n2[:, 1:2], ["n2b"], ["n2c"])
        stt(rst[b_][:, :], xnw[b_][:, :], n2[:, 2:3], fgbc[:, :], ALU.mult, ALU.mult, [f"xnw{b_}", "n2c", "fgbc"], [f"rst{b_}"])
        dma("pool", f"out{b_}", out_d[i * 128:(i + 1) * 128, :], rst[b_][:, :], [f"rst{b_}"], [f"out{b_}"])
    return finish()


_CACHE = {}


def kernel(x, c, ctx, c_ctx, w_mod, b_mod, norm1_g, w_in, conv_w, a_log, dt_bias, gdn_norm_g,
           lam_q1, lam_k1, lam_q2, lam_k2, da_subln_g, w_out, norm2_g,
           w_router, w_gate, w_up, w_down, final_g):
    f32 = np.float32
    A_ = lambda a: np.ascontiguousarray(np.asarray(a, dtype=f32))
    x, c, ctx, c_ctx = A_(x), A_(c), A_(ctx), A_(c_ctx)
    w_mod, b_mod, w_in, conv_w = A_(w_mod)[0], A_(b_mod)[0], A_(w_in)[0], A_(conv_w)[0]
    a_log, dt_bias = A_(a_log)[0], A_(dt_bias)[0]
    w_out, w_router = A_(w_out)[0], A_(w_router)[0]
    w_gate, w_up, w_down = A_(w_gate)[0], A_(w_up)[0], A_(w_down)[0]
    norm1_g, norm2_g, final_g = A_(norm1_g)[0], A_(norm2_g)[0], A_(final_g)
    gdn_norm_g, da_subln_g = A_(gdn_norm_g)[0], A_(da_subln_g)[0]
    lamcat = np.concatenate([A_(lam_q1)[0], A_(lam_k1)[0], A_(lam_q2)[0], A_(lam_k2)[0]])

    if MAPS_ONLY[0]:
        nc = None
    else:
        key = (tuple(DEBUG), STAGE[0])
        if key not in _CACHE:
            _CACHE[key] = build(debug=key[0], stage=key[1])
        nc, dbg_outs = _CACHE[key]

    consts = make_consts()
    rope = make_rope()

    def colT(v, n):
        return np.ascontiguousarray(v.reshape(n, 128).T)

    in_maps = []
    for core in range(8):
        b, h = core // 4, core % 4
        j = h
        cT = np.zeros((128, 8, 2), f32)
        cT[:, :, 0] = colT(c[b], 8)
        cT[:, :, 1] = colT(c_ctx, 8)
        cols = np.concatenate([
            np.arange(h * 128, h * 128 + 128),
            512 + np.arange(h * 128, h * 128 + 128),
            1536 + np.arange(h * 128, h * 128 + 128),
            1536 + 512 + np.arange(h * 128, h * 128 + 128),
            1536 + 1024 + np.arange(h * 128, h * 128 + 128),
            1024 + np.arange(h * 128, h * 128 + 128),
            3072 + np.arange(h * 128, h * 128 + 128),
        ])
        abcols = 3584 + np.array([0 * 8 + 0 * 4 + h, 0 * 8 + 1 * 4 + h, 1 * 8 + 0 * 4 + h, 1 * 8 + 1 * 4 + h])
        cw = np.zeros((128, 3, 3), f32)
        for cc in range(3):
            for tap in range(3):
                cw[:, cc, tap] = conv_w[tap, cc * 512 + h * 128:cc * 512 + h * 128 + 128]
        gsc = np.tile(np.array([a_log[0, h], a_log[1, h], dt_bias[0, h], dt_bias[1, h]], f32)[None, :], (128, 1))
        m = {
            "xb": x[b], "ctxb": ctx[b], "xo": np.ascontiguousarray(x[b, j * 2048:(j + 1) * 2048]),
            "cT": cT.reshape(128, 16), "wmod": w_mod,
            "bm1T": colT(b_mod[0:2048], 16), "bm2": np.ascontiguousarray(b_mod[2048:].reshape(1, 4096)),
            "g1T": colT(norm1_g, 8), "g2T": colT(norm2_g, 8),
            "fgbc": np.ascontiguousarray(np.tile(final_g[None, :], (128, 1))),
            "wslab": np.ascontiguousarray(w_in[:, cols]), "wab": np.ascontiguousarray(w_in[:, abcols]),
            "cwT": cw.reshape(128, 9), "gsc": np.ascontiguousarray(gsc),
            "gng": np.ascontiguousarray(np.tile(gdn_norm_g[None, :], (128, 1))),
            "sgg": np.ascontiguousarray(np.tile(da_subln_g[None, :], (128, 1))),
            "lamv": np.ascontiguousarray(np.tile(lamcat[None, :], (128, 1))),
            "wout": np.ascontiguousarray(np.stack([w_out[h * 128:(h + 1) * 128], w_out[512 + h * 128:512 + (h + 1) * 128]], 0)),
            "rope": rope, "consts": consts,
            "wr": np.ascontiguousarray(w_router.reshape(8, 128, 16).transpose(1, 0, 2).reshape(128, 128)),
            "wg": w_gate if STAGE[0] > 4 else w_gate[0:1, 0:8, 0:8].copy(),
            "wu": w_up if STAGE[0] > 4 else w_up[0:1, 0:8, 0:8].copy(),
            "wd": w_down if STAGE[0] > 4 else w_down[0:1, 0:8, 0:8].copy(),
        }
        in_maps.append(m)
    if MAPS_ONLY[0]:
        return in_maps
    res = run_bass_kernel_spmd(nc, in_maps, core_ids=list(range(8)), **RUN_KW)
    out = np.zeros((2, L, D), f32)
    LAST["res"] = res
    for core in range(8):
        b, j = core // 4, core % 4
        out[b, j * 2048:(j + 1) * 2048] = res.results[core]["out"]
    return out
```

```python
import math
import numpy as np
import concourse.bass as bass
import concourse.mybir as mybir
from concourse.bass_utils import run_bass_kernel_spmd

F32 = mybir.dt.float32
BF16 = mybir.dt.bfloat16
AF = mybir.ActivationFunctionType
ALU = mybir.AluOpType
AX = mybir.AxisListType

ENGS = ("pe", "act", "dve", "pool", "sp")
EPOCH = 30000
EPS = 1e-6
L = 8192
LC = 256
D = 1024
NSC = 66
LAM_INIT = 0.8 - 0.6 * math.exp(-0.3 * 0)

STAGE = [99]
MAPS_ONLY = [False]
SEQ = [False]
RUN_KW = {}
DEBUG = []
LAST = {}


class Prog:
    def __init__(self, nc, sync_same_engine=True):
        self.nc = nc
        self.ops = {e: [] for e in ENGS}
        self.count = {e: 0 for e in ENGS}
        self.sems = {}
        self.chan_count = {}
        self.chan_inc = {}
        self.waited = {e: {} for e in ENGS}
        self.bufs = {}
        self.sync_same = sync_same_engine

    def sem(self, name):
        if name not in self.sems:
            ctx = self.nc.semaphore(name)
            self.sems[name] = ctx.__enter__()
        return self.sems[name]

    def _need(self, eng, tok, waits):
        if tok is None:
            return
        sname, val, teng = tok
        if teng == eng and (eng == "pe" or not self.sync_same):
            return
        if val <= self.waited[eng].get(sname, 0):
            return
        self.waited[eng][sname] = val
        waits.append((sname, val))

    def _deps(self, eng, reads, writes):
        waits = []
        for k in reads:
            b = self.bufs.get(k)
            if b is not None:
                self._need(eng, b["w"], waits)
                if k[0] == "B":
                    for t in b["r"]:
                        if t[2] != eng:
                            self._need(eng, t, waits)
        for k in writes:
            b = self.bufs.get(k)
            if b is not None:
                self._need(eng, b["w"], waits)
                for t in b["r"]:
                    self._need(eng, t, waits)
        return waits

    def _commit(self, tok, reads, writes):
        for k in reads:
            b = self.bufs.setdefault(k, {"w": None, "r": []})
            b["r"].append(tok)
            if len(b["r"]) > 64:
                b["r"] = b["r"][-48:]
        for k in writes:
            self.bufs[k] = {"w": tok, "r": []}

    def op(self, eng, fn, reads=(), writes=()):
        waits = self._deps(eng, reads, writes)
        idx = self.count[eng]
        self.count[eng] += 1
        ep, k = divmod(idx, EPOCH)
        sname = f"p_{eng}_{ep}"
        self.sem(sname)
        tok = (sname, k + 1, eng)
        self.ops[eng].append((waits, fn, (sname, 1)))
        self._commit(tok, reads, writes)
        return tok

    def dma(self, eng, chan, fn, reads=(), writes=(), inc=16):
        waits = self._deps(eng, reads, writes)
        n = self.chan_count.get(chan, 0)
        sname = f"d_{chan}"
        self.sem(sname)
        self.chan_inc[chan] = inc
        if n > 0:
            self._need(eng, (sname, inc * n, "dma"), waits)
        self.chan_count[chan] = n + 1
        tok = (sname, inc * (n + 1), "dma")
        self.ops[eng].append((waits, fn, (sname, inc)))
        self._commit(tok, reads, writes)
        return tok

    def barrier(self):
        toks = []
        for e in ENGS:
            n = self.count[e]
            if n:
                ep, k = divmod(n - 1, EPOCH)
                toks.append((f"p_{e}_{ep}", k + 1, e))
        for c, n in self.chan_count.items():
            if c.startswith("ccrs"):
                continue
            toks.append((f"d_{c}", self.chan_inc[c] * n, "dma"))
        for e in ENGS:
            waits = []
            for t in toks:
                if t[2] == e and t[2] != "dma":
                    pass
                sname, val, _ = t
                if val > self.waited[e].get(sname, 0):
                    self.waited[e][sname] = val
                    waits.append((sname, val))
            self.ops[e].append((waits, None, None))

    def emit(self):
        nc = self.nc
        sems = self.sems
        ops = self.ops
        with nc.Block() as block:
            def run(e, engobj):
                for waits, fn, inc in ops[e]:
                    for sname, val in waits:
                        engobj.wait_ge(sems[sname], val)
                    if fn is not None:
                        fn(engobj).then_inc(sems[inc[0]], inc[1])

            @block.tensor
            def _(eng):
                run("pe", eng)

            @block.scalar
            def _(eng):
                run("act", eng)

            @block.vector
            def _(eng):
                run("dve", eng)

            @block.gpsimd
            def _(eng):
                run("pool", eng)

            @block.sync
            def _(eng):
                run("sp", eng)


def _isz(dt):
    return 2 if dt == BF16 else 4


class Arena:
    def __init__(self, nc, limit=229376 - 1024):
        self.nc = nc
        self.off = 16384 + 1024
        self.n = 0
        self.limit = limit

    def alloc(self, name, shape, dt):
        sz = _isz(dt)
        for s in shape[1:]:
            sz *= s
        sz = (sz + 63) // 64 * 64
        t = self.nc.alloc_sbuf_tensor_at(f"{name}_{self.n}", list(shape), dt, offset=self.off)
        self.off += sz
        self.n += 1
        assert self.off <= self.limit, (name, self.off)
        return t


NCONST = 13


def make_consts():
    idx = np.arange(128)
    blk = (idx[:, None] // 64) == (idx[None, :] // 64)
    k = idx[:, None]
    m = idx[None, :]
    c = np.zeros((NCONST, 128, 128), np.float32)
    c[0] = np.eye(128)
    c[1] = 1.0
    c[2] = blk & (k <= m)
    c[3] = blk & (k >= m)
    c[4] = blk
    c[5] = (k < 64) & (m >= 0)
    c[6] = (k >= 64) & (m >= 0)
    c[7] = blk & (m > k)
    c[8] = blk & (m >= k)
    c[9] = blk & (m < k)
    c[10] = blk & (m <= k)
    pm = np.zeros((128, 128), np.float32)
    for mm_ in range(128):
        i = mm_ % 64
        r = i % 32
        partner = mm_ + 16 if r < 16 else mm_ - 16
        pm[partner, mm_] = 1.0
    c[11] = pm
    g = np.zeros((128, 128), np.float32)
    g[:64, :64] = (idx[:64, None] % 16) == (idx[None, :64] % 16)
    c[12] = g
    return np.ascontiguousarray(c.transpose(1, 0, 2).reshape(128, NCONST * 128))


def make_rope():
    f32 = np.float32
    t = np.arange(L)
    rows = (t // 64).astype(f32)
    cols = (t % 64).astype(f32)
    inv_freq = np.power(f32(10000.0), -np.arange(0, 32, 2, dtype=f32) / f32(32)).astype(f32)
    ang_row = (rows[:, None] * inv_freq[None, :]).astype(f32)
    ang_col = (cols[:, None] * inv_freq[None, :]).astype(f32)
    cosT = np.zeros((128, L), f32)
    sinT = np.zeros((128, L), f32)
    for p in range(128):
        i = p % 64
        ang = ang_row if i < 32 else ang_col
        r = i % 32
        f = r % 16
        sign = -1.0 if r < 16 else 1.0
        cosT[p] = np.cos(ang[:, f]).astype(f32)
        sinT[p] = (sign * np.sin(ang[:, f])).astype(f32)
    return np.stack([cosT, sinT], 0)


def build(debug=(), stage=99):
    nc = bass.Bass("TRN2", target_bir_lowering=False)
    P = Prog(nc)

    def din(name, shape, dt=F32):
        return nc.dram_tensor(name, list(shape), dt, kind="ExternalInput").ap()

    xb = din("xb", [L, D])
    ctxb = din("ctxb", [LC, D])
    xo = din("xo", [2048, D])
    cT_d = din("cT", [128, 16])
    wmod = din("wmod", [D, 6 * D])
    bm1T_d = din("bm1T", [128, 16])
    bm2_d = din("bm2", [1, 4096])
    g1T_d = din("g1T", [128, 8])
    g2T_d = din("g2T", [128, 8])
    fgbc_d = din("fgbc", [128, D])
    wslab_d = din("wslab", [D, 896])
    wab_d = din("wab", [D, 4])
    cwT_d = din("cwT", [128, 9])
    gsc_d = din("gsc", [128, 4])
    gng_d = din("gng", [128, 128])
    sgg_d = din("sgg", [128, 128])
    lamv_d = din("lamv", [128, 256])
    wout_d = din("wout", [2, 128, D])
    rope_d = din("rope", [2, 128, L])
    consts_d = din("consts", [128, NCONST * 128])
    wr_d = din("wr", [128, 128])
    wshape = [16, D, D] if stage > 4 else [1, 8, 8]
    wg_d = din("wg", wshape)
    wu_d = din("wu", wshape)
    wd_d = din("wd", wshape)
    out_d = nc.dram_tensor("out", [2048, D], F32, kind="ExternalOutput").ap()

    kt_s = nc.dram_tensor("kt_s", [128, LC + L], BF16).ap()
    qt_s = nc.dram_tensor("qt_s", [128, L], BF16).ap()
    v_s = nc.dram_tensor("v_s", [NSC, 128, 130], BF16).ap()
    rs_in = [nc.dram_tensor(f"rs_in{i}", [2048, D], F32) for i in range(4)]
    rs_out = [nc.dram_tensor(f"rs_out{i}", [512, D], F32) for i in range(4)]
    ag_in = nc.dram_tensor("ag_in", [16, 2048], F32)
    ag_out = nc.dram_tensor("ag_out", [64, 2048], F32)
    xnew_s = nc.dram_tensor("xnew_s", [2048, D], F32).ap()

    dbg_outs = {}

    def finish():
        waits = []
        for k in ["out0", "out1"] + ["dbg_" + n for n in dbg_outs]:
            b = P.bufs.get(k)
            if b is not None:
                P._need("pool", b["w"], waits)
        P.ops["pool"].append((waits, None, None))
        P.emit()
        return nc, dbg_outs

    def dbg(name, src_ap, shape, key):
        if name in debug:
            o = nc.dram_tensor("dbg_" + name, list(shape), src_ap.tensor.dtype, kind="ExternalOutput").ap()
            dbg_outs[name] = o
            P.dma("pool", "dbg_" + name, lambda e: e.dma_start(out=o, in_=src_ap), reads=[key], writes=["dbg_" + name])

    psum = nc.alloc_psum_tensor("ps", [128, 8, 512], F32)

    def bank(i):
        return psum[:, i, :]

    def mm(out, lhsT, rhs, r, w, start=True, stop=True):
        P.op("pe", lambda e: e.matmul(out, lhsT=lhsT, rhs=rhs, start=start, stop=stop), reads=r, writes=w)

    def tr(out, in_, idt, r, w):
        P.op("pe", lambda e: e.transpose(out, in_, idt), reads=r, writes=w)

    def act(out, in_, func, r, w, bias=None, scale=None, accum=None):
        kw = {}
        if bias is not None:
            kw["bias"] = bias
        if scale is not None:
            kw["scale"] = scale
        if accum is not None:
            kw["accum_out"] = accum
        P.op("act", lambda e: e.activation(out=out, in_=in_, func=func, **kw), reads=r, writes=w)

    def ts(eng, out, in0, s1, s2, op0, op1, r, w, accum=None):
        if op1 is None:
            P.op(eng, lambda e: e.tensor_scalar(out=out, in0=in0, scalar1=s1, scalar2=None, op0=op0), reads=r, writes=w)
        elif accum is None:
            P.op(eng, lambda e: e.tensor_scalar(out=out, in0=in0, scalar1=s1, scalar2=s2, op0=op0, op1=op1), reads=r, writes=w)
        else:
            P.op(eng, lambda e: e.tensor_scalar(out=out, in0=in0, scalar1=s1, scalar2=s2, op0=op0, op1=op1, accum_out=accum),
                 reads=r, writes=w)

    def tt(eng, out, in0, in1, op, r, w):
        P.op(eng, lambda e: e.tensor_tensor(out=out, in0=in0, in1=in1, op=op), reads=r, writes=w)

    def stt(out, in0, scalar, in1, op0, op1, r, w):
        P.op("dve", lambda e: e.scalar_tensor_tensor(out=out, in0=in0, scalar=scalar, in1=in1, op0=op0, op1=op1), reads=r, writes=w)

    def cp(eng, out, in_, r, w):
        if eng == "act":
            P.op("act", lambda e: e.copy(out=out, in_=in_), reads=r, writes=w)
        else:
            P.op(eng, lambda e: e.tensor_copy(out=out, in_=in_), reads=r, writes=w)

    def recip(out, in_, r, w):
        P.op("dve", lambda e: e.reciprocal(out=out, in_=in_), reads=r, writes=w)

    def mset(eng, ap, val, w):
        P.op(eng, lambda e: e.memset(ap, val), reads=(), writes=w)

    def dma(eng, chan, out, in_, r, w):
        P.dma(eng, chan, lambda e: e.dma_start(out=out, in_=in_), reads=r, writes=w)

    A = Arena(nc)
    cst = A.alloc("cst", [128, NCONST, 128], F32)
    dma("sp", "cst", cst[:, :, :].rearrange("p a b -> p (a b)"), consts_d, (), ["cst"])

    def C(i):
        return cst[:, i, :]
    ident, ones, triF, triB, blkm, H0, H1 = C(0), C(1), C(2), C(3), C(4), C(5), C(6)
    mS = [C(7), C(9)]
    mI = [C(8), C(10)]
    tri = [triF, triB]
    identb = A.alloc("identb", [128, 128], BF16)
    pmb = A.alloc("pmb", [128, 128], BF16)
    cp("dve", identb[:, :], ident, ["cst"], ["identb"])
    cp("dve", pmb[:, :], C(11), ["cst"], ["pmb"])

    small = A.alloc("small", [128, 512], F32)
    _sc = [0]

    def col(n=1):
        c0 = _sc[0]
        _sc[0] += n
        assert _sc[0] <= 512
        return small[:, c0:c0 + n]

    cT = col(16)
    bm1T = col(16)
    g1T = col(8)
    g2T = col(8)
    cwT = col(9)
    gsc = col(4)
    dma("sp", "sm0", cT, cT_d, (), ["cT"])
    dma("sp", "sm1", bm1T, bm1T_d, (), ["bm1T"])
    dma("sp", "sm2", g1T, g1T_d, (), ["g1T"])
    dma("sp", "sm3", g2T, g2T_d, (), ["g2T"])
    dma("sp", "sm4", cwT, cwT_d, (), ["cwT"])
    dma("sp", "sm5", gsc, gsc_d, (), ["gsc"])
    gng = A.alloc("gng", [128, 128], F32)
    sgg = A.alloc("sgg", [128, 128], F32)
    dma("sp", "sm6", gng[:, :], gng_d, (), ["gng"])
    dma("sp", "sm7", sgg[:, :], sgg_d, (), ["sgg"])
    lamv = A.alloc("lamv", [128, 256], F32)
    dma("sp", "sm8", lamv[:, :], lamv_d, (), ["lamv"])

    lamt = col(8)
    lsc = A.alloc("lsc", [128, 64], F32)
    P.op("dve", lambda e: e.tensor_tensor(out=lsc[:, :], in0=lamv[:, 0:64], in1=lamv[:, 64:128], op=ALU.mult), reads=["lamv"], writes=["lsc"])
    P.op("dve", lambda e: e.tensor_reduce(out=lamt[:, 0:1], in_=lsc[:, :], axis=AX.X, op=ALU.add), reads=["lsc"], writes=["lam0"])
    P.op("dve", lambda e: e.tensor_tensor(out=lsc[:, :], in0=lamv[:, 128:192], in1=lamv[:, 192:256], op=ALU.mult), reads=["lamv", "lam0"], writes=["lsc"])
    P.op("dve", lambda e: e.tensor_reduce(out=lamt[:, 1:2], in_=lsc[:, :], axis=AX.X, op=ALU.add), reads=["lsc"], writes=["lam1"])
    act(lamt[:, 2:4], lamt[:, 0:2], AF.Exp, ["lam0", "lam1"], ["lam2"])
    tt("dve", lamt[:, 4:5], lamt[:, 2:3], lamt[:, 3:4], ALU.subtract, ["lam2"], ["lam3"])
    ts("dve", lamt[:, 5:6], lamt[:, 4:5], -1.0, -LAM_INIT, ALU.mult, ALU.add, ["lam3"], ["nlam"])
    nlam = lamt[:, 5:6]

    scb = A.alloc("scb", [128, 8, 2], BF16)
    persist_mark = A.off

    KnT = A.alloc("KnT", [128, NSC * 128], BF16)
    QnT = A.alloc("QnT", [128, NSC * 128], BF16)
    Vg = A.alloc("Vg", [128, NSC, 128], BF16)
    gate_s = A.alloc("gate_s", [128, 64, 128], BF16)
    abt = A.alloc("abt", [128, NSC, 4], F32)
    gdn_mark = A.off

    wslab = A.alloc("wslab", [128, 8, 896], BF16)
    wab = A.alloc("wab", [128, 8, 4], BF16)
    for kc in range(8):
        dma("pool", f"wsl{kc % 2}", wslab[:, kc, :], wslab_d[kc * 128:(kc + 1) * 128, :], (), ["wslab"])
    dma("pool", "wab", wab[:, :, :], wab_d.rearrange("(kc p) n -> p kc n", p=128), (), ["wab"])

    m1 = A.alloc("m1", [128, 16, 2], F32)
    a1 = col(8)
    a1c = col(8)
    ipw_mark = A.off
    wm1 = A.alloc("wm1", [128, 8, 2048], BF16)
    for kc in range(8):
        dma("pool", f"wm{kc % 2}", wm1[:, kc, :], wmod[kc * 128:(kc + 1) * 128, 0:2048], (), [f"wm1_{kc}"])
    act(scb[:, :, :].rearrange("p a b -> p (a b)"), cT, AF.Silu, ["cT"], ["scb"])
    pmod = bank(0)[:, 0:32]
    for c_ in range(16):
        for kc in range(8):
            mm(pmod[:, c_ * 2:(c_ + 1) * 2], wm1[:, kc, c_ * 128:(c_ + 1) * 128], scb[:, kc, :], [f"wm1_{kc}", "scb"], ["B0"],
               start=(kc == 0), stop=(kc == 7))
    pmod3 = pmod.rearrange("p (c v) -> p c v", v=2)
    for v in range(2):
        tt("dve", m1[:, :, v], pmod3[:, :, v], bm1T, ALU.add, ["B0", "bm1T"], ["m1"])
    stt(a1, m1[:, 8:16, 0], 1.0, g1T, ALU.add, ALU.mult, ["m1", "g1T"], ["a1"])
    stt(a1c, m1[:, 8:16, 1], 1.0, g1T, ALU.add, ALU.mult, ["m1", "g1T"], ["a1c"])
    if stage == 0:
        return finish()
    P.barrier()
    A.off = ipw_mark

    NT = 33
    xt = [A.alloc(f"xt{i}", [128, 2, D], F32) for i in range(3)]
    hT = [A.alloc(f"hT{i}", [128, 8, 256], BF16) for i in range(2)]
    pre = [A.alloc(f"pre{i}", [128, 3, 258], F32) for i in range(4)]
    rtab = [A.alloc(f"rtab{i}", [128, 2, 256], F32) for i in range(3)]
    sqj = A.alloc("sqj", [128, D], BF16)
    nrm = A.alloc("nrm", [128, 8], F32)
    qb = [A.alloc(f"qb{i}", [128, 256], BF16) for i in range(2)]
    rt1 = [A.alloc(f"rt1_{i}", [128, 256], F32) for i in range(2)]
    rt2 = [A.alloc(f"rt2_{i}", [128, 256], F32) for i in range(2)]
    qkst = [A.alloc(f"qkst{i}", [128, 256], BF16) for i in range(4)]
    vst = [A.alloc(f"vst{i}", [128, 2, 130], BF16) for i in range(2)]
    cacc = A.alloc("cacc", [128, 3, 256], F32)
    csil = A.alloc("csil", [128, 3, 256], F32)
    csq = A.alloc("csq", [128, 2, 256], F32)
    rinv = A.alloc("rinv", [128, 2, 256], F32)
    for i in range(2):
        mset("pool", vst[i][:, :, 128:130], 1.0, [f"vst{i}"])

    def tile_info(t):
        if t == 0:
            return ctxb, 0, True
        return xb[(t - 1) * 256:t * 256, :], LC + (t - 1) * 256, False

    fm_cnt = [0]

    def loadx(t):
        src, koff, is_ctx = tile_info(t)
        dma("sp", f"x{t % 3}", xt[t % 3][:, :, :], src.rearrange("(s p) d -> p s d", p=128), (), [f"xt{t % 3}"])
        if not is_ctx:
            dma("sp", f"rt{t % 3}", rtab[t % 3][:, :, :], rope_d[:, :, (t - 1) * 256:t * 256].rearrange("a p n -> p a n"), (), [f"rtab{t % 3}"])

    def front(t):
        src, koff, is_ctx = tile_info(t)
        xs = xt[t % 3]
        hs = hT[t % 2]
        kx, kh = f"xt{t % 3}", f"hT{t % 2}"
        sh = m1[:, 0:8, 1] if is_ctx else m1[:, 0:8, 0]
        aa = a1c if is_ctx else a1
        for s in range(2):
            act(sqj[:, :], xs[:, s, :], AF.Square, [kx], ["sqj", f"nrm{s}"], accum=nrm[:, s:s + 1])
            yield
        act(nrm[:, 2:4], nrm[:, 0:2], AF.Sqrt, ["nrm0", "nrm1"], ["nrmB"], bias=EPS, scale=1.0 / D)
        yield
        recip(nrm[:, 4:6], nrm[:, 2:4], ["nrmB"], ["nrmC"])
        yield
        for s in range(2):
            ts("pool", xs[:, s, :], xs[:, s, :], nrm[:, 4 + s:5 + s], 1.0, ALU.mult, ALU.mult, [kx, "nrmC"], [kx])
            yield
        for kc in range(8):
            pb = kc % 2
            pT = bank(pb)[:, 0:256]
            for s in range(2):
                tr(pT[:, s * 128:(s + 1) * 128], xs[:, s, kc * 128:(kc + 1) * 128], ident, [kx, "cst"], [f"B{pb}"])
                yield
            if kc % 2 == 0:
                act(hs[:, kc, :], pT, AF.Identity, [f"B{pb}", "a1", "a1c", "m1"], [f"{kh}_{kc}"], bias=sh[:, kc:kc + 1], scale=aa[:, kc:kc + 1])
                yield
            else:
                ts("dve", hs[:, kc, :], pT, aa[:, kc:kc + 1], sh[:, kc:kc + 1], ALU.mult, ALU.add, [f"B{pb}", "a1", "a1c", "m1"], [f"{kh}_{kc}"])
                yield

    def back(t):
        src, koff, is_ctx = tile_info(t)
        hs = hT[t % 2]
        kh = f"hT{t % 2}"
        pr = pre[t % 4]
        kp = f"pre{t % 4}"
        for blk_ in range(5):
            if is_ctx and blk_ == 0:
                continue
            fb = 2 + (fm_cnt[0] % 2)
            fm_cnt[0] += 1
            pf = bank(fb)[:, 0:256]
            for kc in range(8):
                mm(pf, wslab[:, kc, blk_ * 128:(blk_ + 1) * 128], hs[:, kc, :], ["wslab", f"{kh}_{kc}"], [f"B{fb}"], start=(kc == 0), stop=(kc == 7))
                yield
            if blk_ < 2:
                st = qkst[(t % 2) * 2 + blk_]
                kst = f"qkst{(t % 2) * 2 + blk_}"
                dst = (qt_s[:, (t - 1) * 256:t * 256] if blk_ == 0 else kt_s[:, koff:koff + 256]) if not is_ctx else kt_s[:, 0:256]
                if is_ctx:
                    cp("act", st[:, :], pf, [f"B{fb}"], [kst])
                    yield
                else:
                    q_b = qb[blk_]
                    rs_ = rtab[t % 3]
                    cp("act", q_b[:, :], pf, [f"B{fb}"], [f"qb{blk_}"])
                    yield
                    pp = bank(6)[:, blk_ * 256:(blk_ + 1) * 256]
                    mm(pp, pmb[:, :], q_b[:, :], ["pmb", f"qb{blk_}"], ["B6"])
                    yield
                    tt("dve", rt1[blk_][:, :], pf, rs_[:, 0, :], ALU.mult, [f"B{fb}", f"rtab{t % 3}"], [f"rt1_{blk_}"])
                    yield
                    tt("dve", rt2[blk_][:, :], pp, rs_[:, 1, :], ALU.mult, ["B6", f"rtab{t % 3}"], [f"rt2_{blk_}"])
                    yield
                    tt("pool", st[:, :], rt1[blk_][:, :], rt2[blk_][:, :], ALU.add, [f"rt1_{blk_}", f"rt2_{blk_}"], [kst])
                    yield
                if stage != 0.325:
                    dma("pool", f"qk{(t % 2) * 2 + blk_}", dst, st[:, :], [kst], ["ktqt_s"])
                    yield
            else:
                c_ = blk_ - 2
                if c_ % 2 == 0:
                    cp("act", pr[:, c_, 1:257], pf, [f"B{fb}"], [kp])
                    yield
                else:
                    cp("dve", pr[:, c_, 1:257], pf, [f"B{fb}"], [kp])
                    yield
        vs_ = vst[t % 2]
        for s in range(2):
            sc = (koff // 128) + s
            pv = bank(4 + s)
            kb = f"B{4 + s}"
            for kc in range(8):
                mm(pv[:, 0:128], hs[:, kc, s * 128:(s + 1) * 128], wslab[:, kc, 640:768], [f"{kh}_{kc}", "wslab"], [kb], start=(kc == 0), stop=(kc == 7))
                yield
            if not is_ctx:
                for kc in range(8):
                    mm(pv[:, 128:256], hs[:, kc, s * 128:(s + 1) * 128], wslab[:, kc, 768:896], [f"{kh}_{kc}", "wslab"], [kb], start=(kc == 0), stop=(kc == 7))
                    yield
            if stage != 0.331:
                for kc in range(8):
                    mm(pv[:, 256:260], hs[:, kc, s * 128:(s + 1) * 128], wab[:, kc, :], [f"{kh}_{kc}", "wab"], [kb], start=(kc == 0), stop=(kc == 7))
                    yield
            if stage != 0.333:
                cp("act", vs_[:, s, 0:128], pv[:, 0:128], [kb], [f"vst{t % 2}"])
                yield
            if not is_ctx:
                act(gate_s[:, sc - 2, :], pv[:, 128:256], AF.Silu, [kb], ["gate_s"])
                yield
            if stage not in (0.331, 0.332):
                cp("dve", abt[:, sc, :], pv[:, 256:260], [kb], ["abt"])
                yield
        dma("pool", f"v{t % 2}", v_s[(koff // 128):(koff // 128) + 2, :, :].rearrange("s p n -> p s n"), vs_[:, :, :], [f"vst{t % 2}"], ["v_s"])
        yield

    def conv_stage(t, left, right):
        src, koff, is_ctx = tile_info(t)
        pr = pre[t % 4]
        kp = f"pre{t % 4}"
        if left is None:
            mset("pool", pr[:, :, 0:1], 0.0, [kp])
            yield
        else:
            cp("pool", pr[:, :, 0:1], pre[left % 4][:, :, 256:257], [f"pre{left % 4}", kp], [kp])
            yield
        if right is None:
            mset("pool", pr[:, :, 257:258], 0.0, [kp])
            yield
        else:
            cp("pool", pr[:, :, 257:258], pre[right % 4][:, :, 1:2], [f"pre{right % 4}", kp], [kp])
            yield
        for c_ in range(3):
            ts("dve", cacc[:, c_, :], pr[:, c_, 0:256], cwT[:, c_ * 3:c_ * 3 + 1], None, ALU.mult, None, [kp, "cwT"], ["cacc"])
            yield
            stt(cacc[:, c_, :], pr[:, c_, 1:257], cwT[:, c_ * 3 + 1:c_ * 3 + 2], cacc[:, c_, :], ALU.mult, ALU.add, [kp, "cacc"], ["cacc"])
            yield
            stt(cacc[:, c_, :], pr[:, c_, 2:258], cwT[:, c_ * 3 + 2:c_ * 3 + 3], cacc[:, c_, :], ALU.mult, ALU.add, [kp, "cacc"], ["cacc"])
            yield
        act(csil[:, :, :], cacc[:, :, :], AF.Silu, ["cacc"], ["csil"])
        yield
        tt("pool", csq[:, :, :], csil[:, 0:2, :], csil[:, 0:2, :], ALU.mult, ["csil"], ["csq"])
        yield
        pss = bank(7)
        mm(pss, ones, csq[:, :, :].rearrange("p a b -> p (a b)"), ["cst", "csq"], ["B7"])
        yield
        act(rinv[:, :, :].rearrange("p a b -> p (a b)"), pss, AF.Sqrt, ["B7"], ["rinvA"], bias=EPS, scale=1.0)
        yield
        recip(rinv[:, :, :].rearrange("p a b -> p (a b)"), rinv[:, :, :].rearrange("p a b -> p (a b)"), ["rinvA"], ["rinv"])
        yield
        stt(QnT[:, koff:koff + 256], csil[:, 0, :], 128.0 ** -0.5, rinv[:, 0, :], ALU.mult, ALU.mult, ["csil", "rinv"], ["QnT"])
        yield
        tt("dve", KnT[:, koff:koff + 256], csil[:, 1, :], rinv[:, 1, :], ALU.mult, ["csil", "rinv"], ["KnT"])
        yield
        pvt = bank(7)
        for s in range(2):
            tr(pvt[:, 256 + s * 128:256 + (s + 1) * 128], csil[:, 2, s * 128:(s + 1) * 128], ident, ["csil", "cst"], ["B7"])
            yield
        cp("act", Vg[:, koff // 128:koff // 128 + 2, :], pvt[:, 256:512].rearrange("p (s n) -> p s n", s=2), ["B7"], ["Vg"])
        yield

    def interleave0(*gens):
        gens = list(gens)
        if SEQ[0] == 1:
            for g in gens:
                for _ in g:
                    pass
            return
        while gens:
            for g in list(gens):
                try:
                    next(g)
                except StopIteration:
                    gens.remove(g)

    def conv_for(k):
        left = None if k in (0, 1) else k - 1
        right = None if k in (0, NT - 1) else k + 1
        return conv_stage(k, left, right)

    loadx(0)
    loadx(1)
    interleave0(front(0))
    for t in range(NT):
        if t + 2 < NT:
            loadx(t + 2)
        gl = [back(t)]
        if t + 1 < NT:
            gl.append(front(t + 1))
        if t - 2 >= 0:
            if SEQ[0] == 2:
                interleave0(*gl)
                gl = []
            gl.append(conv_for(t - 2))
        interleave0(*gl)
    interleave0(conv_for(NT - 2))
    interleave0(conv_for(NT - 1))

    dbg("KnT", KnT[:, :], [128, NSC * 128], "KnT")
    dbg("QnT", QnT[:, :], [128, NSC * 128], "QnT")
    dbg("Vg", Vg[:, :, :], [128, NSC, 128], "Vg")
    dbg("abt", abt[:, :, :], [128, NSC, 4], "abt")

    if stage == 1:
        return finish()
    P.barrier()
    A.off = gdn_mark

    og = A.alloc("og", [128, 64, 128], F32)
    mset("pool", og[:, :, :], 0.0, [f"og{i}" for i in range(64)])
    gg = A.alloc("gg", [128, 2, NSC], F32)
    bet = A.alloc("bet", [128, 2, NSC], F32)
    nbet = A.alloc("nbet", [128, 2, NSC], F32)
    Gc = A.alloc("Gc", [128, 2, NSC], F32)
    eG = A.alloc("eG", [128, 2, NSC], F32)
    eGl = A.alloc("eGl", [128, 2, NSC], F32)
    egl = A.alloc("egl", [128, 2, 2, NSC], F32)
    gtmp = A.alloc("gtmp", [128, 2, NSC], F32)
    nea = col(2)
    act(nea, gsc[:, 0:2], AF.Exp, ["gsc"], ["neaA"])
    ts("dve", nea, nea, -1.0, None, ALU.mult, None, ["neaA"], ["nea"])
    for d in range(2):
        act(gtmp[:, d, :], abt[:, :, d], AF.Exp, ["abt", "gsc"], ["gtmpA"], bias=gsc[:, 2 + d:3 + d], scale=1.0)
        act(gtmp[:, d, :], gtmp[:, d, :], AF.Ln, ["gtmpA"], ["gtmpB"], bias=1.0, scale=1.0)
        ts("dve", gg[:, d, :], gtmp[:, d, :], nea[:, d:d + 1], None, ALU.mult, None, ["gtmpB", "nea"], ["gg"])
        act(bet[:, d, :], abt[:, :, 2 + d], AF.Sigmoid, ["abt"], ["bet"])
        ts("dve", nbet[:, d, :], bet[:, d, :], -1.0, None, ALU.mult, None, ["bet"], ["nbet"])
        pg_ = bank(0)
        mm(pg_[:, 0:NSC], tri[d], gg[:, d, :], ["cst", "gg"], ["B0"])
        mm(pg_[:, 128:128 + NSC], blkm, gg[:, d, :], ["cst", "gg"], ["B0"])
        mm(pg_[:, 256:256 + NSC], H0, gg[:, d, :], ["cst", "gg"], ["B0"])
        mm(pg_[:, 384:384 + NSC], H1, gg[:, d, :], ["cst", "gg"], ["B0"])
        cp("dve", Gc[:, d, :], pg_[:, 0:NSC], ["B0"], ["Gc"])
        act(eG[:, d, :], pg_[:, 0:NSC], AF.Exp, ["B0"], ["eG"])
        tt("dve", gtmp[:, d, :], pg_[:, 128:128 + NSC], Gc[:, d, :], ALU.subtract, ["B0", "Gc", "gtmpB"], ["gtmpC"])
        act(eGl[:, d, :], gtmp[:, d, :], AF.Exp, ["gtmpC"], ["eGl"])
        act(egl[:, d, 0, :], pg_[:, 256:256 + NSC], AF.Exp, ["B0"], ["egl"])
        act(egl[:, d, 1, :], pg_[:, 384:384 + NSC], AF.Exp, ["B0"], ["egl"])

    dbg("gg", gg[:, :, :], [128, 2, NSC], "gg")
    dbg("Gc", Gc[:, :, :], [128, 2, NSC], "Gc")

    def pcbuf(name, dt, n=128):
        return [A.alloc(f"{name}{d}", [128, n], dt) for d in range(2)]
    gram = pcbuf("gram", F32, 256)
    Rw = pcbuf("Rw", BF16)
    kdp = [[A.alloc(f"kd{d}_{q}", [128, 128], BF16) for d in range(2)] for q in range(2)]
    qd = pcbuf("qd", BF16)
    qdTp = [[A.alloc(f"qdT{d}_{q}", [128, 128], BF16) for d in range(2)] for q in range(2)]
    dg = pcbuf("dg", F32)
    tE = pcbuf("tE", F32)
    Es = pcbuf("Es", F32)
    Ei = pcbuf("Ei", F32)
    aqkTp = [[A.alloc(f"aqkT{d}_{q}", [128, 128], BF16) for d in range(2)] for q in range(2)]
    Xb = [pcbuf("Xa", BF16), pcbuf("Xb", BF16)]
    Yb = [pcbuf("Ya", BF16), pcbuf("Yb", BF16)]
    Rb = [pcbuf("Ra", BF16), pcbuf("Rb", BF16)]
    ATb = pcbuf("AT", BF16)
    WTp = [[A.alloc(f"WT{d}_{q}", [128, 128], BF16) for d in range(2)] for q in range(2)]
    UBbp = [[A.alloc(f"UBb{d}_{q}", [128, 128], F32) for d in range(2)] for q in range(2)]
    S32 = pcbuf("S32", F32)
    Sbf = pcbuf("Sbf", BF16)
    ub = pcbuf("ub", BF16)
    for d in range(2):
        mset("pool", S32[d][:, :], 0.0, [f"S32{d}"])
        mset("pool", Sbf[d][:, :], 0.0, [f"Sbf{d}"])

    slot = [0]

    def pslot():
        s = slot[0] % 4
        slot[0] += 1
        return bank(s)[:, 0:128], f"B{s}"

    def pslot_bf():
        ap_, k_ = pslot()
        return ap_[:, 0:64].bitcast(BF16), k_

    def precompute(sc, d, par):
        WT, UBb, kd, qdT, aqkT = WTp[par], UBbp[par], kdp[par], qdTp[par], aqkTp[par]
        pq = str(par)
        lat = sc >= 2
        c0 = sc * 128
        sd = str(d)
        p1, k1 = pslot()
        mm(p1, KnT[:, c0:c0 + 128], KnT[:, c0:c0 + 128], ["KnT"], [k1])
        yield
        cp("act", gram[d][:, 0:128], p1, [k1], ["gramA" + sd])
        yield
        if lat:
            p2, k2 = pslot()
            mm(p2, KnT[:, c0:c0 + 128], QnT[:, c0:c0 + 128], ["KnT", "QnT"], [k2])
            yield
            cp("act", gram[d][:, 128:256], p2, [k2], ["gramB" + sd])
            yield
        p3, k3 = pslot_bf()
        tr(p3, KnT[:, c0:c0 + 128], identb[:, :], ["KnT", "identb"], [k3])
        yield
        act(Rw[d][:, :], p3, AF.Identity, [k3, "eG"], ["Rw" + sd], scale=eG[:, d, sc:sc + 1])
        yield
        ts("dve", kd[d][:, :], p3, eGl[:, d, sc:sc + 1], None, ALU.mult, None, [k3, "eGl"], ["kd" + sd + pq])
        yield
        if lat:
            p4, k4 = pslot_bf()
            tr(p4, QnT[:, c0:c0 + 128], identb[:, :], ["QnT", "identb"], [k4])
            yield
            act(qd[d][:, :], p4, AF.Identity, [k4, "eG"], ["qd" + sd], scale=eG[:, d, sc:sc + 1])
            yield
            p5, k5 = pslot_bf()
            tr(p5, qd[d][:, :], identb[:, :], ["qd" + sd, "identb"], [k5])
            yield
            cp("act", qdT[d][:, :], p5, [k5], ["qdT" + sd + pq])
            yield
        ts("pool", dg[d][:, :], ident, Gc[:, d, sc:sc + 1], 1.0, ALU.mult, ALU.mult, ["cst", "Gc"], ["dg" + sd])
        yield
        p6, k6 = pslot()
        mm(p6, ones, dg[d][:, :], ["cst", "dg" + sd], [k6])
        yield
        ts("dve", tE[d][:, :], p6, Gc[:, d, sc:sc + 1], 0.0, ALU.subtract, ALU.min, [k6, "Gc"], ["tEA" + sd])
        yield
        act(tE[d][:, :], tE[d][:, :], AF.Exp, ["tEA" + sd], ["tE" + sd])
        yield
        tt("pool", Es[d][:, :], tE[d][:, :], mS[d], ALU.mult, ["tE" + sd, "cst"], ["Es" + sd])
        yield
        X0 = Xb[0][d]
        stt(X0[:, :], gram[d][:, 0:128], nbet[:, d, sc:sc + 1], Es[d][:, :], ALU.mult, ALU.mult, ["gramA" + sd, "nbet", "Es" + sd], ["X0" + sd])
        yield
        if lat:
            tt("pool", Ei[d][:, :], tE[d][:, :], mI[d], ALU.mult, ["tE" + sd, "cst"], ["Ei" + sd])
            yield
            tt("dve", aqkT[d][:, :], gram[d][:, 128:256], Ei[d][:, :], ALU.mult, ["gramB" + sd, "Ei" + sd], ["aqkT" + sd + pq])
            yield
        p7, k7 = pslot_bf()
        tr(p7, X0[:, :], identb[:, :], ["X0" + sd, "identb"], [k7])
        yield
        Y0 = Yb[0][d]
        cp("act", Y0[:, :], p7, [k7], ["Y0" + sd])
        yield
        R0 = Rb[0][d]
        tt("pool", R0[:, :], X0[:, :], ident, ALU.add, ["X0" + sd, "cst"], ["R0" + sd])
        yield
        for lv in range(1, 6):
            a_, b_ = (lv - 1) % 2, lv % 2
            Xp, Yp, Rp = Xb[a_][d], Yb[a_][d], Rb[a_][d]
            Xn, Yn, Rn = Xb[b_][d], Yb[b_][d], Rb[b_][d]
            kXp, kYp, kRp = f"X{a_}{sd}", f"Y{a_}{sd}", f"R{a_}{sd}"
            kXn, kYn, kRn = f"X{b_}{sd}", f"Y{b_}{sd}", f"R{b_}{sd}"
            py, ky = pslot()
            mm(py, Xp[:, :], Yp[:, :], [kXp, kYp], [ky])
            yield
            if lv <= 4:
                px, kx_ = pslot()
                mm(px, Yp[:, :], Xp[:, :], [kXp, kYp], [kx_])
                yield
            cp("act", Yn[:, :], py, [ky], [kYn])
            yield
            if lv <= 4:
                cp("dve", Xn[:, :], px, [kx_], [kXn])
                yield
            pr_, kr_ = pslot()
            mm(pr_, Yn[:, :], Rp[:, :], [kYn, kRp], [kr_])
            yield
            if lv < 5:
                tt("dve", Rn[:, :], pr_, Rp[:, :], ALU.add, [kr_, kRp], [kRn])
                yield
            else:
                tt("dve", ATb[d][:, :], pr_, Rp[:, :], ALU.add, [kr_, kRp], ["AT" + sd])
                yield
        p8, k8 = pslot()
        mm(p8, Rw[d][:, :], ATb[d][:, :], ["Rw" + sd, "AT" + sd], [k8])
        yield
        cp("act", WT[d][:, :], p8, [k8], ["WT" + sd + pq])
        yield
        p9, k9 = pslot()
        mm(p9, ATb[d][:, :], Vg[:, sc, :], ["AT" + sd, "Vg"], [k9])
        yield
        act(UBb[d][:, :], p9, AF.Identity, [k9, "bet"], ["UBb" + sd + pq], scale=bet[:, d, sc:sc + 1])
        yield

    def step(sc, hh, d, par):
        WT, UBb, kd, qdT, aqkT = WTp[par], UBbp[par], kdp[par], qdTp[par], aqkTp[par]
        pq = str(par)
        lat = sc >= 2
        sd = str(d)
        r0, r1 = hh * 64, hh * 64 + 64
        bA = bank(4 + 2 * d)
        bO = bank(5 + 2 * d)
        pws = bA[r0:r1, 0:128]
        mm(pws, WT[d][:, r0:r1], Sbf[d][:, :], ["WT" + sd + pq, "Sbf" + sd], [f"B{4 + 2 * d}"])
        yield
        stt(ub[d][r0:r1, :], pws, nbet[r0:r1, d, sc:sc + 1], UBb[d][r0:r1, :], ALU.mult, ALU.add, [f"B{4 + 2 * d}", "nbet", "UBb" + sd + pq], ["ub" + sd])
        yield
        if lat:
            po = bO[r0:r1, 0:128]
            mm(po, qdT[d][:, r0:r1], Sbf[d][:, :], ["qdT" + sd + pq, "Sbf" + sd], [f"B{5 + 2 * d}"], start=True, stop=False)
            yield
            mm(po, aqkT[d][r0:r1, r0:r1], ub[d][r0:r1, :], ["aqkT" + sd + pq, "ub" + sd], [f"B{5 + 2 * d}"], start=False, stop=True)
            yield
            tt("dve", og[r0:r1, sc - 2, :], po, og[r0:r1, sc - 2, :], ALU.add, [f"B{5 + 2 * d}", f"og{sc - 2}"], [f"og{sc - 2}"])
            yield
        pS = bA[:, 128:256]
        mm(pS, kd[d][r0:r1, :], ub[d][r0:r1, :], ["kd" + sd + pq, "ub" + sd], [f"B{4 + 2 * d}"])
        yield
        stt(S32[d][:, :], S32[d][:, :], egl[:, d, hh, sc:sc + 1], pS, ALU.mult, ALU.add, ["S32" + sd, "egl", f"B{4 + 2 * d}"], ["S32" + sd])
        yield
        cp("act", Sbf[d][:, :], S32[d][:, :], ["S32" + sd], ["Sbf" + sd])
        yield

    fwd = list(range(NSC))
    bwd = [1, 0] + list(range(NSC - 1, 1, -1))
    def seq(*gs):
        for g in gs:
            yield from g

    def interleave(*gens):
        gens = list(gens)
        while gens:
            for g in list(gens):
                try:
                    next(g)
                except StopIteration:
                    gens.remove(g)

    interleave(precompute(fwd[0], 0, 0), precompute(bwd[0], 1, 0))
    for i in range(NSC):
        par = i % 2
        gl = [seq(step(fwd[i], 0, 0, par), step(fwd[i], 1, 0, par)), seq(step(bwd[i], 1, 1, par), step(bwd[i], 0, 1, par))]
        if i + 1 < NSC:
            gl += [precompute(fwd[i + 1], 0, 1 - par), precompute(bwd[i + 1], 1, 1 - par)]
        interleave(*gl)

    P.op("pool", lambda e: e.memset(small[:, 500:501], 0.0), reads=[f"og{i}" for i in range(64)], writes=["og_all"])
    dbg("og", og[:, :, :], [128, 64, 128], "og_all")

    ogb = A.alloc("ogb", [128, 64, 128], BF16)
    ogs = A.alloc("ogs", [128, 64], F32)
    ogr = A.alloc("ogr", [128, 64], F32)
    ogt = A.alloc("ogt", [128, 128], F32)
    sqj2 = A.alloc("sqj2", [128, 128], BF16)
    for i in range(64):
        act(sqj2[:, :], og[:, i, :], AF.Square, ["og_all"], ["sqj2", "ogs"], accum=ogs[:, i:i + 1])
    act(ogr[:, :], ogs[:, :], AF.Sqrt, ["ogs"], ["ogrA"], bias=EPS, scale=1.0 / 128)
    recip(ogr[:, :], ogr[:, :], ["ogrA"], ["ogr"])
    for i in range(64):
        stt(ogt[:, :], og[:, i, :], ogr[:, i:i + 1], gng[:, :], ALU.mult, ALU.mult, ["og_all", "ogr", "gng"], ["ogt"])
        tt("dve", ogb[:, i, :], ogt[:, :], gate_s[:, i, :], ALU.mult, ["ogt", "gate_s"], ["ogb"])
    dbg("ogb", ogb[:, :, :], [128, 64, 128], "ogb")
    if stage == 2:
        return finish()

    P.barrier()
    after_gdn = A.off
    A.off = persist_mark
    KT0 = A.alloc("KT0", [128, LC + L], BF16)
    KT1 = A.alloc("KT1", [128, LC + L], BF16)
    QT = A.alloc("QT", [128, L], BF16)
    Vv = A.alloc("Vv", [128, NSC, 130], BF16)
    assert A.off <= gdn_mark, A.off
    lim1 = A.off
    A.off = after_gdn
    wout = A.alloc("wout", [128, 2, D], BF16)
    pbuf = [A.alloc(f"pb{i}", [128, 512], BF16) for i in range(4)]
    oas = [A.alloc(f"oa{i}", [128, 128], F32) for i in range(2)]
    obs = [A.alloc(f"ob{i}", [128, 128], F32) for i in range(2)]
    omx = A.alloc("omx", [128, 128], BF16)
    omT = [A.alloc(f"omT{i}", [128, 128], BF16) for i in range(2)]
    rsst = [A.alloc(f"rsst{i}", [128, D], F32) for i in range(2)]
    att = A.alloc("att", [128, 16], F32)
    sqj3 = A.alloc("sqj3", [128, 128], BF16)
    for kc in range(4):
        dma("sp", f"ld{kc % 2}", KT0[0:64, kc * 2112:(kc + 1) * 2112], kt_s[0:64, kc * 2112:(kc + 1) * 2112], ["ktqt_s"], ["KT0a"])
        dma("sp", f"ld{kc % 2}", KT1[64:128, kc * 2112:(kc + 1) * 2112], kt_s[64:128, kc * 2112:(kc + 1) * 2112], ["ktqt_s"], ["KT1a"])
        dma("sp", f"ld{kc % 2}", QT[:, kc * 2048:(kc + 1) * 2048], qt_s[:, kc * 2048:(kc + 1) * 2048], ["ktqt_s"], ["QT"])
    mset("pool", KT0[64:128, :], 0.0, ["KT0b"])
    mset("pool", KT1[0:64, :], 0.0, ["KT1b"])
    for kc in range(6):
        dma("sp", f"ld{kc % 2}", Vv[:, kc * 11:(kc + 1) * 11, :], v_s[kc * 11:(kc + 1) * 11, :, :].rearrange("s p n -> p s n"), ["v_s"], ["Vv"])
    for f_ in range(2):
        dma("pool", f"wo{f_}", wout[:, f_, :], wout_d[f_, :, :], (), ["wout"])

    NKT = NSC
    pcnt = [0]
    if stage == 2.91:
        dbg("KT", KT0[:, :], [128, LC + L], "KT0a")
        dbg("Vv", Vv[:, :, :], [128, NSC, 130], "Vv")
        return finish()
    qorder = [(sblk, rr, qq) for sblk in range(4) for rr in range(4) for qq in range(2)]
    SB = [0, 1, 7]

    def q0_of(ent):
        sblk_, rr_, qq_ = ent
        return rr_ * 2048 + sblk_ * 512 + qq_ * 256

    def s_mm(q0, kt_):
        sb_ = SB[kt_ % 3]
        pS_ = bank(sb_)
        mm(pS_[:, 0:256], KT0[:, kt_ * 128:(kt_ + 1) * 128], QT[:, q0:q0 + 256], ["KT0a", "KT0b", "QT"], [f"B{sb_}"])
        mm(pS_[:, 256:512], KT1[:, kt_ * 128:(kt_ + 1) * 128], QT[:, q0:q0 + 256], ["KT1a", "KT1b", "QT"], [f"B{sb_}"])

    qlist = qorder[:1] if stage in (2.92, 2.93) else qorder
    s_mm(q0_of(qlist[0]), 0)
    s_mm(q0_of(qlist[0]), 1)
    for qi_, (sblk, rr, qq) in enumerate(qlist):
        q0 = q0_of((sblk, rr, qq))
        qt_ = q0 // 256
        for kt_ in range(NKT):
            sb_ = SB[kt_ % 3]
            pi = pcnt[0] % 4
            pcnt[0] += 1
            if kt_ + 2 < NKT:
                s_mm(q0, kt_ + 2)
            act(pbuf[pi][:, :], bank(sb_), AF.Exp, [f"B{sb_}"], [f"pb{pi}"], scale=0.125)
            for sub in range(2):
                for mp in range(2):
                    acc = bank(2 + sub * 2 + mp)[:, 0:129]
                    mm(acc, pbuf[pi][:, mp * 256 + sub * 128:mp * 256 + (sub + 1) * 128], Vv[:, kt_, 0:129], [f"pb{pi}", "Vv"],
                       [f"B{2 + sub * 2 + mp}"], start=(kt_ == 0), stop=(kt_ == NKT - 1))
        if qi_ + 1 < len(qlist):
            s_mm(q0_of(qlist[qi_ + 1]), 0)
            s_mm(q0_of(qlist[qi_ + 1]), 1)
        for sub in range(0 if stage == 2.93 else 2):
            a0 = bank(2 + sub * 2)
            a1_ = bank(3 + sub * 2)
            ss_ = str(sub)
            atc = att[:, 8 * sub:8 * sub + 8]
            recip(atc[:, 0:1], a0[:, 128:129], [f"B{2 + sub * 2}"], ["att0" + ss_])
            recip(atc[:, 1:2], a1_[:, 128:129], [f"B{3 + sub * 2}"], ["att1" + ss_])
            tt("dve", atc[:, 2:3], atc[:, 1:2], nlam, ALU.mult, ["att1" + ss_, "nlam"], ["att2" + ss_])
            ts("dve", oas[sub][:, :], a0[:, 0:128], atc[:, 0:1], None, ALU.mult, None, [f"B{2 + sub * 2}", "att0" + ss_], ["oa" + ss_])
            stt(obs[sub][:, :], a1_[:, 0:128], atc[:, 2:3], oas[sub][:, :], ALU.mult, ALU.add, [f"B{3 + sub * 2}", "att2" + ss_, "oa" + ss_], ["ob" + ss_])
        for sub in range(0 if stage == 2.93 else 2):
            ti = qt_ * 2 + sub
            ss_ = str(sub)
            atc = att[:, 8 * sub:8 * sub + 8]
            oa, ob = oas[sub], obs[sub]
            act(sqj3[:, :], ob[:, :], AF.Square, ["ob" + ss_], ["sqj3", "att3" + ss_], accum=atc[:, 3:4])
            act(atc[:, 4:5], atc[:, 3:4], AF.Sqrt, ["att3" + ss_], ["att4" + ss_], bias=EPS, scale=1.0 / 128)
            recip(atc[:, 5:6], atc[:, 4:5], ["att4" + ss_], ["att5" + ss_])
            ts("dve", oa[:, :], ob[:, :], atc[:, 5:6], 1.0 - LAM_INIT, ALU.mult, ALU.mult, ["ob" + ss_, "att5" + ss_], ["oa" + ss_])
            tt("dve", omx[:, :], oa[:, :], sgg[:, :], ALU.mult, ["oa" + ss_, "sgg"], ["omx"])
            pt_ = bank(6)
            ptb = pt_[:, 0:128].bitcast(BF16)
            tr(ptb[:, 0:128], omx[:, :], identb[:, :], ["omx", "identb"], ["B6"])
            tr(ptb[:, 128:256], ogb[:, ti, :], identb[:, :], ["ogb", "identb"], ["B6"])
            cp("act", omT[0][:, :], ptb[:, 0:128], ["B6"], ["omT0"])
            cp("act", omT[1][:, :], ptb[:, 128:256], ["B6"], ["omT1"])
            rb = rsst[ti % 2]
            for nh in range(2):
                po_ = bank(6)
                mm(po_, omT[0][:, :], wout[:, 0, nh * 512:(nh + 1) * 512], ["omT0", "wout"], ["B6"], start=True, stop=False)
                mm(po_, omT[1][:, :], wout[:, 1, nh * 512:(nh + 1) * 512], ["omT1", "wout"], ["B6"], start=False, stop=True)
                if nh == 0:
                    cp("act", rb[:, 0:512], po_, ["B6"], [f"rsst{ti % 2}"])
                else:
                    cp("dve", rb[:, 512:1024], po_, ["B6"], [f"rsst{ti % 2}"])
            rrow = rr * 512 + qq * 256 + sub * 128
            dma("pool", f"rs{ti % 2}", rs_in[sblk].ap()[rrow:rrow + 128, :], rb[:, :], [f"rsst{ti % 2}"], [f"rs_in{sblk}"])
        if rr == 3 and qq == 1 and stage >= 3:
            P.dma("pool", f"ccrs{sblk}", lambda e, sblk=sblk: e.collective_compute(
                "ReduceScatter", ALU.add, replica_groups=[[0, 1, 2, 3], [4, 5, 6, 7]],
                ins=[rs_in[sblk].ap().opt()], outs=[rs_out[sblk].ap().opt()]), reads=[f"rs_in{sblk}"], writes=[f"rs_out{sblk}"], inc=1)

    if 2.9 <= stage < 3:
        return finish()
    if stage == 3:
        return finish()
    P.barrier()
    A.off = persist_mark
    h2T = A.alloc("h2T", [128, 8, 2048], BF16)
    aff = A.alloc("aff", [128, 16, 16], F32)
    gw = A.alloc("gw", [128, 16, 16], F32)
    gt1bc = A.alloc("gt1bc", [128, D], F32)
    gt2bc = A.alloc("gt2bc", [128, D], F32)
    fgbc = A.alloc("fgbc", [128, D], F32)
    wr = A.alloc("wr", [128, 8, 16], BF16)
    sh2T = col(8)
    sc2T = col(8)
    a2 = col(8)
    n2 = A.alloc("n2", [128, 16], F32)
    ex = A.alloc("ex", [128, 16], F32)
    bs = A.alloc("bs", [64, 8], F32)
    taud = A.alloc("taud", [16, 16], F32)
    taubc = A.alloc("taubc", [128, 16], F32)
    xot = [A.alloc(f"xot{i}", [128, D], F32) for i in range(2)]
    rst = [A.alloc(f"rst{i}", [128, D], F32) for i in range(2)]
    xnw = [A.alloc(f"xnw{i}", [128, D], F32) for i in range(2)]
    sqj4 = A.alloc("sqj4", [128, D], BF16)
    m3_mark = A.off
    modrow = A.alloc("modrow", [1, 4096], F32)
    bm2 = A.alloc("bm2", [1, 4096], F32)
    p3_mark = A.off
    wm2 = A.alloc("wm2", [128, 8, 4096], BF16)
    for kc in range(8):
        dma("pool", f"wm{kc % 2}", wm2[:, kc, :], wmod[kc * 128:(kc + 1) * 128, 2048:6144], (), [f"wm2_{kc}"])
    dma("sp", "sm0", bm2[:, :], bm2_d, (), ["bm2"])
    dma("sp", "sm1", fgbc[:, :], fgbc_d, (), ["fgbc"])
    dma("pool", "wab", wr[:, :, :].rearrange("p a b -> p (a b)"), wr_d, (), ["wr"])
    for cb in range(8):
        pm_ = bank(cb % 2)[0:1, :]
        for kc in range(8):
            mm(pm_, scb[:, kc, 0:1], wm2[:, kc, cb * 512:(cb + 1) * 512], ["scb", f"wm2_{kc}"], [f"B{cb % 2}"], start=(kc == 0), stop=(kc == 7))
        tt("dve", modrow[0:1, cb * 512:(cb + 1) * 512], pm_, bm2[0:1, cb * 512:(cb + 1) * 512], ALU.add, [f"B{cb % 2}", "bm2"], ["modrow"])
    for nh in range(2):
        pb_ = bank(2)
        mm(pb_, ones[0:1, :], modrow[0:1, nh * 512:(nh + 1) * 512], ["cst", "modrow"], ["B2"])
        cp("act", gt1bc[:, nh * 512:(nh + 1) * 512], pb_, ["B2"], ["gt1bc"])
        mm(pb_, ones[0:1, :], modrow[0:1, 3072 + nh * 512:3072 + (nh + 1) * 512], ["cst", "modrow"], ["B2"])
        cp("act", gt2bc[:, nh * 512:(nh + 1) * 512], pb_, ["B2"], ["gt2bc"])
    pc_ = bank(3)
    for kc in range(8):
        mm(pc_[:, kc:kc + 1], modrow[0:1, 1024 + kc * 128:1024 + (kc + 1) * 128], ones[0:1, 0:1], ["modrow", "cst"], ["B3"])
        mm(pc_[:, 8 + kc:9 + kc], modrow[0:1, 2048 + kc * 128:2048 + (kc + 1) * 128], ones[0:1, 0:1], ["modrow", "cst"], ["B3"])
    cp("dve", sh2T, pc_[:, 0:8], ["B3"], ["sh2T"])
    cp("dve", sc2T, pc_[:, 8:16], ["B3"], ["sc2T"])
    stt(a2, sc2T, 1.0, g2T, ALU.add, ALU.mult, ["sc2T", "g2T"], ["a2"])
    P.barrier()
    A.off = p3_mark
    xn2 = [A.alloc(f"xn2_{i}", [128, D], F32) for i in range(2)]
    exs = [ex, A.alloc("exb", [128, 16], F32)]
    affT = A.alloc("affT", [16, 2048], F32)
    affall = A.alloc("affall", [64, 2048], F32)
    cmpj = A.alloc("cmpj", [64, 2048], BF16)
    def p2_front(i):
        b_ = i % 2
        c0 = b_ * 3
        dma("sp", f"xo{b_}", xot[b_][:, :], xo[i * 128:(i + 1) * 128, :], (), [f"xot{b_}"])
        yield
        dma("sp", f"rsl{b_}", rst[b_][:, :], rs_out[i // 4].ap()[(i % 4) * 128:(i % 4 + 1) * 128, :], [f"rs_out{i // 4}"], [f"rst{b_}"])
        yield
        tt("pool", rst[b_][:, :], rst[b_][:, :], gt1bc[:, :], ALU.mult, [f"rst{b_}", "gt1bc"], [f"rst{b_}"])
        yield
        tt("dve", xnw[b_][:, :], rst[b_][:, :], xot[b_][:, :], ALU.add, [f"rst{b_}", f"xot{b_}"], [f"xnw{b_}"])
        yield
        dma("pool", f"xns{b_}", xnew_s[i * 128:(i + 1) * 128, :], xnw[b_][:, :], [f"xnw{b_}"], ["xnew_s"])
        yield
        act(sqj4[:, :], xnw[b_][:, :], AF.Square, [f"xnw{b_}"], ["sqj4", f"n2a{b_}"], accum=n2[:, c0:c0 + 1])
        yield
        act(n2[:, c0 + 1:c0 + 2], n2[:, c0:c0 + 1], AF.Sqrt, [f"n2a{b_}"], [f"n2b{b_}"], bias=EPS, scale=1.0 / D)
        yield
        recip(n2[:, c0 + 2:c0 + 3], n2[:, c0 + 1:c0 + 2], [f"n2b{b_}"], [f"n2c{b_}"])
        yield
        ts("pool", xn2[b_][:, :], xnw[b_][:, :], n2[:, c0 + 2:c0 + 3], 1.0, ALU.mult, ALU.mult, [f"xnw{b_}", f"n2c{b_}"], [f"xn2{b_}"])
        yield

    def p2_back(i):
        b_ = i % 2
        c0 = 6 + b_ * 3
        for kc in range(8):
            pb2 = kc % 2
            pT = bank(pb2)[:, 0:128]
            tr(pT, xn2[b_][:, kc * 128:(kc + 1) * 128], ident, [f"xn2{b_}", "cst"], [f"B{pb2}"])
            yield
            if kc % 2 == 0:
                act(h2T[:, kc, i * 128:(i + 1) * 128], pT, AF.Identity, [f"B{pb2}", "a2", "sh2T"], [f"h2T_{kc}"], bias=sh2T[:, kc:kc + 1], scale=a2[:, kc:kc + 1])
            else:
                ts("dve", h2T[:, kc, i * 128:(i + 1) * 128], pT, a2[:, kc:kc + 1], sh2T[:, kc:kc + 1], ALU.mult, ALU.add, [f"B{pb2}", "a2", "sh2T"], [f"h2T_{kc}"])
            yield
        pl = bank(2)[:, 0:16]
        for kc in range(8):
            mm(pl, h2T[:, kc, i * 128:(i + 1) * 128], wr[:, kc, :], [f"h2T_{kc}", "wr"], ["B2"], start=(kc == 0), stop=(kc == 7))
        yield
        P.op("dve", lambda e, pl=pl, c0=c0: e.tensor_reduce(out=n2[:, c0:c0 + 1], in_=pl, axis=AX.X, op=ALU.max, negate=True), reads=["B2"], writes=[f"n2d{b_}"])
        yield
        act(exs[b_][:, :], pl, AF.Exp, ["B2", f"n2d{b_}"], [f"ex{b_}", f"n2e{b_}"], bias=n2[:, c0:c0 + 1], scale=1.0, accum=n2[:, c0 + 1:c0 + 2])
        yield
        recip(n2[:, c0 + 2:c0 + 3], n2[:, c0 + 1:c0 + 2], [f"n2e{b_}"], [f"n2f{b_}"])
        yield
        ts("dve", aff[:, i, :], exs[b_][:, :], n2[:, c0 + 2:c0 + 3], None, ALU.mult, None, [f"ex{b_}", f"n2f{b_}"], ["aff"])
        yield
        pa_ = bank(3)[0:16, 0:128]
        tr(pa_, aff[:, i, :], ident, ["aff", "cst"], ["B3"])
        yield
        cp("act", affT[:, i * 128:(i + 1) * 128], pa_, ["B3"], ["affT"])
        yield

    interleave(p2_front(0))
    for i in range(16):
        gl = [p2_back(i)]
        if i + 1 < 16:
            gl.append(p2_front(i + 1))
        interleave(*gl)
    dma("pool", "agi", ag_in.ap()[:, :], affT[:, :], ["affT"], ["ag_in"])
    P.dma("pool", "ccag", lambda e: e.collective_compute(
        "AllGather", ALU.bypass, replica_groups=[[0, 1, 2, 3], [4, 5, 6, 7]],
        ins=[ag_in.ap().opt()], outs=[ag_out.ap().opt()]), reads=["ag_in"], writes=["ag_out"], inc=1)
    dma("sp", "ago", affall[:, :], ag_out.ap()[:, :], ["ag_out"], ["affall"])
    mset("pool", bs[:, 0:1], 0.0, ["lo"])
    G64 = C(12)
    for it in range(26):
        hw = 2.0 ** -(it + 1)
        ts("dve", bs[:, 1:2], bs[:, 0:1], hw, None, ALU.add, None, ["lo"], ["mid"])
        ts("dve", cmpj[:, :], affall[:, :], bs[:, 1:2], 0.0, ALU.is_ge, ALU.add, ["affall", "mid"], ["cmpj", "cnt"], accum=bs[:, 2:3])
        pc2 = bank(4)[0:64, 0:1]
        mm(pc2, G64[0:64, 0:64], bs[:, 2:3], ["cst", "cnt"], ["B4"])
        ts("dve", bs[:, 3:4], pc2, 1023.5, hw, ALU.is_ge, ALU.mult, ["B4"], ["gd"])
        tt("dve", bs[:, 0:1], bs[:, 0:1], bs[:, 3:4], ALU.add, ["lo", "gd"], ["lo"])
    ts("dve", taud[:, :], ident[0:16, 0:16], bs[0:16, 0:1], None, ALU.mult, None, ["cst", "lo"], ["taud"])
    ptau = bank(5)[:, 0:16]
    mm(ptau, ones[0:16, :], taud[:, :], ["cst", "taud"], ["B5"])
    cp("dve", taubc[:, :], ptau, ["B5"], ["taubc"])
    for i in range(16):
        tt("dve", gw[:, i, :], aff[:, i, :], taubc[:, :], ALU.is_ge, ["aff", "taubc"], ["gwA"])
        tt("dve", gw[:, i, :], gw[:, i, :], aff[:, i, :], ALU.mult, ["gwA", "aff"], ["gw"])
    dbg("xnew", xnew_s[:, :], [2048, D], "xnew_s")
    dbg("taubc", taubc[:, :], [128, 16], "taubc")
    dbg("aff", aff[:, :, :], [128, 16, 16], "aff")
    dbg("gw", gw[:, :, :], [128, 16, 16], "gw")

    if stage == 4:
        return finish()
    P.barrier()
    A.off = m3_mark
    yacc = A.alloc("yacc", [128, 16, D], F32)
    mset("pool", yacc[:, :, :], 0.0, [f"yacc{a}_{b}" for a in range(16) for b in range(2)])
    wgs = [A.alloc(f"wgs{i}", [128, 8, 512], BF16) for i in range(2)]
    wus = [A.alloc(f"wus{i}", [128, 8, 512], BF16) for i in range(2)]
    wds = [A.alloc(f"wds{i}", [128, 4, D], BF16) for i in range(2)]
    sg = [A.alloc(f"sg{i}", [128, 512], F32) for i in range(1)]
    hid = [A.alloc(f"hid{i}", [128, 512], BF16) for i in range(8)]
    gcnt = [0]

    def gu(e_, hf, TB, ws):
        for fc in range(4):
            gp = gcnt[0] % 2
            gcnt[0] += 1
            bG, bU = gp * 2, gp * 2 + 1
            pG, pU = bank(bG), bank(bU)
            for kc in range(8):
                mm(pG, wgs[ws][:, kc, fc * 128:(fc + 1) * 128], h2T[:, kc, TB * 512:(TB + 1) * 512], [f"wgs{ws}", f"h2T_{kc}"], [f"B{bG}"],
                   start=(kc == 0), stop=(kc == 7))
            for kc in range(8):
                mm(pU, wus[ws][:, kc, fc * 128:(fc + 1) * 128], h2T[:, kc, TB * 512:(TB + 1) * 512], [f"wus{ws}", f"h2T_{kc}"], [f"B{bU}"],
                   start=(kc == 0), stop=(kc == 7))
            act(sg[0][:, :], pG, AF.Silu, [f"B{bG}"], ["sg0"])
            hi = (TB % 2) * 4 + fc
            tt("dve", hid[hi][:, :], sg[0][:, :], pU, ALU.mult, ["sg0", f"B{bU}"], [f"hid{hi}"])

    def down(e_, hf, TB, ws):
        for half in range(2):
            for sub in range(2):
                for nh in range(2):
                    bY = 4 + sub * 2 + nh
                    py_ = bank(bY)
                    c0 = half * 256 + sub * 128
                    for fc in range(4):
                        hi = (TB % 2) * 4 + fc
                        mm(py_, hid[hi][:, c0:c0 + 128], wds[ws][:, fc, nh * 512:(nh + 1) * 512], [f"hid{hi}", f"wds{ws}"],
                           [f"B{bY}"], start=(fc == 0), stop=(fc == 3))
                    ti = TB * 4 + half * 2 + sub
                    stt(yacc[:, ti, nh * 512:(nh + 1) * 512], py_, gw[:, ti, e_:e_ + 1], yacc[:, ti, nh * 512:(nh + 1) * 512], ALU.mult, ALU.add,
                        [f"B{bY}", "gw", f"yacc{ti}_{nh}"], [f"yacc{ti}_{nh}"])

    prev = None
    for e_ in range(16):
        for hf in range(2):
            ws = (e_ * 2 + hf) % 2
            for kk in range(2):
                dma("pool", f"wg{ws}{kk}", wgs[ws][:, kk * 4:(kk + 1) * 4, :],
                    wg_d[e_, kk * 512:(kk + 1) * 512, hf * 512:(hf + 1) * 512].rearrange("(kc p) f -> p kc f", p=128), (), [f"wgs{ws}"])
                dma("pool", f"wu{ws}{kk}", wus[ws][:, kk * 4:(kk + 1) * 4, :],
                    wu_d[e_, kk * 512:(kk + 1) * 512, hf * 512:(hf + 1) * 512].rearrange("(kc p) f -> p kc f", p=128), (), [f"wus{ws}"])
                dma("pool", f"wd{ws}{kk}", wds[ws][:, kk * 2:(kk + 1) * 2, :],
                    wd_d[e_, hf * 512 + kk * 256:hf * 512 + (kk + 1) * 256, :].rearrange("(fc p) n -> p fc n", p=128), (), [f"wds{ws}"])
            for TB in range(4):
                gu(e_, hf, TB, ws)
                if prev is not None:
                    down(*prev)
                prev = (e_, hf, TB, ws)
    down(*prev)


    for i in range(16):
        b_ = i % 2
        dma("sp", f"xo{b_}", xot[b_][:, :], xnew_s[i * 128:(i + 1) * 128, :], ["xnew_s"], [f"xot{b_}"])
        tt("pool", rst[b_][:, :], yacc[:, i, :], gt2bc[:, :], ALU.mult, [f"yacc{i}_0", f"yacc{i}_1", "gt2bc"], [f"rst{b_}"])
        tt("dve", xnw[b_][:, :], rst[b_][:, :], xot[b_][:, :], ALU.add, [f"rst{b_}", f"xot{b_}"], [f"xnw{b_}"])
        act(sqj4[:, :], xnw[b_][:, :], AF.Square, [f"xnw{b_}"], ["sqj4", "n2a"], accum=n2[:, 0:1])
        act(n2[:, 1:2], n2[:, 0:1], AF.Sqrt, ["n2a"], ["n2b"], bias=EPS, scale=1.0 / D)
        recip(n2[:, 2:3], n2[:, 1:2], ["n2b"], ["n2c"])
        stt(rst[b_][:, :], xnw[b_][:, :], n2[:, 2:3], fgbc[:, :], ALU.mult, ALU.mult, [f"xnw{b_}", "n2c", "fgbc"], [f"rst{b_}"])
        dma("pool", f"out{b_}", out_d[i * 128:(i + 1) * 128, :], rst[b_][:, :], [f"rst{b_}"], [f"out{b_}"])
    return finish()


_CACHE = {}


def kernel(x, c, ctx, c_ctx, w_mod, b_mod, norm1_g, w_in, conv_w, a_log, dt_bias, gdn_norm_g,
           lam_q1, lam_k1, lam_q2, lam_k2, da_subln_g, w_out, norm2_g,
           w_router, w_gate, w_up, w_down, final_g):
    f32 = np.float32
    A_ = lambda a: np.ascontiguousarray(np.asarray(a, dtype=f32))
    x, c, ctx, c_ctx = A_(x), A_(c), A_(ctx), A_(c_ctx)
    w_mod, b_mod, w_in, conv_w = A_(w_mod)[0], A_(b_mod)[0], A_(w_in)[0], A_(conv_w)[0]
    a_log, dt_bias = A_(a_log)[0], A_(dt_bias)[0]
    w_out, w_router = A_(w_out)[0], A_(w_router)[0]
    w_gate, w_up, w_down = A_(w_gate)[0], A_(w_up)[0], A_(w_down)[0]
    norm1_g, norm2_g, final_g = A_(norm1_g)[0], A_(norm2_g)[0], A_(final_g)
    gdn_norm_g, da_subln_g = A_(gdn_norm_g)[0], A_(da_subln_g)[0]
    lamcat = np.concatenate([A_(lam_q1)[0], A_(lam_k1)[0], A_(lam_q2)[0], A_(lam_k2)[0]])

    if MAPS_ONLY[0]:
        nc = None
    else:
        key = (tuple(DEBUG), STAGE[0])
        if key not in _CACHE:
            _CACHE[key] = build(debug=key[0], stage=key[1])
        nc, dbg_outs = _CACHE[key]

    consts = make_consts()
    rope = make_rope()

    def colT(v, n):
        return np.ascontiguousarray(v.reshape(n, 128).T)

    in_maps = []
    for core in range(8):
        b, h = core // 4, core % 4
        j = h
        cT = np.zeros((128, 8, 2), f32)
        cT[:, :, 0] = colT(c[b], 8)
        cT[:, :, 1] = colT(c_ctx, 8)
        cols = np.concatenate([
            np.arange(h * 128, h * 128 + 128),
            512 + np.arange(h * 128, h * 128 + 128),
            1536 + np.arange(h * 128, h * 128 + 128),
            1536 + 512 + np.arange(h * 128, h * 128 + 128),
            1536 + 1024 + np.arange(h * 128, h * 128 + 128),
            1024 + np.arange(h * 128, h * 128 + 128),
            3072 + np.arange(h * 128, h * 128 + 128),
        ])
        abcols = 3584 + np.array([0 * 8 + 0 * 4 + h, 0 * 8 + 1 * 4 + h, 1 * 8 + 0 * 4 + h, 1 * 8 + 1 * 4 + h])
        cw = np.zeros((128, 3, 3), f32)
        for cc in range(3):
            for tap in range(3):
                cw[:, cc, tap] = conv_w[tap, cc * 512 + h * 128:cc * 512 + h * 128 + 128]
        gsc = np.tile(np.array([a_log[0, h], a_log[1, h], dt_bias[0, h], dt_bias[1, h]], f32)[None, :], (128, 1))
        m = {
            "xb": x[b], "ctxb": ctx[b], "xo": np.ascontiguousarray(x[b, j * 2048:(j + 1) * 2048]),
            "cT": cT.reshape(128, 16), "wmod": w_mod,
            "bm1T": colT(b_mod[0:2048], 16), "bm2": np.ascontiguousarray(b_mod[2048:].reshape(1, 4096)),
            "g1T": colT(norm1_g, 8), "g2T": colT(norm2_g, 8),
            "fgbc": np.ascontiguousarray(np.tile(final_g[None, :], (128, 1))),
            "wslab": np.ascontiguousarray(w_in[:, cols]), "wab": np.ascontiguousarray(w_in[:, abcols]),
            "cwT": cw.reshape(128, 9), "gsc": np.ascontiguousarray(gsc),
            "gng": np.ascontiguousarray(np.tile(gdn_norm_g[None, :], (128, 1))),
            "sgg": np.ascontiguousarray(np.tile(da_subln_g[None, :], (128, 1))),
            "lamv": np.ascontiguousarray(np.tile(lamcat[None, :], (128, 1))),
            "wout": np.ascontiguousarray(np.stack([w_out[h * 128:(h + 1) * 128], w_out[512 + h * 128:512 + (h + 1) * 128]], 0)),
            "rope": rope, "consts": consts,
            "wr": np.ascontiguousarray(w_router.reshape(8, 128, 16).transpose(1, 0, 2).reshape(128, 128)),
            "wg": w_gate if STAGE[0] > 4 else w_gate[0:1, 0:8, 0:8].copy(),
            "wu": w_up if STAGE[0] > 4 else w_up[0:1, 0:8, 0:8].copy(),
            "wd": w_down if STAGE[0] > 4 else w_down[0:1, 0:8, 0:8].copy(),
        }
        in_maps.append(m)
    if MAPS_ONLY[0]:
        return in_maps
    res = run_bass_kernel_spmd(nc, in_maps, core_ids=list(range(8)), **RUN_KW)
    out = np.zeros((2, L, D), f32)
    LAST["res"] = res
    for core in range(8):
        b, j = core // 4, core % 4
        out[b, j * 2048:(j + 1) * 2048] = res.results[core]["out"]
    return out
```

```python
import math
import numpy as np
import concourse.bass as bass
import concourse.mybir as mybir
from concourse.bass_utils import run_bass_kernel_spmd

F32 = mybir.dt.float32
BF16 = mybir.dt.bfloat16
AF = mybir.ActivationFunctionType
ALU = mybir.AluOpType
AX = mybir.AxisListType

ENGS = ("pe", "act", "dve", "pool", "sp")
EPOCH = 30000
EPS = 1e-6
L = 8192
LC = 256
D = 1024
NSC = 66
LAM_INIT = 0.8 - 0.6 * math.exp(-0.3 * 0)

STAGE = [99]
MAPS_ONLY = [False]
SEQ = [False]
RUN_KW = {}
DEBUG = []
LAST = {}


class Prog:
    def __init__(self, nc, sync_same_engine=True):
        self.nc = nc
        self.ops = {e: [] for e in ENGS}
        self.count = {e: 0 for e in ENGS}
        self.sems = {}
        self.chan_count = {}
        self.chan_inc = {}
        self.waited = {e: {} for e in ENGS}
        self.bufs = {}
        self.sync_same = sync_same_engine

    def sem(self, name):
        if name not in self.sems:
            ctx = self.nc.semaphore(name)
            self.sems[name] = ctx.__enter__()
        return self.sems[name]

    def _need(self, eng, tok, waits):
        if tok is None:
            return
        sname, val, teng = tok
        if teng == eng and (eng == "pe" or not self.sync_same):
            return
        if val <= self.waited[eng].get(sname, 0):
            return
        self.waited[eng][sname] = val
        waits.append((sname, val))

    def _deps(self, eng, reads, writes):
        waits = []
        for k in reads:
            b = self.bufs.get(k)
            if b is not None:
                self._need(eng, b["w"], waits)
                if k[0] == "B":
                    for t in b["r"]:
                        if t[2] != eng:
                            self._need(eng, t, waits)
        for k in writes:
            b = self.bufs.get(k)
            if b is not None:
                self._need(eng, b["w"], waits)
                for t in b["r"]:
                    self._need(eng, t, waits)
        return waits

    def _commit(self, tok, reads, writes):
        for k in reads:
            b = self.bufs.setdefault(k, {"w": None, "r": []})
            b["r"].append(tok)
            if len(b["r"]) > 64:
                b["r"] = b["r"][-48:]
        for k in writes:
            self.bufs[k] = {"w": tok, "r": []}

    def op(self, eng, fn, reads=(), writes=()):
        waits = self._deps(eng, reads, writes)
        idx = self.count[eng]
        self.count[eng] += 1
        ep, k = divmod(idx, EPOCH)
        sname = f"p_{eng}_{ep}"
        self.sem(sname)
        tok = (sname, k + 1, eng)
        self.ops[eng].append((waits, fn, (sname, 1)))
        self._commit(tok, reads, writes)
        return tok

    def dma(self, eng, chan, fn, reads=(), writes=(), inc=16):
        waits = self._deps(eng, reads, writes)
        n = self.chan_count.get(chan, 0)
        sname = f"d_{chan}"
        self.sem(sname)
        self.chan_inc[chan] = inc
        if n > 0:
            self._need(eng, (sname, inc * n, "dma"), waits)
        self.chan_count[chan] = n + 1
        tok = (sname, inc * (n + 1), "dma")
        self.ops[eng].append((waits, fn, (sname, inc)))
        self._commit(tok, reads, writes)
        return tok

    def barrier(self):
        toks = []
        for e in ENGS:
            n = self.count[e]
            if n:
                ep, k = divmod(n - 1, EPOCH)
                toks.append((f"p_{e}_{ep}", k + 1, e))
        for c, n in self.chan_count.items():
            if c.startswith("ccrs"):
                continue
            toks.append((f"d_{c}", self.chan_inc[c] * n, "dma"))
        for e in ENGS:
            waits = []
            for t in toks:
                if t[2] == e and t[2] != "dma":
                    pass
                sname, val, _ = t
                if val > self.waited[e].get(sname, 0):
                    self.waited[e][sname] = val
                    waits.append((sname, val))
            self.ops[e].append((waits, None, None))

    def emit(self):
        nc = self.nc
        sems = self.sems
        ops = self.ops
        with nc.Block() as block:
            def run(e, engobj):
                for waits, fn, inc in ops[e]:
                    for sname, val in waits:
                        engobj.wait_ge(sems[sname], val)
                    if fn is not None:
                        fn(engobj).then_inc(sems[inc[0]], inc[1])

            @block.tensor
            def _(eng):
                run("pe", eng)

            @block.scalar
            def _(eng):
                run("act", eng)

            @block.vector
            def _(eng):
                run("dve", eng)

            @block.gpsimd
            def _(eng):
                run("pool", eng)

            @block.sync
            def _(eng):
                run("sp", eng)


def _isz(dt):
    return 2 if dt == BF16 else 4


class Arena:
    def __init__(self, nc, limit=229376 - 1024):
        self.nc = nc
        self.off = 16384 + 1024
        self.n = 0
        self.limit = limit

    def alloc(self, name, shape, dt):
        sz = _isz(dt)
        for s in shape[1:]:
            sz *= s
        sz = (sz + 63) // 64 * 64
        t = self.nc.alloc_sbuf_tensor_at(f"{name}_{self.n}", list(shape), dt, offset=self.off)
        self.off += sz
        self.n += 1
        assert self.off <= self.limit, (name, self.off)
        return t


NCONST = 13


def make_consts():
    idx = np.arange(128)
    blk = (idx[:, None] // 64) == (idx[None, :] // 64)
    k = idx[:, None]
    m = idx[None, :]
    c = np.zeros((NCONST, 128, 128), np.float32)
    c[0] = np.eye(128)
    c[1] = 1.0
    c[2] = blk & (k <= m)
    c[3] = blk & (k >= m)
    c[4] = blk
    c[5] = (k < 64) & (m >= 0)
    c[6] = (k >= 64) & (m >= 0)
    c[7] = blk & (m > k)
    c[8] = blk & (m >= k)
    c[9] = blk & (m < k)
    c[10] = blk & (m <= k)
    pm = np.zeros((128, 128), np.float32)
    for mm_ in range(128):
        i = mm_ % 64
        r = i % 32
        partner = mm_ + 16 if r < 16 else mm_ - 16
        pm[partner, mm_] = 1.0
    c[11] = pm
    g = np.zeros((128, 128), np.float32)
    g[:64, :64] = (idx[:64, None] % 16) == (idx[None, :64] % 16)
    c[12] = g
    return np.ascontiguousarray(c.transpose(1, 0, 2).reshape(128, NCONST * 128))


def make_rope():
    f32 = np.float32
    t = np.arange(L)
    rows = (t // 64).astype(f32)
    cols = (t % 64).astype(f32)
    inv_freq = np.power(f32(10000.0), -np.arange(0, 32, 2, dtype=f32) / f32(32)).astype(f32)
    ang_row = (rows[:, None] * inv_freq[None, :]).astype(f32)
    ang_col = (cols[:, None] * inv_freq[None, :]).astype(f32)
    cosT = np.zeros((128, L), f32)
    sinT = np.zeros((128, L), f32)
    for p in range(128):
        i = p % 64
        ang = ang_row if i < 32 else ang_col
        r = i % 32
        f = r % 16
        sign = -1.0 if r < 16 else 1.0
        cosT[p] = np.cos(ang[:, f]).astype(f32)
        sinT[p] = (sign * np.sin(ang[:, f])).astype(f32)
    return np.stack([cosT, sinT], 0)


def build(debug=(), stage=99):
    nc = bass.Bass("TRN2", target_bir_lowering=False)
    P = Prog(nc)

    def din(name, shape, dt=F32):
        return nc.dram_tensor(name, list(shape), dt, kind="ExternalInput").ap()

    xb = din("xb", [L, D])
    ctxb = din("ctxb", [LC, D])
    xo = din("xo", [2048, D])
    cT_d = din("cT", [128, 16])
    wmod = din("wmod", [D, 6 * D])
    bm1T_d = din("bm1T", [128, 16])
    bm2_d = din("bm2", [1, 4096])
    g1T_d = din("g1T", [128, 8])
    g2T_d = din("g2T", [128, 8])
    fgbc_d = din("fgbc", [128, D])
    wslab_d = din("wslab", [D, 896])
    wab_d = din("wab", [D, 4])
    cwT_d = din("cwT", [128, 9])
    gsc_d = din("gsc", [128, 4])
    gng_d = din("gng", [128, 128])
    sgg_d = din("sgg", [128, 128])
    lamv_d = din("lamv", [128, 256])
    wout_d = din("wout", [2, 128, D])
    rope_d = din("rope", [2, 128, L])
    consts_d = din("consts", [128, NCONST * 128])
    wr_d = din("wr", [128, 128])
    wshape = [16, D, D] if stage > 4 else [1, 8, 8]
    wg_d = din("wg", wshape)
    wu_d = din("wu", wshape)
    wd_d = din("wd", wshape)
    out_d = nc.dram_tensor("out", [2048, D], F32, kind="ExternalOutput").ap()

    kt_s = nc.dram_tensor("kt_s", [128, LC + L], BF16).ap()
    qt_s = nc.dram_tensor("qt_s", [128, L], BF16).ap()
    v_s = nc.dram_tensor("v_s", [NSC, 128, 130], BF16).ap()
    rs_in = [nc.dram_tensor(f"rs_in{i}", [2048, D], F32) for i in range(4)]
    rs_out = [nc.dram_tensor(f"rs_out{i}", [512, D], F32) for i in range(4)]
    ag_in = nc.dram_tensor("ag_in", [16, 2048], F32)
    ag_out = nc.dram_tensor("ag_out", [64, 2048], F32)
    xnew_s = nc.dram_tensor("xnew_s", [2048, D], F32).ap()

    dbg_outs = {}

    def finish():
        waits = []
        for k in ["out0", "out1"] + ["dbg_" + n for n in dbg_outs]:
            b = P.bufs.get(k)
            if b is not None:
                P._need("pool", b["w"], waits)
        P.ops["pool"].append((waits, None, None))
        P.emit()
        return nc, dbg_outs

    def dbg(name, src_ap, shape, key):
        if name in debug:
            o = nc.dram_tensor("dbg_" + name, list(shape), src_ap.tensor.dtype, kind="ExternalOutput").ap()
            dbg_outs[name] = o
            P.dma("pool", "dbg_" + name, lambda e: e.dma_start(out=o, in_=src_ap), reads=[key], writes=["dbg_" + name])

    psum = nc.alloc_psum_tensor("ps", [128, 8, 512], F32)

    def bank(i):
        return psum[:, i, :]

    def mm(out, lhsT, rhs, r, w, start=True, stop=True):
        P.op("pe", lambda e: e.matmul(out, lhsT=lhsT, rhs=rhs, start=start, stop=stop), reads=r, writes=w)

    def tr(out, in_, idt, r, w):
        P.op("pe", lambda e: e.transpose(out, in_, idt), reads=r, writes=w)

    def act(out, in_, func, r, w, bias=None, scale=None, accum=None):
        kw = {}
        if bias is not None:
            kw["bias"] = bias
        if scale is not None:
            kw["scale"] = scale
        if accum is not None:
            kw["accum_out"] = accum
        P.op("act", lambda e: e.activation(out=out, in_=in_, func=func, **kw), reads=r, writes=w)

    def ts(eng, out, in0, s1, s2, op0, op1, r, w, accum=None):
        if op1 is None:
            P.op(eng, lambda e: e.tensor_scalar(out=out, in0=in0, scalar1=s1, scalar2=None, op0=op0), reads=r, writes=w)
        elif accum is None:
            P.op(eng, lambda e: e.tensor_scalar(out=out, in0=in0, scalar1=s1, scalar2=s2, op0=op0, op1=op1), reads=r, writes=w)
        else:
            P.op(eng, lambda e: e.tensor_scalar(out=out, in0=in0, scalar1=s1, scalar2=s2, op0=op0, op1=op1, accum_out=accum),
                 reads=r, writes=w)

    def tt(eng, out, in0, in1, op, r, w):
        P.op(eng, lambda e: e.tensor_tensor(out=out, in0=in0, in1=in1, op=op), reads=r, writes=w)

    def stt(out, in0, scalar, in1, op0, op1, r, w):
        P.op("dve", lambda e: e.scalar_tensor_tensor(out=out, in0=in0, scalar=scalar, in1=in1, op0=op0, op1=op1), reads=r, writes=w)

    def cp(eng, out, in_, r, w):
        if eng == "act":
            P.op("act", lambda e: e.copy(out=out, in_=in_), reads=r, writes=w)
        else:
            P.op(eng, lambda e: e.tensor_copy(out=out, in_=in_), reads=r, writes=w)

    def recip(out, in_, r, w):
        P.op("dve", lambda e: e.reciprocal(out=out, in_=in_), reads=r, writes=w)

    def mset(eng, ap, val, w):
        P.op(eng, lambda e: e.memset(ap, val), reads=(), writes=w)

    def dma(eng, chan, out, in_, r, w):
        P.dma(eng, chan, lambda e: e.dma_start(out=out, in_=in_), reads=r, writes=w)

    A = Arena(nc)
    cst = A.alloc("cst", [128, NCONST, 128], F32)
    dma("sp", "cst", cst[:, :, :].rearrange("p a b -> p (a b)"), consts_d, (), ["cst"])

    def C(i):
        return cst[:, i, :]
    ident, ones, triF, triB, blkm, H0, H1 = C(0), C(1), C(2), C(3), C(4), C(5), C(6)
    mS = [C(7), C(9)]
    mI = [C(8), C(10)]
    tri = [triF, triB]
    identb = A.alloc("identb", [128, 128], BF16)
    pmb = A.alloc("pmb", [128, 128], BF16)
    cp("dve", identb[:, :], ident, ["cst"], ["identb"])
    cp("dve", pmb[:, :], C(11), ["cst"], ["pmb"])

    small = A.alloc("small", [128, 512], F32)
    _sc = [0]

    def col(n=1):
        c0 = _sc[0]
        _sc[0] += n
        assert _sc[0] <= 512
        return small[:, c0:c0 + n]

    cT = col(16)
    bm1T = col(16)
    g1T = col(8)
    g2T = col(8)
    cwT = col(9)
    gsc = col(4)
    dma("sp", "sm0", cT, cT_d, (), ["cT"])
    dma("sp", "sm1", bm1T, bm1T_d, (), ["bm1T"])
    dma("sp", "sm2", g1T, g1T_d, (), ["g1T"])
    dma("sp", "sm3", g2T, g2T_d, (), ["g2T"])
    dma("sp", "sm4", cwT, cwT_d, (), ["cwT"])
    dma("sp", "sm5", gsc, gsc_d, (), ["gsc"])
    gng = A.alloc("gng", [128, 128], F32)
    sgg = A.alloc("sgg", [128, 128], F32)
    dma("sp", "sm6", gng[:, :], gng_d, (), ["gng"])
    dma("sp", "sm7", sgg[:, :], sgg_d, (), ["sgg"])
    lamv = A.alloc("lamv", [128, 256], F32)
    dma("sp", "sm8", lamv[:, :], lamv_d, (), ["lamv"])

    lamt = col(8)
    lsc = A.alloc("lsc", [128, 64], F32)
    P.op("dve", lambda e: e.tensor_tensor(out=lsc[:, :], in0=lamv[:, 0:64], in1=lamv[:, 64:128], op=ALU.mult), reads=["lamv"], writes=["lsc"])
    P.op("dve", lambda e: e.tensor_reduce(out=lamt[:, 0:1], in_=lsc[:, :], axis=AX.X, op=ALU.add), reads=["lsc"], writes=["lam0"])
    P.op("dve", lambda e: e.tensor_tensor(out=lsc[:, :], in0=lamv[:, 128:192], in1=lamv[:, 192:256], op=ALU.mult), reads=["lamv", "lam0"], writes=["lsc"])
    P.op("dve", lambda e: e.tensor_reduce(out=lamt[:, 1:2], in_=lsc[:, :], axis=AX.X, op=ALU.add), reads=["lsc"], writes=["lam1"])
    act(lamt[:, 2:4], lamt[:, 0:2], AF.Exp, ["lam0", "lam1"], ["lam2"])
    tt("dve", lamt[:, 4:5], lamt[:, 2:3], lamt[:, 3:4], ALU.subtract, ["lam2"], ["lam3"])
    ts("dve", lamt[:, 5:6], lamt[:, 4:5], -1.0, -LAM_INIT, ALU.mult, ALU.add, ["lam3"], ["nlam"])
    nlam = lamt[:, 5:6]

    scb = A.alloc("scb", [128, 8, 2], BF16)
    persist_mark = A.off

    KnT = A.alloc("KnT", [128, NSC * 128], BF16)
    QnT = A.alloc("QnT", [128, NSC * 128], BF16)
    Vg = A.alloc("Vg", [128, NSC, 128], BF16)
    gate_s = A.alloc("gate_s", [128, 64, 128], BF16)
    abt = A.alloc("abt", [128, NSC, 4], F32)
    gdn_mark = A.off

    wslab = A.alloc("wslab", [128, 8, 896], BF16)
    wab = A.alloc("wab", [128, 8, 4], BF16)
    for kc in range(8):
        dma("pool", f"wsl{kc % 2}", wslab[:, kc, :], wslab_d[kc * 128:(kc + 1) * 128, :], (), ["wslab"])
    dma("pool", "wab", wab[:, :, :], wab_d.rearrange("(kc p) n -> p kc n", p=128), (), ["wab"])

    m1 = A.alloc("m1", [128, 16, 2], F32)
    a1 = col(8)
    a1c = col(8)
    ipw_mark = A.off
    wm1 = A.alloc("wm1", [128, 8, 2048], BF16)
    for kc in range(8):
        dma("pool", f"wm{kc}", wm1[:, kc, :], wmod[kc * 128:(kc + 1) * 128, 0:2048], (), [f"wm1_{kc}"])
    act(scb[:, :, :].rearrange("p a b -> p (a b)"), cT, AF.Silu, ["cT"], ["scb"])
    pmod = bank(0)[:, 0:32]
    for c_ in range(16):
        for kc in range(8):
            mm(pmod[:, c_ * 2:(c_ + 1) * 2], wm1[:, kc, c_ * 128:(c_ + 1) * 128], scb[:, kc, :], [f"wm1_{kc}", "scb"], ["B0"],
               start=(kc == 0), stop=(kc == 7))
    pmod3 = pmod.rearrange("p (c v) -> p c v", v=2)
    for v in range(2):
        tt("dve", m1[:, :, v], pmod3[:, :, v], bm1T, ALU.add, ["B0", "bm1T"], ["m1"])
    stt(a1, m1[:, 8:16, 0], 1.0, g1T, ALU.add, ALU.mult, ["m1", "g1T"], ["a1"])
    stt(a1c, m1[:, 8:16, 1], 1.0, g1T, ALU.add, ALU.mult, ["m1", "g1T"], ["a1c"])
    if stage == 0:
        return finish()
    P.barrier()
    A.off = ipw_mark

    NT = 33
    xt = [A.alloc(f"xt{i}", [128, 2, D], F32) for i in range(3)]
    hT = [A.alloc(f"hT{i}", [128, 8, 256], BF16) for i in range(2)]
    pre = [A.alloc(f"pre{i}", [128, 3, 258], F32) for i in range(4)]
    rtab = [A.alloc(f"rtab{i}", [128, 2, 256], F32) for i in range(3)]
    sqj = A.alloc("sqj", [128, D], BF16)
    nrm = A.alloc("nrm", [128, 8], F32)
    qb = [A.alloc(f"qb{i}", [128, 256], BF16) for i in range(2)]
    rt1 = [A.alloc(f"rt1_{i}", [128, 256], F32) for i in range(2)]
    rt2 = [A.alloc(f"rt2_{i}", [128, 256], F32) for i in range(2)]
    qkst = [A.alloc(f"qkst{i}", [128, 256], BF16) for i in range(4)]
    vst = [A.alloc(f"vst{i}", [128, 2, 130], BF16) for i in range(2)]
    cacc = A.alloc("cacc", [128, 3, 256], F32)
    csil = A.alloc("csil", [128, 3, 256], F32)
    csq = A.alloc("csq", [128, 2, 256], F32)
    rinv = A.alloc("rinv", [128, 2, 256], F32)
    for i in range(2):
        mset("pool", vst[i][:, :, 128:130], 1.0, [f"vst{i}"])

    def tile_info(t):
        if t == 0:
            return ctxb, 0, True
        return xb[(t - 1) * 256:t * 256, :], LC + (t - 1) * 256, False

    fm_cnt = [0]

    def loadx(t):
        src, koff, is_ctx = tile_info(t)
        dma("sp", f"x{t % 3}", xt[t % 3][:, :, :], src.rearrange("(s p) d -> p s d", p=128), (), [f"xt{t % 3}"])
        if not is_ctx:
            dma("sp", f"rt{t % 3}", rtab[t % 3][:, :, :], rope_d[:, :, (t - 1) * 256:t * 256].rearrange("a p n -> p a n"), (), [f"rtab{t % 3}"])

    def front(t):
        src, koff, is_ctx = tile_info(t)
        xs = xt[t % 3]
        hs = hT[t % 2]
        kx, kh = f"xt{t % 3}", f"hT{t % 2}"
        sh = m1[:, 0:8, 1] if is_ctx else m1[:, 0:8, 0]
        aa = a1c if is_ctx else a1
        for s in range(2):
            act(sqj[:, :], xs[:, s, :], AF.Square, [kx], ["sqj", f"nrm{s}"], accum=nrm[:, s:s + 1])
            yield
        act(nrm[:, 2:4], nrm[:, 0:2], AF.Sqrt, ["nrm0", "nrm1"], ["nrmB"], bias=EPS, scale=1.0 / D)
        yield
        recip(nrm[:, 4:6], nrm[:, 2:4], ["nrmB"], ["nrmC"])
        yield
        for s in range(2):
            ts("pool", xs[:, s, :], xs[:, s, :], nrm[:, 4 + s:5 + s], 1.0, ALU.mult, ALU.mult, [kx, "nrmC"], [kx])
            yield
        for kc in range(8):
            pb = kc % 2
            pT = bank(pb)[:, 0:256]
            for s in range(2):
                tr(pT[:, s * 128:(s + 1) * 128], xs[:, s, kc * 128:(kc + 1) * 128], ident, [kx, "cst"], [f"B{pb}"])
                yield
            if kc % 2 == 0:
                act(hs[:, kc, :], pT, AF.Identity, [f"B{pb}", "a1", "a1c", "m1"], [f"{kh}_{kc}"], bias=sh[:, kc:kc + 1], scale=aa[:, kc:kc + 1])
                yield
            else:
                ts("dve", hs[:, kc, :], pT, aa[:, kc:kc + 1], sh[:, kc:kc + 1], ALU.mult, ALU.add, [f"B{pb}", "a1", "a1c", "m1"], [f"{kh}_{kc}"])
                yield

    def back(t, part):
        src, koff, is_ctx = tile_info(t)
        hs = hT[t % 2]
        kh = f"hT{t % 2}"
        pr = pre[t % 4]
        kp = f"pre{t % 4}"
        for blk_ in (range(5) if part == 0 else ()):
            if is_ctx and blk_ == 0:
                continue
            fb = 2 + (fm_cnt[0] % 2)
            fm_cnt[0] += 1
            pf = bank(fb)[:, 0:256]
            for kc in range(8):
                mm(pf, wslab[:, kc, blk_ * 128:(blk_ + 1) * 128], hs[:, kc, :], ["wslab", f"{kh}_{kc}"], [f"B{fb}"], start=(kc == 0), stop=(kc == 7))
                yield
            if blk_ < 2:
                st = qkst[(t % 2) * 2 + blk_]
                kst = f"qkst{(t % 2) * 2 + blk_}"
                dst = (qt_s[:, (t - 1) * 256:t * 256] if blk_ == 0 else kt_s[:, koff:koff + 256]) if not is_ctx else kt_s[:, 0:256]
                if is_ctx:
                    cp("act", st[:, :], pf, [f"B{fb}"], [kst])
                    yield
                else:
                    q_b = qb[blk_]
                    rs_ = rtab[t % 3]
                    cp("act", q_b[:, :], pf, [f"B{fb}"], [f"qb{blk_}"])
                    yield
                    pp = bank(6)[:, blk_ * 256:(blk_ + 1) * 256]
                    mm(pp, pmb[:, :], q_b[:, :], ["pmb", f"qb{blk_}"], ["B6"])
                    yield
                    tt("dve", rt1[blk_][:, :], pf, rs_[:, 0, :], ALU.mult, [f"B{fb}", f"rtab{t % 3}"], [f"rt1_{blk_}"])
                    yield
                    tt("dve", rt2[blk_][:, :], pp, rs_[:, 1, :], ALU.mult, ["B6", f"rtab{t % 3}"], [f"rt2_{blk_}"])
                    yield
                    tt("pool", st[:, :], rt1[blk_][:, :], rt2[blk_][:, :], ALU.add, [f"rt1_{blk_}", f"rt2_{blk_}"], [kst])
                    yield
                if stage != 0.325:
                    dma("pool", f"qk{(t % 2) * 2 + blk_}", dst, st[:, :], [kst], ["ktqt_s"])
                    yield
            else:
                c_ = blk_ - 2
                if c_ % 2 == 0:
                    cp("act", pr[:, c_, 1:257], pf, [f"B{fb}"], [kp])
                    yield
                else:
                    cp("dve", pr[:, c_, 1:257], pf, [f"B{fb}"], [kp])
                    yield
        if part == 0:
            return
        vs_ = vst[t % 2]
        for s in range(2):
            sc = (koff // 128) + s
            pv = bank(4 + s)
            kb = f"B{4 + s}"
            for kc in range(8):
                mm(pv[:, 0:128], hs[:, kc, s * 128:(s + 1) * 128], wslab[:, kc, 640:768], [f"{kh}_{kc}", "wslab"], [kb], start=(kc == 0), stop=(kc == 7))
                yield
            if not is_ctx:
                for kc in range(8):
                    mm(pv[:, 128:256], hs[:, kc, s * 128:(s + 1) * 128], wslab[:, kc, 768:896], [f"{kh}_{kc}", "wslab"], [kb], start=(kc == 0), stop=(kc == 7))
                    yield
            if stage != 0.331:
                for kc in range(8):
                    mm(pv[:, 256:260], hs[:, kc, s * 128:(s + 1) * 128], wab[:, kc, :], [f"{kh}_{kc}", "wab"], [kb], start=(kc == 0), stop=(kc == 7))
                    yield
            if stage != 0.333:
                cp("act", vs_[:, s, 0:128], pv[:, 0:128], [kb], [f"vst{t % 2}"])
                yield
            if not is_ctx:
                act(gate_s[:, sc - 2, :], pv[:, 128:256], AF.Silu, [kb], ["gate_s"])
                yield
            if stage not in (0.331, 0.332):
                cp("dve", abt[:, sc, :], pv[:, 256:260], [kb], ["abt"])
                yield
        dma("pool", f"v{t % 2}", v_s[(koff // 128):(koff // 128) + 2, :, :].rearrange("s p n -> p s n"), vs_[:, :, :], [f"vst{t % 2}"], ["v_s"])
        yield

    def conv_stage(t, left, right):
        src, koff, is_ctx = tile_info(t)
        pr = pre[t % 4]
        kp = f"pre{t % 4}"
        if left is None:
            mset("pool", pr[:, :, 0:1], 0.0, [kp])
            yield
        else:
            cp("pool", pr[:, :, 0:1], pre[left % 4][:, :, 256:257], [f"pre{left % 4}", kp], [kp])
            yield
        if right is None:
            mset("pool", pr[:, :, 257:258], 0.0, [kp])
            yield
        else:
            cp("pool", pr[:, :, 257:258], pre[right % 4][:, :, 1:2], [f"pre{right % 4}", kp], [kp])
            yield
        for c_ in range(3):
            ts("dve", cacc[:, c_, :], pr[:, c_, 0:256], cwT[:, c_ * 3:c_ * 3 + 1], None, ALU.mult, None, [kp, "cwT"], ["cacc"])
            yield
            stt(cacc[:, c_, :], pr[:, c_, 1:257], cwT[:, c_ * 3 + 1:c_ * 3 + 2], cacc[:, c_, :], ALU.mult, ALU.add, [kp, "cacc"], ["cacc"])
            yield
            stt(cacc[:, c_, :], pr[:, c_, 2:258], cwT[:, c_ * 3 + 2:c_ * 3 + 3], cacc[:, c_, :], ALU.mult, ALU.add, [kp, "cacc"], ["cacc"])
            yield
        act(csil[:, :, :], cacc[:, :, :], AF.Silu, ["cacc"], ["csil"])
        yield
        tt("pool", csq[:, :, :], csil[:, 0:2, :], csil[:, 0:2, :], ALU.mult, ["csil"], ["csq"])
        yield
        pss = bank(7)
        mm(pss, ones, csq[:, :, :].rearrange("p a b -> p (a b)"), ["cst", "csq"], ["B7"])
        yield
        act(rinv[:, :, :].rearrange("p a b -> p (a b)"), pss, AF.Sqrt, ["B7"], ["rinvA"], bias=EPS, scale=1.0)
        yield
        recip(rinv[:, :, :].rearrange("p a b -> p (a b)"), rinv[:, :, :].rearrange("p a b -> p (a b)"), ["rinvA"], ["rinv"])
        yield
        stt(QnT[:, koff:koff + 256], csil[:, 0, :], 128.0 ** -0.5, rinv[:, 0, :], ALU.mult, ALU.mult, ["csil", "rinv"], ["QnT"])
        yield
        tt("dve", KnT[:, koff:koff + 256], csil[:, 1, :], rinv[:, 1, :], ALU.mult, ["csil", "rinv"], ["KnT"])
        yield
        pvt = bank(7)
        for s in range(2):
            tr(pvt[:, 256 + s * 128:256 + (s + 1) * 128], csil[:, 2, s * 128:(s + 1) * 128], ident, ["csil", "cst"], ["B7"])
            yield
        cp("act", Vg[:, koff // 128:koff // 128 + 2, :], pvt[:, 256:512].rearrange("p (s n) -> p s n", s=2), ["B7"], ["Vg"])
        yield

    def interleave0(*gens):
        gens = list(gens)
        if SEQ[0] == 1:
            for g in gens:
                for _ in g:
                    pass
            return
        while gens:
            for g in list(gens):
                try:
                    next(g)
                except StopIteration:
                    gens.remove(g)

    def conv_for(k):
        left = None if k in (0, 1) else k - 1
        right = None if k in (0, NT - 1) else k + 1
        return conv_stage(k, left, right)

    loadx(0)
    loadx(1)
    interleave0(front(0))
    for t in range(NT):
        if t + 2 < NT:
            loadx(t + 2)
        gl = [back(t, 0), back(t, 1)]
        if t + 1 < NT:
            gl.append(front(t + 1))
        if t - 2 >= 0:
            if SEQ[0] == 2:
                interleave0(*gl)
                gl = []
            gl.append(conv_for(t - 2))
        interleave0(*gl)
    interleave0(conv_for(NT - 2))
    interleave0(conv_for(NT - 1))

    dbg("KnT", KnT[:, :], [128, NSC * 128], "KnT")
    dbg("QnT", QnT[:, :], [128, NSC * 128], "QnT")
    dbg("Vg", Vg[:, :, :], [128, NSC, 128], "Vg")
    dbg("abt", abt[:, :, :], [128, NSC, 4], "abt")

    if stage == 1:
        return finish()
    P.barrier()
    A.off = gdn_mark

    og = A.alloc("og", [128, 64, 128], F32)
    mset("pool", og[:, :, :], 0.0, [f"og{i}" for i in range(64)])
    gg = A.alloc("gg", [128, 2, NSC], F32)
    bet = A.alloc("bet", [128, 2, NSC], F32)
    nbet = A.alloc("nbet", [128, 2, NSC], F32)
    Gc = A.alloc("Gc", [128, 2, NSC], F32)
    eG = A.alloc("eG", [128, 2, NSC], F32)
    eGl = A.alloc("eGl", [128, 2, NSC], F32)
    egl = A.alloc("egl", [128, 2, 2, NSC], F32)
    gtmp = A.alloc("gtmp", [128, 2, NSC], F32)
    nea = col(2)
    act(nea, gsc[:, 0:2], AF.Exp, ["gsc"], ["neaA"])
    ts("dve", nea, nea, -1.0, None, ALU.mult, None, ["neaA"], ["nea"])
    for d in range(2):
        act(gtmp[:, d, :], abt[:, :, d], AF.Exp, ["abt", "gsc"], ["gtmpA"], bias=gsc[:, 2 + d:3 + d], scale=1.0)
        act(gtmp[:, d, :], gtmp[:, d, :], AF.Ln, ["gtmpA"], ["gtmpB"], bias=1.0, scale=1.0)
        ts("dve", gg[:, d, :], gtmp[:, d, :], nea[:, d:d + 1], None, ALU.mult, None, ["gtmpB", "nea"], ["gg"])
        act(bet[:, d, :], abt[:, :, 2 + d], AF.Sigmoid, ["abt"], ["bet"])
        ts("dve", nbet[:, d, :], bet[:, d, :], -1.0, None, ALU.mult, None, ["bet"], ["nbet"])
        pg_ = bank(0)
        mm(pg_[:, 0:NSC], tri[d], gg[:, d, :], ["cst", "gg"], ["B0"])
        mm(pg_[:, 128:128 + NSC], blkm, gg[:, d, :], ["cst", "gg"], ["B0"])
        mm(pg_[:, 256:256 + NSC], H0, gg[:, d, :], ["cst", "gg"], ["B0"])
        mm(pg_[:, 384:384 + NSC], H1, gg[:, d, :], ["cst", "gg"], ["B0"])
        cp("dve", Gc[:, d, :], pg_[:, 0:NSC], ["B0"], ["Gc"])
        act(eG[:, d, :], pg_[:, 0:NSC], AF.Exp, ["B0"], ["eG"])
        tt("dve", gtmp[:, d, :], pg_[:, 128:128 + NSC], Gc[:, d, :], ALU.subtract, ["B0", "Gc", "gtmpB"], ["gtmpC"])
        act(eGl[:, d, :], gtmp[:, d, :], AF.Exp, ["gtmpC"], ["eGl"])
        act(egl[:, d, 0, :], pg_[:, 256:256 + NSC], AF.Exp, ["B0"], ["egl"])
        act(egl[:, d, 1, :], pg_[:, 384:384 + NSC], AF.Exp, ["B0"], ["egl"])

    dbg("gg", gg[:, :, :], [128, 2, NSC], "gg")
    dbg("Gc", Gc[:, :, :], [128, 2, NSC], "Gc")

    def pcbuf(name, dt, n=128):
        return [A.alloc(f"{name}{d}", [128, n], dt) for d in range(2)]
    gram = pcbuf("gram", F32, 256)
    Rw = pcbuf("Rw", BF16)
    kdp = [[A.alloc(f"kd{d}_{q}", [128, 128], BF16) for d in range(2)] for q in range(2)]
    qd = pcbuf("qd", BF16)
    qdTp = [[A.alloc(f"qdT{d}_{q}", [128, 128], BF16) for d in range(2)] for q in range(2)]
    dg = pcbuf("dg", F32)
    tE = pcbuf("tE", F32)
    Es = pcbuf("Es", F32)
    Ei = pcbuf("Ei", F32)
    aqkTp = [[A.alloc(f"aqkT{d}_{q}", [128, 128], BF16) for d in range(2)] for q in range(2)]
    Xb = [pcbuf("Xa", BF16), pcbuf("Xb", BF16)]
    Yb = [pcbuf("Ya", BF16), pcbuf("Yb", BF16)]
    Rb = [pcbuf("Ra", BF16), pcbuf("Rb", BF16)]
    ATb = pcbuf("AT", BF16)
    WTp = [[A.alloc(f"WT{d}_{q}", [128, 128], BF16) for d in range(2)] for q in range(2)]
    UBbp = [[A.alloc(f"UBb{d}_{q}", [128, 128], F32) for d in range(2)] for q in range(2)]
    S32 = pcbuf("S32", F32)
    Sbf = pcbuf("Sbf", BF16)
    ub = pcbuf("ub", BF16)
    for d in range(2):
        mset("pool", S32[d][:, :], 0.0, [f"S32{d}"])
        mset("pool", Sbf[d][:, :], 0.0, [f"Sbf{d}"])

    slot = [0]

    def pslot():
        s = slot[0] % 4
        slot[0] += 1
        return bank(s)[:, 0:128], f"B{s}"

    def pslot_bf():
        ap_, k_ = pslot()
        return ap_[:, 0:64].bitcast(BF16), k_

    def precompute(sc, d, par):
        WT, UBb, kd, qdT, aqkT = WTp[par], UBbp[par], kdp[par], qdTp[par], aqkTp[par]
        pq = str(par)
        lat = sc >= 2
        c0 = sc * 128
        sd = str(d)
        p1, k1 = pslot()
        mm(p1, KnT[:, c0:c0 + 128], KnT[:, c0:c0 + 128], ["KnT"], [k1])
        yield
        cp("act", gram[d][:, 0:128], p1, [k1], ["gramA" + sd])
        yield
        if lat:
            p2, k2 = pslot()
            mm(p2, KnT[:, c0:c0 + 128], QnT[:, c0:c0 + 128], ["KnT", "QnT"], [k2])
            yield
            cp("act", gram[d][:, 128:256], p2, [k2], ["gramB" + sd])
            yield
        p3, k3 = pslot_bf()
        tr(p3, KnT[:, c0:c0 + 128], identb[:, :], ["KnT", "identb"], [k3])
        yield
        act(Rw[d][:, :], p3, AF.Identity, [k3, "eG"], ["Rw" + sd], scale=eG[:, d, sc:sc + 1])
        yield
        ts("dve", kd[d][:, :], p3, eGl[:, d, sc:sc + 1], None, ALU.mult, None, [k3, "eGl"], ["kd" + sd + pq])
        yield
        if lat:
            p4, k4 = pslot_bf()
            tr(p4, QnT[:, c0:c0 + 128], identb[:, :], ["QnT", "identb"], [k4])
            yield
            act(qd[d][:, :], p4, AF.Identity, [k4, "eG"], ["qd" + sd], scale=eG[:, d, sc:sc + 1])
            yield
            p5, k5 = pslot_bf()
            tr(p5, qd[d][:, :], identb[:, :], ["qd" + sd, "identb"], [k5])
            yield
            cp("act", qdT[d][:, :], p5, [k5], ["qdT" + sd + pq])
            yield
        ts("pool", dg[d][:, :], ident, Gc[:, d, sc:sc + 1], 1.0, ALU.mult, ALU.mult, ["cst", "Gc"], ["dg" + sd])
        yield
        p6, k6 = pslot()
        mm(p6, ones, dg[d][:, :], ["cst", "dg" + sd], [k6])
        yield
        ts("dve", tE[d][:, :], p6, Gc[:, d, sc:sc + 1], 0.0, ALU.subtract, ALU.min, [k6, "Gc"], ["tEA" + sd])
        yield
        act(tE[d][:, :], tE[d][:, :], AF.Exp, ["tEA" + sd], ["tE" + sd])
        yield
        tt("pool", Es[d][:, :], tE[d][:, :], mS[d], ALU.mult, ["tE" + sd, "cst"], ["Es" + sd])
        yield
        X0 = Xb[0][d]
        stt(X0[:, :], gram[d][:, 0:128], nbet[:, d, sc:sc + 1], Es[d][:, :], ALU.mult, ALU.mult, ["gramA" + sd, "nbet", "Es" + sd], ["X0" + sd])
        yield
        if lat:
            tt("pool", Ei[d][:, :], tE[d][:, :], mI[d], ALU.mult, ["tE" + sd, "cst"], ["Ei" + sd])
            yield
            tt("dve", aqkT[d][:, :], gram[d][:, 128:256], Ei[d][:, :], ALU.mult, ["gramB" + sd, "Ei" + sd], ["aqkT" + sd + pq])
            yield
        p7, k7 = pslot_bf()
        tr(p7, X0[:, :], identb[:, :], ["X0" + sd, "identb"], [k7])
        yield
        Y0 = Yb[0][d]
        cp("act", Y0[:, :], p7, [k7], ["Y0" + sd])
        yield
        R0 = Rb[0][d]
        tt("pool", R0[:, :], X0[:, :], ident, ALU.add, ["X0" + sd, "cst"], ["R0" + sd])
        yield
        for lv in range(1, 6):
            a_, b_ = (lv - 1) % 2, lv % 2
            Xp, Yp, Rp = Xb[a_][d], Yb[a_][d], Rb[a_][d]
            Xn, Yn, Rn = Xb[b_][d], Yb[b_][d], Rb[b_][d]
            kXp, kYp, kRp = f"X{a_}{sd}", f"Y{a_}{sd}", f"R{a_}{sd}"
            kXn, kYn, kRn = f"X{b_}{sd}", f"Y{b_}{sd}", f"R{b_}{sd}"
            py, ky = pslot()
            mm(py, Xp[:, :], Yp[:, :], [kXp, kYp], [ky])
            yield
            if lv <= 4:
                px, kx_ = pslot()
                mm(px, Yp[:, :], Xp[:, :], [kXp, kYp], [kx_])
                yield
            cp("act", Yn[:, :], py, [ky], [kYn])
            yield
            if lv <= 4:
                cp("dve", Xn[:, :], px, [kx_], [kXn])
                yield
            pr_, kr_ = pslot()
            mm(pr_, Yn[:, :], Rp[:, :], [kYn, kRp], [kr_])
            yield
            if lv < 5:
                tt("dve", Rn[:, :], pr_, Rp[:, :], ALU.add, [kr_, kRp], [kRn])
                yield
            else:
                tt("dve", ATb[d][:, :], pr_, Rp[:, :], ALU.add, [kr_, kRp], ["AT" + sd])
                yield
        p8, k8 = pslot()
        mm(p8, Rw[d][:, :], ATb[d][:, :], ["Rw" + sd, "AT" + sd], [k8])
        yield
        cp("act", WT[d][:, :], p8, [k8], ["WT" + sd + pq])
        yield
        p9, k9 = pslot()
        mm(p9, ATb[d][:, :], Vg[:, sc, :], ["AT" + sd, "Vg"], [k9])
        yield
        act(UBb[d][:, :], p9, AF.Identity, [k9, "bet"], ["UBb" + sd + pq], scale=bet[:, d, sc:sc + 1])
        yield

    def step(sc, hh, d, par):
        WT, UBb, kd, qdT, aqkT = WTp[par], UBbp[par], kdp[par], qdTp[par], aqkTp[par]
        pq = str(par)
        lat = sc >= 2
        sd = str(d)
        r0, r1 = hh * 64, hh * 64 + 64
        bA = bank(4 + 2 * d)
        bO = bank(5 + 2 * d)
        pws = bA[r0:r1, 0:128]
        mm(pws, WT[d][:, r0:r1], Sbf[d][:, :], ["WT" + sd + pq, "Sbf" + sd], [f"B{4 + 2 * d}"])
        yield
        stt(ub[d][r0:r1, :], pws, nbet[r0:r1, d, sc:sc + 1], UBb[d][r0:r1, :], ALU.mult, ALU.add, [f"B{4 + 2 * d}", "nbet", "UBb" + sd + pq], ["ub" + sd])
        yield
        if lat:
            po = bO[r0:r1, 0:128]
            mm(po, qdT[d][:, r0:r1], Sbf[d][:, :], ["qdT" + sd + pq, "Sbf" + sd], [f"B{5 + 2 * d}"], start=True, stop=False)
            yield
            mm(po, aqkT[d][r0:r1, r0:r1], ub[d][r0:r1, :], ["aqkT" + sd + pq, "ub" + sd], [f"B{5 + 2 * d}"], start=False, stop=True)
            yield
            tt("dve", og[r0:r1, sc - 2, :], po, og[r0:r1, sc - 2, :], ALU.add, [f"B{5 + 2 * d}", f"og{sc - 2}"], [f"og{sc - 2}"])
            yield
        pS = bA[:, 128:256]
        mm(pS, kd[d][r0:r1, :], ub[d][r0:r1, :], ["kd" + sd + pq, "ub" + sd], [f"B{4 + 2 * d}"])
        yield
        stt(S32[d][:, :], S32[d][:, :], egl[:, d, hh, sc:sc + 1], pS, ALU.mult, ALU.add, ["S32" + sd, "egl", f"B{4 + 2 * d}"], ["S32" + sd])
        yield
        cp("act", Sbf[d][:, :], S32[d][:, :], ["S32" + sd], ["Sbf" + sd])
        yield

    fwd = list(range(NSC))
    bwd = [1, 0] + list(range(NSC - 1, 1, -1))
    def seq(*gs):
        for g in gs:
            yield from g

    def interleave(*gens):
        gens = list(gens)
        while gens:
            for g in list(gens):
                try:
                    next(g)
                except StopIteration:
                    gens.remove(g)

    interleave(precompute(fwd[0], 0, 0), precompute(bwd[0], 1, 0))
    for i in range(NSC):
        par = i % 2
        gl = [seq(step(fwd[i], 0, 0, par), step(fwd[i], 1, 0, par)), seq(step(bwd[i], 1, 1, par), step(bwd[i], 0, 1, par))]
        if i + 1 < NSC:
            gl += [precompute(fwd[i + 1], 0, 1 - par), precompute(bwd[i + 1], 1, 1 - par)]
        interleave(*gl)

    P.op("pool", lambda e: e.memset(small[:, 500:501], 0.0), reads=[f"og{i}" for i in range(64)], writes=["og_all"])
    dbg("og", og[:, :, :], [128, 64, 128], "og_all")

    ogb = A.alloc("ogb", [128, 64, 128], BF16)
    ogs = A.alloc("ogs", [128, 64], F32)
    ogr = A.alloc("ogr", [128, 64], F32)
    ogt = A.alloc("ogt", [128, 128], F32)
    sqj2 = A.alloc("sqj2", [128, 128], BF16)
    for i in range(64):
        act(sqj2[:, :], og[:, i, :], AF.Square, ["og_all"], ["sqj2", "ogs"], accum=ogs[:, i:i + 1])
    act(ogr[:, :], ogs[:, :], AF.Sqrt, ["ogs"], ["ogrA"], bias=EPS, scale=1.0 / 128)
    recip(ogr[:, :], ogr[:, :], ["ogrA"], ["ogr"])
    for i in range(64):
        stt(ogt[:, :], og[:, i, :], ogr[:, i:i + 1], gng[:, :], ALU.mult, ALU.mult, ["og_all", "ogr", "gng"], ["ogt"])
        tt("dve", ogb[:, i, :], ogt[:, :], gate_s[:, i, :], ALU.mult, ["ogt", "gate_s"], ["ogb"])
    dbg("ogb", ogb[:, :, :], [128, 64, 128], "ogb")
    if stage == 2:
        return finish()

    P.barrier()
    after_gdn = A.off
    A.off = persist_mark
    KT0 = A.alloc("KT0", [128, LC + L], BF16)
    KT1 = A.alloc("KT1", [128, LC + L], BF16)
    QT = A.alloc("QT", [128, L], BF16)
    Vv = A.alloc("Vv", [128, NSC, 130], BF16)
    assert A.off <= gdn_mark, A.off
    lim1 = A.off
    A.off = after_gdn
    wout = A.alloc("wout", [128, 2, D], BF16)
    pbuf = [A.alloc(f"pb{i}", [128, 512], BF16) for i in range(4)]
    oas = [A.alloc(f"oa{i}", [128, 128], F32) for i in range(2)]
    obs = [A.alloc(f"ob{i}", [128, 128], F32) for i in range(2)]
    omx = A.alloc("omx", [128, 128], BF16)
    omT = [A.alloc(f"omT{i}", [128, 128], BF16) for i in range(2)]
    rsst = [A.alloc(f"rsst{i}", [128, D], F32) for i in range(2)]
    att = A.alloc("att", [128, 16], F32)
    sqj3 = A.alloc("sqj3", [128, 128], BF16)
    for kc in range(4):
        dma("sp", f"ld{kc % 2}", KT0[0:64, kc * 2112:(kc + 1) * 2112], kt_s[0:64, kc * 2112:(kc + 1) * 2112], ["ktqt_s"], ["KT0a"])
        dma("sp", f"ld{kc % 2}", KT1[64:128, kc * 2112:(kc + 1) * 2112], kt_s[64:128, kc * 2112:(kc + 1) * 2112], ["ktqt_s"], ["KT1a"])
        dma("sp", f"ld{kc % 2}", QT[:, kc * 2048:(kc + 1) * 2048], qt_s[:, kc * 2048:(kc + 1) * 2048], ["ktqt_s"], ["QT"])
    mset("pool", KT0[64:128, :], 0.0, ["KT0b"])
    mset("pool", KT1[0:64, :], 0.0, ["KT1b"])
    for kc in range(6):
        dma("sp", f"ld{kc % 2}", Vv[:, kc * 11:(kc + 1) * 11, :], v_s[kc * 11:(kc + 1) * 11, :, :].rearrange("s p n -> p s n"), ["v_s"], ["Vv"])
    for f_ in range(2):
        dma("pool", f"wo{f_}", wout[:, f_, :], wout_d[f_, :, :], (), ["wout"])

    NKT = NSC
    pcnt = [0]
    if stage == 2.91:
        dbg("KT", KT0[:, :], [128, LC + L], "KT0a")
        dbg("Vv", Vv[:, :, :], [128, NSC, 130], "Vv")
        return finish()
    qorder = [(sblk, rr, qq) for sblk in range(4) for rr in range(4) for qq in range(2)]
    SB = [0, 1, 7]

    def q0_of(ent):
        sblk_, rr_, qq_ = ent
        return rr_ * 2048 + sblk_ * 512 + qq_ * 256

    def s_mm(q0, kt_):
        sb_ = SB[kt_ % 3]
        pS_ = bank(sb_)
        mm(pS_[:, 0:256], KT0[:, kt_ * 128:(kt_ + 1) * 128], QT[:, q0:q0 + 256], ["KT0a", "KT0b", "QT"], [f"B{sb_}"])
        mm(pS_[:, 256:512], KT1[:, kt_ * 128:(kt_ + 1) * 128], QT[:, q0:q0 + 256], ["KT1a", "KT1b", "QT"], [f"B{sb_}"])

    qlist = qorder[:1] if stage in (2.92, 2.93) else qorder
    s_mm(q0_of(qlist[0]), 0)
    s_mm(q0_of(qlist[0]), 1)
    for qi_, (sblk, rr, qq) in enumerate(qlist):
        q0 = q0_of((sblk, rr, qq))
        qt_ = q0 // 256
        for kt_ in range(NKT):
            sb_ = SB[kt_ % 3]
            pi = pcnt[0] % 4
            pcnt[0] += 1
            if kt_ + 2 < NKT:
                s_mm(q0, kt_ + 2)
            act(pbuf[pi][:, :], bank(sb_), AF.Exp, [f"B{sb_}"], [f"pb{pi}"], scale=0.125)
            for sub in range(2):
                for mp in range(2):
                    acc = bank(2 + sub * 2 + mp)[:, 0:129]
                    mm(acc, pbuf[pi][:, mp * 256 + sub * 128:mp * 256 + (sub + 1) * 128], Vv[:, kt_, 0:129], [f"pb{pi}", "Vv"],
                       [f"B{2 + sub * 2 + mp}"], start=(kt_ == 0), stop=(kt_ == NKT - 1))
        if qi_ + 1 < len(qlist):
            s_mm(q0_of(qlist[qi_ + 1]), 0)
            s_mm(q0_of(qlist[qi_ + 1]), 1)
        for sub in range(0 if stage == 2.93 else 2):
            a0 = bank(2 + sub * 2)
            a1_ = bank(3 + sub * 2)
            ss_ = str(sub)
            atc = att[:, 8 * sub:8 * sub + 8]
            recip(atc[:, 0:1], a0[:, 128:129], [f"B{2 + sub * 2}"], ["att0" + ss_])
            recip(atc[:, 1:2], a1_[:, 128:129], [f"B{3 + sub * 2}"], ["att1" + ss_])
            tt("dve", atc[:, 2:3], atc[:, 1:2], nlam, ALU.mult, ["att1" + ss_, "nlam"], ["att2" + ss_])
            ts("dve", oas[sub][:, :], a0[:, 0:128], atc[:, 0:1], None, ALU.mult, None, [f"B{2 + sub * 2}", "att0" + ss_], ["oa" + ss_])
            stt(obs[sub][:, :], a1_[:, 0:128], atc[:, 2:3], oas[sub][:, :], ALU.mult, ALU.add, [f"B{3 + sub * 2}", "att2" + ss_, "oa" + ss_], ["ob" + ss_])
        for sub in range(0 if stage == 2.93 else 2):
            ti = qt_ * 2 + sub
            ss_ = str(sub)
            atc = att[:, 8 * sub:8 * sub + 8]
            oa, ob = oas[sub], obs[sub]
            act(sqj3[:, :], ob[:, :], AF.Square, ["ob" + ss_], ["sqj3", "att3" + ss_], accum=atc[:, 3:4])
            act(atc[:, 4:5], atc[:, 3:4], AF.Sqrt, ["att3" + ss_], ["att4" + ss_], bias=EPS, scale=1.0 / 128)
            recip(atc[:, 5:6], atc[:, 4:5], ["att4" + ss_], ["att5" + ss_])
            ts("dve", oa[:, :], ob[:, :], atc[:, 5:6], 1.0 - LAM_INIT, ALU.mult, ALU.mult, ["ob" + ss_, "att5" + ss_], ["oa" + ss_])
            tt("dve", omx[:, :], oa[:, :], sgg[:, :], ALU.mult, ["oa" + ss_, "sgg"], ["omx"])
            pt_ = bank(6)
            ptb = pt_[:, 0:128].bitcast(BF16)
            tr(ptb[:, 0:128], omx[:, :], identb[:, :], ["omx", "identb"], ["B6"])
            tr(ptb[:, 128:256], ogb[:, ti, :], identb[:, :], ["ogb", "identb"], ["B6"])
            cp("act", omT[0][:, :], ptb[:, 0:128], ["B6"], ["omT0"])
            cp("act", omT[1][:, :], ptb[:, 128:256], ["B6"], ["omT1"])
            rb = rsst[ti % 2]
            for nh in range(2):
                po_ = bank(6)
                mm(po_, omT[0][:, :], wout[:, 0, nh * 512:(nh + 1) * 512], ["omT0", "wout"], ["B6"], start=True, stop=False)
                mm(po_, omT[1][:, :], wout[:, 1, nh * 512:(nh + 1) * 512], ["omT1", "wout"], ["B6"], start=False, stop=True)
                if nh == 0:
                    cp("act", rb[:, 0:512], po_, ["B6"], [f"rsst{ti % 2}"])
                else:
                    cp("dve", rb[:, 512:1024], po_, ["B6"], [f"rsst{ti % 2}"])
            rrow = rr * 512 + qq * 256 + sub * 128
            dma("pool", f"rs{ti % 2}", rs_in[sblk].ap()[rrow:rrow + 128, :], rb[:, :], [f"rsst{ti % 2}"], [f"rs_in{sblk}"])
        if rr == 3 and qq == 1 and stage >= 3:
            P.dma("pool", f"ccrs{sblk}", lambda e, sblk=sblk: e.collective_compute(
                "ReduceScatter", ALU.add, replica_groups=[[0, 1, 2, 3], [4, 5, 6, 7]],
                ins=[rs_in[sblk].ap().opt()], outs=[rs_out[sblk].ap().opt()]), reads=[f"rs_in{sblk}"], writes=[f"rs_out{sblk}"], inc=1)

    if 2.9 <= stage < 3:
        return finish()
    if stage == 3:
        return finish()
    P.barrier()
    A.off = persist_mark
    h2T = A.alloc("h2T", [128, 8, 2048], BF16)
    aff = A.alloc("aff", [128, 16, 16], F32)
    gw = A.alloc("gw", [128, 16, 16], F32)
    gt1bc = A.alloc("gt1bc", [128, D], F32)
    gt2bc = A.alloc("gt2bc", [128, D], F32)
    fgbc = A.alloc("fgbc", [128, D], F32)
    wr = A.alloc("wr", [128, 8, 16], BF16)
    sh2T = col(8)
    sc2T = col(8)
    a2 = col(8)
    n2 = A.alloc("n2", [128, 16], F32)
    ex = A.alloc("ex", [128, 16], F32)
    bs = A.alloc("bs", [64, 8], F32)
    taud = A.alloc("taud", [16, 16], F32)
    taubc = A.alloc("taubc", [128, 16], F32)
    xot = [A.alloc(f"xot{i}", [128, D], F32) for i in range(2)]
    rst = [A.alloc(f"rst{i}", [128, D], F32) for i in range(2)]
    xnw = [A.alloc(f"xnw{i}", [128, D], F32) for i in range(2)]
    sqj4 = A.alloc("sqj4", [128, D], BF16)
    m3_mark = A.off
    modrow = A.alloc("modrow", [1, 4096], F32)
    bm2 = A.alloc("bm2", [1, 4096], F32)
    p3_mark = A.off
    wm2 = A.alloc("wm2", [128, 8, 4096], BF16)
    for kc in range(8):
        dma("pool", f"wm{kc}", wm2[:, kc, :], wmod[kc * 128:(kc + 1) * 128, 2048:6144], (), [f"wm2_{kc}"])
    dma("sp", "sm0", bm2[:, :], bm2_d, (), ["bm2"])
    dma("sp", "sm1", fgbc[:, :], fgbc_d, (), ["fgbc"])
    dma("pool", "wab", wr[:, :, :].rearrange("p a b -> p (a b)"), wr_d, (), ["wr"])
    for cb in range(8):
        pm_ = bank(cb % 2)[0:1, :]
        for kc in range(8):
            mm(pm_, scb[:, kc, 0:1], wm2[:, kc, cb * 512:(cb + 1) * 512], ["scb", f"wm2_{kc}"], [f"B{cb % 2}"], start=(kc == 0), stop=(kc == 7))
        tt("dve", modrow[0:1, cb * 512:(cb + 1) * 512], pm_, bm2[0:1, cb * 512:(cb + 1) * 512], ALU.add, [f"B{cb % 2}", "bm2"], ["modrow"])
    for nh in range(2):
        pb_ = bank(2)
        mm(pb_, ones[0:1, :], modrow[0:1, nh * 512:(nh + 1) * 512], ["cst", "modrow"], ["B2"])
        cp("act", gt1bc[:, nh * 512:(nh + 1) * 512], pb_, ["B2"], ["gt1bc"])
        mm(pb_, ones[0:1, :], modrow[0:1, 3072 + nh * 512:3072 + (nh + 1) * 512], ["cst", "modrow"], ["B2"])
        cp("act", gt2bc[:, nh * 512:(nh + 1) * 512], pb_, ["B2"], ["gt2bc"])
    pc_ = bank(3)
    for kc in range(8):
        mm(pc_[:, kc:kc + 1], modrow[0:1, 1024 + kc * 128:1024 + (kc + 1) * 128], ones[0:1, 0:1], ["modrow", "cst"], ["B3"])
        mm(pc_[:, 8 + kc:9 + kc], modrow[0:1, 2048 + kc * 128:2048 + (kc + 1) * 128], ones[0:1, 0:1], ["modrow", "cst"], ["B3"])
    cp("dve", sh2T, pc_[:, 0:8], ["B3"], ["sh2T"])
    cp("dve", sc2T, pc_[:, 8:16], ["B3"], ["sc2T"])
    stt(a2, sc2T, 1.0, g2T, ALU.add, ALU.mult, ["sc2T", "g2T"], ["a2"])
    P.barrier()
    A.off = p3_mark
    xn2 = [A.alloc(f"xn2_{i}", [128, D], F32) for i in range(2)]
    exs = [ex, A.alloc("exb", [128, 16], F32)]
    affT = A.alloc("affT", [16, 2048], F32)
    affall = A.alloc("affall", [64, 2048], F32)
    cmpj = A.alloc("cmpj", [64, 2048], BF16)
    def p2_front(i):
        b_ = i % 2
        c0 = b_ * 3
        dma("sp", f"xo{b_}", xot[b_][:, :], xo[i * 128:(i + 1) * 128, :], (), [f"xot{b_}"])
        yield
        dma("sp", f"rsl{b_}", rst[b_][:, :], rs_out[i // 4].ap()[(i % 4) * 128:(i % 4 + 1) * 128, :], [f"rs_out{i // 4}"], [f"rst{b_}"])
        yield
        tt("pool", rst[b_][:, :], rst[b_][:, :], gt1bc[:, :], ALU.mult, [f"rst{b_}", "gt1bc"], [f"rst{b_}"])
        yield
        tt("dve", xnw[b_][:, :], rst[b_][:, :], xot[b_][:, :], ALU.add, [f"rst{b_}", f"xot{b_}"], [f"xnw{b_}"])
        yield
        dma("pool", f"xns{b_}", xnew_s[i * 128:(i + 1) * 128, :], xnw[b_][:, :], [f"xnw{b_}"], ["xnew_s"])
        yield
        act(sqj4[:, :], xnw[b_][:, :], AF.Square, [f"xnw{b_}"], ["sqj4", f"n2a{b_}"], accum=n2[:, c0:c0 + 1])
        yield
        act(n2[:, c0 + 1:c0 + 2], n2[:, c0:c0 + 1], AF.Sqrt, [f"n2a{b_}"], [f"n2b{b_}"], bias=EPS, scale=1.0 / D)
        yield
        recip(n2[:, c0 + 2:c0 + 3], n2[:, c0 + 1:c0 + 2], [f"n2b{b_}"], [f"n2c{b_}"])
        yield
        ts("pool", xn2[b_][:, :], xnw[b_][:, :], n2[:, c0 + 2:c0 + 3], 1.0, ALU.mult, ALU.mult, [f"xnw{b_}", f"n2c{b_}"], [f"xn2{b_}"])
        yield

    def p2_back(i):
        b_ = i % 2
        c0 = 6 + b_ * 3
        for kc in range(8):
            pb2 = kc % 2
            pT = bank(pb2)[:, 0:128]
            tr(pT, xn2[b_][:, kc * 128:(kc + 1) * 128], ident, [f"xn2{b_}", "cst"], [f"B{pb2}"])
            yield
            if kc % 2 == 0:
                act(h2T[:, kc, i * 128:(i + 1) * 128], pT, AF.Identity, [f"B{pb2}", "a2", "sh2T"], [f"h2T_{kc}"], bias=sh2T[:, kc:kc + 1], scale=a2[:, kc:kc + 1])
            else:
                ts("dve", h2T[:, kc, i * 128:(i + 1) * 128], pT, a2[:, kc:kc + 1], sh2T[:, kc:kc + 1], ALU.mult, ALU.add, [f"B{pb2}", "a2", "sh2T"], [f"h2T_{kc}"])
            yield
        pl = bank(2)[:, 0:16]
        for kc in range(8):
            mm(pl, h2T[:, kc, i * 128:(i + 1) * 128], wr[:, kc, :], [f"h2T_{kc}", "wr"], ["B2"], start=(kc == 0), stop=(kc == 7))
        yield
        P.op("dve", lambda e, pl=pl, c0=c0: e.tensor_reduce(out=n2[:, c0:c0 + 1], in_=pl, axis=AX.X, op=ALU.max, negate=True), reads=["B2"], writes=[f"n2d{b_}"])
        yield
        act(exs[b_][:, :], pl, AF.Exp, ["B2", f"n2d{b_}"], [f"ex{b_}", f"n2e{b_}"], bias=n2[:, c0:c0 + 1], scale=1.0, accum=n2[:, c0 + 1:c0 + 2])
        yield
        recip(n2[:, c0 + 2:c0 + 3], n2[:, c0 + 1:c0 + 2], [f"n2e{b_}"], [f"n2f{b_}"])
        yield
        ts("dve", aff[:, i, :], exs[b_][:, :], n2[:, c0 + 2:c0 + 3], None, ALU.mult, None, [f"ex{b_}", f"n2f{b_}"], ["aff"])
        yield
        pa_ = bank(3)[0:16, 0:128]
        tr(pa_, aff[:, i, :], ident, ["aff", "cst"], ["B3"])
        yield
        cp("act", affT[:, i * 128:(i + 1) * 128], pa_, ["B3"], ["affT"])
        yield

    interleave(p2_front(0))
    for i in range(16):
        gl = [p2_back(i)]
        if i + 1 < 16:
            gl.append(p2_front(i + 1))
        interleave(*gl)
    dma("pool", "agi", ag_in.ap()[:, :], affT[:, :], ["affT"], ["ag_in"])
    P.dma("pool", "ccag", lambda e: e.collective_compute(
        "AllGather", ALU.bypass, replica_groups=[[0, 1, 2, 3], [4, 5, 6, 7]],
        ins=[ag_in.ap().opt()], outs=[ag_out.ap().opt()]), reads=["ag_in"], writes=["ag_out"], inc=1)
    dma("sp", "ago", affall[:, :], ag_out.ap()[:, :], ["ag_out"], ["affall"])
    mset("pool", bs[:, 0:1], 0.0, ["lo"])
    G64 = C(12)
    for it in range(24):
        hw = 2.0 ** -(it + 1)
        ts("dve", bs[:, 1:2], bs[:, 0:1], hw, None, ALU.add, None, ["lo"], ["mid"])
        ts("dve", cmpj[:, :], affall[:, :], bs[:, 1:2], 0.0, ALU.is_ge, ALU.add, ["affall", "mid"], ["cmpj", "cnt"], accum=bs[:, 2:3])
        pc2 = bank(4)[0:64, 0:1]
        mm(pc2, G64[0:64, 0:64], bs[:, 2:3], ["cst", "cnt"], ["B4"])
        ts("dve", bs[:, 3:4], pc2, 1023.5, hw, ALU.is_ge, ALU.mult, ["B4"], ["gd"])
        tt("dve", bs[:, 0:1], bs[:, 0:1], bs[:, 3:4], ALU.add, ["lo", "gd"], ["lo"])
    ts("dve", taud[:, :], ident[0:16, 0:16], bs[0:16, 0:1], None, ALU.mult, None, ["cst", "lo"], ["taud"])
    ptau = bank(5)[:, 0:16]
    mm(ptau, ones[0:16, :], taud[:, :], ["cst", "taud"], ["B5"])
    cp("dve", taubc[:, :], ptau, ["B5"], ["taubc"])
    for i in range(16):
        tt("dve", gw[:, i, :], aff[:, i, :], taubc[:, :], ALU.is_ge, ["aff", "taubc"], ["gwA"])
        tt("dve", gw[:, i, :], gw[:, i, :], aff[:, i, :], ALU.mult, ["gwA", "aff"], ["gw"])
    dbg("xnew", xnew_s[:, :], [2048, D], "xnew_s")
    dbg("taubc", taubc[:, :], [128, 16], "taubc")
    dbg("aff", aff[:, :, :], [128, 16, 16], "aff")
    dbg("gw", gw[:, :, :], [128, 16, 16], "gw")

    if stage == 4:
        return finish()
    P.barrier()
    A.off = m3_mark
    yacc = A.alloc("yacc", [128, 16, D], F32)
    mset("pool", yacc[:, :, :], 0.0, [f"yacc{a}_{b}" for a in range(16) for b in range(2)])
    wgs = [A.alloc(f"wgs{i}", [128, 8, 512], BF16) for i in range(2)]
    wus = [A.alloc(f"wus{i}", [128, 8, 512], BF16) for i in range(2)]
    wds = [A.alloc(f"wds{i}", [128, 4, D], BF16) for i in range(2)]
    sg = [A.alloc(f"sg{i}", [128, 512], F32) for i in range(1)]
    hid = [A.alloc(f"hid{i}", [128, 512], BF16) for i in range(8)]
    gcnt = [0]

    def gu(e_, hf, TB, ws):
        for fc in range(4):
            gp = gcnt[0] % 2
            gcnt[0] += 1
            bG, bU = gp * 2, gp * 2 + 1
            pG, pU = bank(bG), bank(bU)
            for kc in range(8):
                mm(pG, wgs[ws][:, kc, fc * 128:(fc + 1) * 128], h2T[:, kc, TB * 512:(TB + 1) * 512], [f"wgs{ws}", f"h2T_{kc}"], [f"B{bG}"],
                   start=(kc == 0), stop=(kc == 7))
            for kc in range(8):
                mm(pU, wus[ws][:, kc, fc * 128:(fc + 1) * 128], h2T[:, kc, TB * 512:(TB + 1) * 512], [f"wus{ws}", f"h2T_{kc}"], [f"B{bU}"],
                   start=(kc == 0), stop=(kc == 7))
            act(sg[0][:, :], pG, AF.Silu, [f"B{bG}"], ["sg0"])
            hi = (TB % 2) * 4 + fc
            tt("dve", hid[hi][:, :], sg[0][:, :], pU, ALU.mult, ["sg0", f"B{bU}"], [f"hid{hi}"])

    def down(e_, hf, TB, ws):
        for half in range(2):
            for sub in range(2):
                for nh in range(2):
                    bY = 4 + sub * 2 + nh
                    py_ = bank(bY)
                    c0 = half * 256 + sub * 128
                    for fc in range(4):
                        hi = (TB % 2) * 4 + fc
                        mm(py_, hid[hi][:, c0:c0 + 128], wds[ws][:, fc, nh * 512:(nh + 1) * 512], [f"hid{hi}", f"wds{ws}"],
                           [f"B{bY}"], start=(fc == 0), stop=(fc == 3))
                    ti = TB * 4 + half * 2 + sub
                    stt(yacc[:, ti, nh * 512:(nh + 1) * 512], py_, gw[:, ti, e_:e_ + 1], yacc[:, ti, nh * 512:(nh + 1) * 512], ALU.mult, ALU.add,
                        [f"B{bY}", "gw", f"yacc{ti}_{nh}"], [f"yacc{ti}_{nh}"])

    prev = None
    for e_ in range(16):
        for hf in range(2):
            ws = (e_ * 2 + hf) % 2
            for kk in range(2):
                dma("pool", f"wg{ws}{kk}", wgs[ws][:, kk * 4:(kk + 1) * 4, :],
                    wg_d[e_, kk * 512:(kk + 1) * 512, hf * 512:(hf + 1) * 512].rearrange("(kc p) f -> p kc f", p=128), (), [f"wgs{ws}"])
                dma("pool", f"wu{ws}{kk}", wus[ws][:, kk * 4:(kk + 1) * 4, :],
                    wu_d[e_, kk * 512:(kk + 1) * 512, hf * 512:(hf + 1) * 512].rearrange("(kc p) f -> p kc f", p=128), (), [f"wus{ws}"])
                dma("pool", f"wd{ws}{kk}", wds[ws][:, kk * 2:(kk + 1) * 2, :],
                    wd_d[e_, hf * 512 + kk * 256:hf * 512 + (kk + 1) * 256, :].rearrange("(fc p) n -> p fc n", p=128), (), [f"wds{ws}"])
            for TB in range(4):
                gu(e_, hf, TB, ws)
                if prev is not None:
                    down(*prev)
                prev = (e_, hf, TB, ws)
    down(*prev)


    for i in range(16):
        b_ = i % 2
        dma("sp", f"xo{b_}", xot[b_][:, :], xnew_s[i * 128:(i + 1) * 128, :], ["xnew_s"], [f"xot{b_}"])
        tt("pool", rst[b_][:, :], yacc[:, i, :], gt2bc[:, :], ALU.mult, [f"yacc{i}_0", f"yacc{i}_1", "gt2bc"], [f"rst{b_}"])
        tt("dve", xnw[b_][:, :], rst[b_][:, :], xot[b_][:, :], ALU.add, [f"rst{b_}", f"xot{b_}"], [f"xnw{b_}"])
        act(sqj4[:, :], xnw[b_][:, :], AF.Square, [f"xnw{b_}"], ["sqj4", "n2a"], accum=n2[:, 0:1])
        act(n2[:, 1:2], n2[:, 0:1], AF.Sqrt, ["n2a"], ["n2b"], bias=EPS, scale=1.0 / D)
        recip(n2[:, 2:3], n2[:, 1:2], ["n2b"], ["n2c"])
        stt(rst[b_][:, :], xnw[b_][:, :], n2[:, 2:3], fgbc[:, :], ALU.mult, ALU.mult, [f"xnw{b_}", "n2c", "fgbc"], [f"rst{b_}"])
        dma("pool", f"out{b_}", out_d[i * 128:(i + 1) * 128, :], rst[b_][:, :], [f"rst{b_}"], [f"out{b_}"])
    return finish()


_CACHE = {}


def kernel(x, c, ctx, c_ctx, w_mod, b_mod, norm1_g, w_in, conv_w, a_log, dt_bias, gdn_norm_g,
           lam_q1, lam_k1, lam_q2, lam_k2, da_subln_g, w_out, norm2_g,
           w_router, w_gate, w_up, w_down, final_g):
    f32 = np.float32
    A_ = lambda a: np.ascontiguousarray(np.asarray(a, dtype=f32))
    x, c, ctx, c_ctx = A_(x), A_(c), A_(ctx), A_(c_ctx)
    w_mod, b_mod, w_in, conv_w = A_(w_mod)[0], A_(b_mod)[0], A_(w_in)[0], A_(conv_w)[0]
    a_log, dt_bias = A_(a_log)[0], A_(dt_bias)[0]
    w_out, w_router = A_(w_out)[0], A_(w_router)[0]
    w_gate, w_up, w_down = A_(w_gate)[0], A_(w_up)[0], A_(w_down)[0]
    norm1_g, norm2_g, final_g = A_(norm1_g)[0], A_(norm2_g)[0], A_(final_g)
    gdn_norm_g, da_subln_g = A_(gdn_norm_g)[0], A_(da_subln_g)[0]
    lamcat = np.concatenate([A_(lam_q1)[0], A_(lam_k1)[0], A_(lam_q2)[0], A_(lam_k2)[0]])

    if MAPS_ONLY[0]:
        nc = None
    else:
        key = (tuple(DEBUG), STAGE[0])
        if key not in _CACHE:
            _CACHE[key] = build(debug=key[0], stage=key[1])
        nc, dbg_outs = _CACHE[key]

    consts = make_consts()
    rope = make_rope()

    def colT(v, n):
        return np.ascontiguousarray(v.reshape(n, 128).T)

    in_maps = []
    for core in range(8):
        b, h = core // 4, core % 4
        j = h
        cT = np.zeros((128, 8, 2), f32)
        cT[:, :, 0] = colT(c[b], 8)
        cT[:, :, 1] = colT(c_ctx, 8)
        cols = np.concatenate([
            np.arange(h * 128, h * 128 + 128),
            512 + np.arange(h * 128, h * 128 + 128),
            1536 + np.arange(h * 128, h * 128 + 128),
            1536 + 512 + np.arange(h * 128, h * 128 + 128),
            1536 + 1024 + np.arange(h * 128, h * 128 + 128),
            1024 + np.arange(h * 128, h * 128 + 128),
            3072 + np.arange(h * 128, h * 128 + 128),
        ])
        abcols = 3584 + np.array([0 * 8 + 0 * 4 + h, 0 * 8 + 1 * 4 + h, 1 * 8 + 0 * 4 + h, 1 * 8 + 1 * 4 + h])
        cw = np.zeros((128, 3, 3), f32)
        for cc in range(3):
            for tap in range(3):
                cw[:, cc, tap] = conv_w[tap, cc * 512 + h * 128:cc * 512 + h * 128 + 128]
        gsc = np.tile(np.array([a_log[0, h], a_log[1, h], dt_bias[0, h], dt_bias[1, h]], f32)[None, :], (128, 1))
        m = {
            "xb": x[b], "ctxb": ctx[b], "xo": np.ascontiguousarray(x[b, j * 2048:(j + 1) * 2048]),
            "cT": cT.reshape(128, 16), "wmod": w_mod,
            "bm1T": colT(b_mod[0:2048], 16), "bm2": np.ascontiguousarray(b_mod[2048:].reshape(1, 4096)),
            "g1T": colT(norm1_g, 8), "g2T": colT(norm2_g, 8),
            "fgbc": np.ascontiguousarray(np.tile(final_g[None, :], (128, 1))),
            "wslab": np.ascontiguousarray(w_in[:, cols]), "wab": np.ascontiguousarray(w_in[:, abcols]),
            "cwT": cw.reshape(128, 9), "gsc": np.ascontiguousarray(gsc),
            "gng": np.ascontiguousarray(np.tile(gdn_norm_g[None, :], (128, 1))),
            "sgg": np.ascontiguousarray(np.tile(da_subln_g[None, :], (128, 1))),
            "lamv": np.ascontiguousarray(np.tile(lamcat[None, :], (128, 1))),
            "wout": np.ascontiguousarray(np.stack([w_out[h * 128:(h + 1) * 128], w_out[512 + h * 128:512 + (h + 1) * 128]], 0)),
            "rope": rope, "consts": consts,
            "wr": np.ascontiguousarray(w_router.reshape(8, 128, 16).transpose(1, 0, 2).reshape(128, 128)),
            "wg": w_gate if STAGE[0] > 4 else w_gate[0:1, 0:8, 0:8].copy(),
            "wu": w_up if STAGE[0] > 4 else w_up[0:1, 0:8, 0:8].copy(),
            "wd": w_down if STAGE[0] > 4 else w_down[0:1, 0:8, 0:8].copy(),
        }
        in_maps.append(m)
    if MAPS_ONLY[0]:
        return in_maps
    res = run_bass_kernel_spmd(nc, in_maps, core_ids=list(range(8)), **RUN_KW)
    out = np.zeros((2, L, D), f32)
    LAST["res"] = res
    for core in range(8):
        b, j = core // 4, core % 4
        out[b, j * 2048:(j + 1) * 2048] = res.results[core]["out"]
    return out
```

```python
import math
import numpy as np
import concourse.bass as bass
import concourse.mybir as mybir
from concourse.bass_utils import run_bass_kernel_spmd

F32 = mybir.dt.float32
BF16 = mybir.dt.bfloat16
AF = mybir.ActivationFunctionType
ALU = mybir.AluOpType
AX = mybir.AxisListType

ENGS = ("pe", "act", "dve", "pool", "sp")
EPOCH = 30000
EPS = 1e-6
L = 8192
LC = 256
D = 1024
NSC = 66
LAM_INIT = 0.8 - 0.6 * math.exp(-0.3 * 0)

STAGE = [99]
MAPS_ONLY = [False]
SEQ = [False]
RUN_KW = {}
DEBUG = []
LAST = {}


class Prog:
    def __init__(self, nc, sync_same_engine=True):
        self.nc = nc
        self.ops = {e: [] for e in ENGS}
        self.count = {e: 0 for e in ENGS}
        self.sems = {}
        self.chan_count = {}
        self.chan_inc = {}
        self.waited = {e: {} for e in ENGS}
        self.bufs = {}
        self.sync_same = sync_same_engine

    def sem(self, name):
        if name not in self.sems:
            ctx = self.nc.semaphore(name)
            self.sems[name] = ctx.__enter__()
        return self.sems[name]

    def _need(self, eng, tok, waits):
        if tok is None:
            return
        sname, val, teng = tok
        if teng == eng and (eng == "pe" or not self.sync_same):
            return
        if val <= self.waited[eng].get(sname, 0):
            return
        self.waited[eng][sname] = val
        waits.append((sname, val))

    def _deps(self, eng, reads, writes):
        waits = []
        for k in reads:
            b = self.bufs.get(k)
            if b is not None:
                self._need(eng, b["w"], waits)
                if k[0] == "B":
                    for t in b["r"]:
                        if t[2] != eng:
                            self._need(eng, t, waits)
        for k in writes:
            b = self.bufs.get(k)
            if b is not None:
                self._need(eng, b["w"], waits)
                for t in b["r"]:
                    self._need(eng, t, waits)
        return waits

    def _commit(self, tok, reads, writes):
        for k in reads:
            b = self.bufs.setdefault(k, {"w": None, "r": []})
            b["r"].append(tok)
            if len(b["r"]) > 64:
                b["r"] = b["r"][-48:]
        for k in writes:
            self.bufs[k] = {"w": tok, "r": []}

    def op(self, eng, fn, reads=(), writes=()):
        waits = self._deps(eng, reads, writes)
        idx = self.count[eng]
        self.count[eng] += 1
        ep, k = divmod(idx, EPOCH)
        sname = f"p_{eng}_{ep}"
        self.sem(sname)
        tok = (sname, k + 1, eng)
        self.ops[eng].append((waits, fn, (sname, 1)))
        self._commit(tok, reads, writes)
        return tok

    def dma(self, eng, chan, fn, reads=(), writes=(), inc=16):
        waits = self._deps(eng, reads, writes)
        n = self.chan_count.get(chan, 0)
        sname = f"d_{chan}"
        self.sem(sname)
        self.chan_inc[chan] = inc
        if n > 0:
            self._need(eng, (sname, inc * n, "dma"), waits)
        self.chan_count[chan] = n + 1
        tok = (sname, inc * (n + 1), "dma")
        self.ops[eng].append((waits, fn, (sname, inc)))
        self._commit(tok, reads, writes)
        return tok

    def barrier(self):
        toks = []
        for e in ENGS:
            n = self.count[e]
            if n:
                ep, k = divmod(n - 1, EPOCH)
                toks.append((f"p_{e}_{ep}", k + 1, e))
        for c, n in self.chan_count.items():
            if c.startswith("ccrs"):
                continue
            toks.append((f"d_{c}", self.chan_inc[c] * n, "dma"))
        for e in ENGS:
            waits = []
            for t in toks:
                if t[2] == e and t[2] != "dma":
                    pass
                sname, val, _ = t
                if val > self.waited[e].get(sname, 0):
                    self.waited[e][sname] = val
                    waits.append((sname, val))
            self.ops[e].append((waits, None, None))

    def emit(self):
        nc = self.nc
        sems = self.sems
        ops = self.ops
        with nc.Block() as block:
            def run(e, engobj):
                for waits, fn, inc in ops[e]:
                    for sname, val in waits:
                        engobj.wait_ge(sems[sname], val)
                    if fn is not None:
                        fn(engobj).then_inc(sems[inc[0]], inc[1])

            @block.tensor
            def _(eng):
                run("pe", eng)

            @block.scalar
            def _(eng):
                run("act", eng)

            @block.vector
            def _(eng):
                run("dve", eng)

            @block.gpsimd
            def _(eng):
                run("pool", eng)

            @block.sync
            def _(eng):
                run("sp", eng)


def _isz(dt):
    return 2 if dt == BF16 else 4


class Arena:
    def __init__(self, nc, limit=229376 - 1024):
        self.nc = nc
        self.off = 16384 + 1024
        self.n = 0
        self.limit = limit

    def alloc(self, name, shape, dt):
        sz = _isz(dt)
        for s in shape[1:]:
            sz *= s
        sz = (sz + 63) // 64 * 64
        t = self.nc.alloc_sbuf_tensor_at(f"{name}_{self.n}", list(shape), dt, offset=self.off)
        self.off += sz
        self.n += 1
        assert self.off <= self.limit, (name, self.off)
        return t


NCONST = 13


def make_consts():
    idx = np.arange(128)
    blk = (idx[:, None] // 64) == (idx[None, :] // 64)
    k = idx[:, None]
    m = idx[None, :]
    c = np.zeros((NCONST, 128, 128), np.float32)
    c[0] = np.eye(128)
    c[1] = 1.0
    c[2] = blk & (k <= m)
    c[3] = blk & (k >= m)
    c[4] = blk
    c[5] = (k < 64) & (m >= 0)
    c[6] = (k >= 64) & (m >= 0)
    c[7] = blk & (m > k)
    c[8] = blk & (m >= k)
    c[9] = blk & (m < k)
    c[10] = blk & (m <= k)
    pm = np.zeros((128, 128), np.float32)
    for mm_ in range(128):
        i = mm_ % 64
        r = i % 32
        partner = mm_ + 16 if r < 16 else mm_ - 16
        pm[partner, mm_] = 1.0
    c[11] = pm
    g = np.zeros((128, 128), np.float32)
    g[:64, :64] = (idx[:64, None] % 16) == (idx[None, :64] % 16)
    c[12] = g
    return np.ascontiguousarray(c.transpose(1, 0, 2).reshape(128, NCONST * 128))


def make_rope():
    f32 = np.float32
    t = np.arange(L)
    rows = (t // 64).astype(f32)
    cols = (t % 64).astype(f32)
    inv_freq = np.power(f32(10000.0), -np.arange(0, 32, 2, dtype=f32) / f32(32)).astype(f32)
    ang_row = (rows[:, None] * inv_freq[None, :]).astype(f32)
    ang_col = (cols[:, None] * inv_freq[None, :]).astype(f32)
    cosT = np.zeros((128, L), f32)
    sinT = np.zeros((128, L), f32)
    for p in range(128):
        i = p % 64
        ang = ang_row if i < 32 else ang_col
        r = i % 32
        f = r % 16
        sign = -1.0 if r < 16 else 1.0
        cosT[p] = np.cos(ang[:, f]).astype(f32)
        sinT[p] = (sign * np.sin(ang[:, f])).astype(f32)
    return np.stack([cosT, sinT], 0)


def build(debug=(), stage=99):
    nc = bass.Bass("TRN2", target_bir_lowering=False)
    P = Prog(nc)

    def din(name, shape, dt=F32):
        return nc.dram_tensor(name, list(shape), dt, kind="ExternalInput").ap()

    xb = din("xb", [L, D])
    ctxb = din("ctxb", [LC, D])
    xo = din("xo", [2048, D])
    cT_d = din("cT", [128, 16])
    wmod = din("wmod", [D, 6 * D])
    bm1T_d = din("bm1T", [128, 16])
    bm2_d = din("bm2", [1, 4096])
    g1T_d = din("g1T", [128, 8])
    g2T_d = din("g2T", [128, 8])
    fgbc_d = din("fgbc", [128, D])
    wslab_d = din("wslab", [D, 896])
    wab_d = din("wab", [D, 4])
    cwT_d = din("cwT", [128, 9])
    gsc_d = din("gsc", [128, 4])
    gng_d = din("gng", [128, 128])
    sgg_d = din("sgg", [128, 128])
    lamv_d = din("lamv", [128, 256])
    wout_d = din("wout", [2, 128, D])
    rope_d = din("rope", [2, 128, L])
    consts_d = din("consts", [128, NCONST * 128])
    wr_d = din("wr", [128, 128])
    wshape = [16, D, D] if stage > 4 else [1, 8, 8]
    wg_d = din("wg", wshape)
    wu_d = din("wu", wshape)
    wd_d = din("wd", wshape)
    out_d = nc.dram_tensor("out", [2048, D], F32, kind="ExternalOutput").ap()

    kt_s = nc.dram_tensor("kt_s", [128, LC + L], BF16).ap()
    qt_s = nc.dram_tensor("qt_s", [128, L], BF16).ap()
    v_s = nc.dram_tensor("v_s", [NSC, 128, 130], BF16).ap()
    rs_in = [nc.dram_tensor(f"rs_in{i}", [2048, D], F32) for i in range(4)]
    rs_out = [nc.dram_tensor(f"rs_out{i}", [512, D], F32) for i in range(4)]
    ag_in = nc.dram_tensor("ag_in", [16, 2048], F32)
    ag_out = nc.dram_tensor("ag_out", [64, 2048], F32)
    xnew_s = nc.dram_tensor("xnew_s", [2048, D], F32).ap()

    dbg_outs = {}

    def finish():
        waits = []
        for k in ["out0", "out1"] + ["dbg_" + n for n in dbg_outs]:
            b = P.bufs.get(k)
            if b is not None:
                P._need("pool", b["w"], waits)
        P.ops["pool"].append((waits, None, None))
        P.emit()
        return nc, dbg_outs

    def dbg(name, src_ap, shape, key):
        if name in debug:
            o = nc.dram_tensor("dbg_" + name, list(shape), src_ap.tensor.dtype, kind="ExternalOutput").ap()
            dbg_outs[name] = o
            P.dma("pool", "dbg_" + name, lambda e: e.dma_start(out=o, in_=src_ap), reads=[key], writes=["dbg_" + name])

    psum = nc.alloc_psum_tensor("ps", [128, 8, 512], F32)

    def bank(i):
        return psum[:, i, :]

    def mm(out, lhsT, rhs, r, w, start=True, stop=True):
        P.op("pe", lambda e: e.matmul(out, lhsT=lhsT, rhs=rhs, start=start, stop=stop), reads=r, writes=w)

    def tr(out, in_, idt, r, w):
        P.op("pe", lambda e: e.transpose(out, in_, idt), reads=r, writes=w)

    def act(out, in_, func, r, w, bias=None, scale=None, accum=None):
        kw = {}
        if bias is not None:
            kw["bias"] = bias
        if scale is not None:
            kw["scale"] = scale
        if accum is not None:
            kw["accum_out"] = accum
        P.op("act", lambda e: e.activation(out=out, in_=in_, func=func, **kw), reads=r, writes=w)

    def ts(eng, out, in0, s1, s2, op0, op1, r, w, accum=None):
        if op1 is None:
            P.op(eng, lambda e: e.tensor_scalar(out=out, in0=in0, scalar1=s1, scalar2=None, op0=op0), reads=r, writes=w)
        elif accum is None:
            P.op(eng, lambda e: e.tensor_scalar(out=out, in0=in0, scalar1=s1, scalar2=s2, op0=op0, op1=op1), reads=r, writes=w)
        else:
            P.op(eng, lambda e: e.tensor_scalar(out=out, in0=in0, scalar1=s1, scalar2=s2, op0=op0, op1=op1, accum_out=accum),
                 reads=r, writes=w)

    def tt(eng, out, in0, in1, op, r, w):
        P.op(eng, lambda e: e.tensor_tensor(out=out, in0=in0, in1=in1, op=op), reads=r, writes=w)

    def stt(out, in0, scalar, in1, op0, op1, r, w):
        P.op("dve", lambda e: e.scalar_tensor_tensor(out=out, in0=in0, scalar=scalar, in1=in1, op0=op0, op1=op1), reads=r, writes=w)

    def cp(eng, out, in_, r, w):
        if eng == "act":
            P.op("act", lambda e: e.copy(out=out, in_=in_), reads=r, writes=w)
        else:
            P.op(eng, lambda e: e.tensor_copy(out=out, in_=in_), reads=r, writes=w)

    def recip(out, in_, r, w):
        P.op("dve", lambda e: e.reciprocal(out=out, in_=in_), reads=r, writes=w)

    def mset(eng, ap, val, w):
        P.op(eng, lambda e: e.memset(ap, val), reads=(), writes=w)

    def dma(eng, chan, out, in_, r, w):
        P.dma(eng, chan, lambda e: e.dma_start(out=out, in_=in_), reads=r, writes=w)

    A = Arena(nc)
    cst = A.alloc("cst", [128, NCONST, 128], F32)
    dma("sp", "cst", cst[:, :, :].rearrange("p a b -> p (a b)"), consts_d, (), ["cst"])

    def C(i):
        return cst[:, i, :]
    ident, ones, triF, triB, blkm, H0, H1 = C(0), C(1), C(2), C(3), C(4), C(5), C(6)
    mS = [C(7), C(9)]
    mI = [C(8), C(10)]
    tri = [triF, triB]
    identb = A.alloc("identb", [128, 128], BF16)
    pmb = A.alloc("pmb", [128, 128], BF16)
    cp("dve", identb[:, :], ident, ["cst"], ["identb"])
    cp("dve", pmb[:, :], C(11), ["cst"], ["pmb"])

    small = A.alloc("small", [128, 512], F32)
    _sc = [0]

    def col(n=1):
        c0 = _sc[0]
        _sc[0] += n
        assert _sc[0] <= 512
        return small[:, c0:c0 + n]

    cT = col(16)
    bm1T = col(16)
    g1T = col(8)
    g2T = col(8)
    cwT = col(9)
    gsc = col(4)
    dma("sp", "sm0", cT, cT_d, (), ["cT"])
    dma("sp", "sm1", bm1T, bm1T_d, (), ["bm1T"])
    dma("sp", "sm2", g1T, g1T_d, (), ["g1T"])
    dma("sp", "sm3", g2T, g2T_d, (), ["g2T"])
    dma("sp", "sm4", cwT, cwT_d, (), ["cwT"])
    dma("sp", "sm5", gsc, gsc_d, (), ["gsc"])
    gng = A.alloc("gng", [128, 128], F32)
    sgg = A.alloc("sgg", [128, 128], F32)
    dma("sp", "sm6", gng[:, :], gng_d, (), ["gng"])
    dma("sp", "sm7", sgg[:, :], sgg_d, (), ["sgg"])
    lamv = A.alloc("lamv", [128, 256], F32)
    dma("sp", "sm8", lamv[:, :], lamv_d, (), ["lamv"])

    lamt = col(8)
    lsc = A.alloc("lsc", [128, 64], F32)
    P.op("dve", lambda e: e.tensor_tensor(out=lsc[:, :], in0=lamv[:, 0:64], in1=lamv[:, 64:128], op=ALU.mult), reads=["lamv"], writes=["lsc"])
    P.op("dve", lambda e: e.tensor_reduce(out=lamt[:, 0:1], in_=lsc[:, :], axis=AX.X, op=ALU.add), reads=["lsc"], writes=["lam0"])
    P.op("dve", lambda e: e.tensor_tensor(out=lsc[:, :], in0=lamv[:, 128:192], in1=lamv[:, 192:256], op=ALU.mult), reads=["lamv", "lam0"], writes=["lsc"])
    P.op("dve", lambda e: e.tensor_reduce(out=lamt[:, 1:2], in_=lsc[:, :], axis=AX.X, op=ALU.add), reads=["lsc"], writes=["lam1"])
    act(lamt[:, 2:4], lamt[:, 0:2], AF.Exp, ["lam0", "lam1"], ["lam2"])
    tt("dve", lamt[:, 4:5], lamt[:, 2:3], lamt[:, 3:4], ALU.subtract, ["lam2"], ["lam3"])
    ts("dve", lamt[:, 5:6], lamt[:, 4:5], -1.0, -LAM_INIT, ALU.mult, ALU.add, ["lam3"], ["nlam"])
    nlam = lamt[:, 5:6]

    scb = A.alloc("scb", [128, 8, 2], BF16)
    persist_mark = A.off

    KnT = A.alloc("KnT", [128, NSC * 128], BF16)
    QnT = A.alloc("QnT", [128, NSC * 128], BF16)
    Vg = A.alloc("Vg", [128, NSC, 128], BF16)
    gate_s = A.alloc("gate_s", [128, 64, 128], BF16)
    abt = A.alloc("abt", [128, NSC, 4], F32)
    gdn_mark = A.off

    wslab = A.alloc("wslab", [128, 8, 896], BF16)
    wab = A.alloc("wab", [128, 8, 4], BF16)
    for kc in range(8):
        dma("pool", f"wsl{kc % 4}", wslab[:, kc, :], wslab_d[kc * 128:(kc + 1) * 128, :], (), ["wslab"])
    dma("pool", "wab", wab[:, :, :], wab_d.rearrange("(kc p) n -> p kc n", p=128), (), ["wab"])

    m1 = A.alloc("m1", [128, 16, 2], F32)
    a1 = col(8)
    a1c = col(8)
    ipw_mark = A.off
    wm1 = A.alloc("wm1", [128, 8, 2048], BF16)
    for kc in range(8):
        dma("pool", f"wm{kc}", wm1[:, kc, :], wmod[kc * 128:(kc + 1) * 128, 0:2048], (), [f"wm1_{kc}"])
    act(scb[:, :, :].rearrange("p a b -> p (a b)"), cT, AF.Silu, ["cT"], ["scb"])
    pmod = bank(0)[:, 0:32]
    for c_ in range(16):
        for kc in range(8):
            mm(pmod[:, c_ * 2:(c_ + 1) * 2], wm1[:, kc, c_ * 128:(c_ + 1) * 128], scb[:, kc, :], [f"wm1_{kc}", "scb"], ["B0"],
               start=(kc == 0), stop=(kc == 7))
    pmod3 = pmod.rearrange("p (c v) -> p c v", v=2)
    for v in range(2):
        tt("dve", m1[:, :, v], pmod3[:, :, v], bm1T, ALU.add, ["B0", "bm1T"], ["m1"])
    stt(a1, m1[:, 8:16, 0], 1.0, g1T, ALU.add, ALU.mult, ["m1", "g1T"], ["a1"])
    stt(a1c, m1[:, 8:16, 1], 1.0, g1T, ALU.add, ALU.mult, ["m1", "g1T"], ["a1c"])
    if stage == 0:
        return finish()
    P.barrier()
    A.off = ipw_mark

    NT = 33
    xt = [A.alloc(f"xt{i}", [128, 2, D], F32) for i in range(3)]
    hT = [A.alloc(f"hT{i}", [128, 8, 256], BF16) for i in range(2)]
    pre = [A.alloc(f"pre{i}", [128, 3, 258], F32) for i in range(4)]
    rtab = [A.alloc(f"rtab{i}", [128, 2, 256], F32) for i in range(3)]
    sqj = A.alloc("sqj", [128, D], BF16)
    nrm = A.alloc("nrm", [128, 8], F32)
    qb = [A.alloc(f"qb{i}", [128, 256], BF16) for i in range(2)]
    rt1 = [A.alloc(f"rt1_{i}", [128, 256], F32) for i in range(2)]
    rt2 = [A.alloc(f"rt2_{i}", [128, 256], F32) for i in range(2)]
    qkst = [A.alloc(f"qkst{i}", [128, 256], BF16) for i in range(4)]
    vst = [A.alloc(f"vst{i}", [128, 2, 130], BF16) for i in range(2)]
    cacc = A.alloc("cacc", [128, 3, 256], F32)
    csil = A.alloc("csil", [128, 3, 256], F32)
    csq = A.alloc("csq", [128, 2, 256], F32)
    rinv = A.alloc("rinv", [128, 2, 256], F32)
    for i in range(2):
        mset("pool", vst[i][:, :, 128:130], 1.0, [f"vst{i}"])

    def tile_info(t):
        if t == 0:
            return ctxb, 0, True
        return xb[(t - 1) * 256:t * 256, :], LC + (t - 1) * 256, False

    fm_cnt = [0]

    def loadx(t):
        src, koff, is_ctx = tile_info(t)
        dma("sp", f"x{t % 3}", xt[t % 3][:, :, :], src.rearrange("(s p) d -> p s d", p=128), (), [f"xt{t % 3}"])
        if not is_ctx:
            dma("sp", f"rt{t % 3}", rtab[t % 3][:, :, :], rope_d[:, :, (t - 1) * 256:t * 256].rearrange("a p n -> p a n"), (), [f"rtab{t % 3}"])

    def front(t):
        src, koff, is_ctx = tile_info(t)
        xs = xt[t % 3]
        hs = hT[t % 2]
        kx, kh = f"xt{t % 3}", f"hT{t % 2}"
        sh = m1[:, 0:8, 1] if is_ctx else m1[:, 0:8, 0]
        aa = a1c if is_ctx else a1
        for s in range(2):
            act(sqj[:, :], xs[:, s, :], AF.Square, [kx], ["sqj", f"nrm{s}"], accum=nrm[:, s:s + 1])
            yield
        act(nrm[:, 2:4], nrm[:, 0:2], AF.Sqrt, ["nrm0", "nrm1"], ["nrmB"], bias=EPS, scale=1.0 / D)
        yield
        recip(nrm[:, 4:6], nrm[:, 2:4], ["nrmB"], ["nrmC"])
        yield
        for s in range(2):
            ts("pool", xs[:, s, :], xs[:, s, :], nrm[:, 4 + s:5 + s], 1.0, ALU.mult, ALU.mult, [kx, "nrmC"], [kx])
            yield
        for kc in range(8):
            pb = kc % 2
            pT = bank(pb)[:, 0:256]
            for s in range(2):
                tr(pT[:, s * 128:(s + 1) * 128], xs[:, s, kc * 128:(kc + 1) * 128], ident, [kx, "cst"], [f"B{pb}"])
                yield
            if kc % 2 == 0:
                act(hs[:, kc, :], pT, AF.Identity, [f"B{pb}", "a1", "a1c", "m1"], [f"{kh}_{kc}"], bias=sh[:, kc:kc + 1], scale=aa[:, kc:kc + 1])
                yield
            else:
                ts("dve", hs[:, kc, :], pT, aa[:, kc:kc + 1], sh[:, kc:kc + 1], ALU.mult, ALU.add, [f"B{pb}", "a1", "a1c", "m1"], [f"{kh}_{kc}"])
                yield

    def back(t, part):
        src, koff, is_ctx = tile_info(t)
        hs = hT[t % 2]
        kh = f"hT{t % 2}"
        pr = pre[t % 4]
        kp = f"pre{t % 4}"
        for blk_ in (range(5) if part == 0 else ()):
            if is_ctx and blk_ == 0:
                continue
            fb = 2 + (fm_cnt[0] % 2)
            fm_cnt[0] += 1
            pf = bank(fb)[:, 0:256]
            for kc in range(8):
                mm(pf, wslab[:, kc, blk_ * 128:(blk_ + 1) * 128], hs[:, kc, :], ["wslab", f"{kh}_{kc}"], [f"B{fb}"], start=(kc == 0), stop=(kc == 7))
                yield
            if blk_ < 2:
                st = qkst[(t % 2) * 2 + blk_]
                kst = f"qkst{(t % 2) * 2 + blk_}"
                dst = (qt_s[:, (t - 1) * 256:t * 256] if blk_ == 0 else kt_s[:, koff:koff + 256]) if not is_ctx else kt_s[:, 0:256]
                if is_ctx:
                    cp("act", st[:, :], pf, [f"B{fb}"], [kst])
                    yield
                else:
                    q_b = qb[blk_]
                    rs_ = rtab[t % 3]
                    cp("act", q_b[:, :], pf, [f"B{fb}"], [f"qb{blk_}"])
                    yield
                    pp = bank(6)[:, blk_ * 256:(blk_ + 1) * 256]
                    mm(pp, pmb[:, :], q_b[:, :], ["pmb", f"qb{blk_}"], ["B6"])
                    yield
                    tt("dve", rt1[blk_][:, :], pf, rs_[:, 0, :], ALU.mult, [f"B{fb}", f"rtab{t % 3}"], [f"rt1_{blk_}"])
                    yield
                    tt("dve", rt2[blk_][:, :], pp, rs_[:, 1, :], ALU.mult, ["B6", f"rtab{t % 3}"], [f"rt2_{blk_}"])
                    yield
                    tt("pool", st[:, :], rt1[blk_][:, :], rt2[blk_][:, :], ALU.add, [f"rt1_{blk_}", f"rt2_{blk_}"], [kst])
                    yield
                if stage != 0.325:
                    dma("pool", f"qk{(t % 2) * 2 + blk_}", dst, st[:, :], [kst], ["ktqt_s"])
                    yield
            else:
                c_ = blk_ - 2
                if c_ % 2 == 0:
                    cp("act", pr[:, c_, 1:257], pf, [f"B{fb}"], [kp])
                    yield
                else:
                    cp("dve", pr[:, c_, 1:257], pf, [f"B{fb}"], [kp])
                    yield
        if part == 0:
            return
        vs_ = vst[t % 2]
        for s in range(2):
            sc = (koff // 128) + s
            pv = bank(4 + s)
            kb = f"B{4 + s}"
            for kc in range(8):
                mm(pv[:, 0:128], hs[:, kc, s * 128:(s + 1) * 128], wslab[:, kc, 640:768], [f"{kh}_{kc}", "wslab"], [kb], start=(kc == 0), stop=(kc == 7))
                yield
            if not is_ctx:
                for kc in range(8):
                    mm(pv[:, 128:256], hs[:, kc, s * 128:(s + 1) * 128], wslab[:, kc, 768:896], [f"{kh}_{kc}", "wslab"], [kb], start=(kc == 0), stop=(kc == 7))
                    yield
            if stage != 0.331:
                for kc in range(8):
                    mm(pv[:, 256:260], hs[:, kc, s * 128:(s + 1) * 128], wab[:, kc, :], [f"{kh}_{kc}", "wab"], [kb], start=(kc == 0), stop=(kc == 7))
                    yield
            if stage != 0.333:
                cp("act", vs_[:, s, 0:128], pv[:, 0:128], [kb], [f"vst{t % 2}"])
                yield
            if not is_ctx:
                act(gate_s[:, sc - 2, :], pv[:, 128:256], AF.Silu, [kb], ["gate_s"])
                yield
            if stage not in (0.331, 0.332):
                cp("dve", abt[:, sc, :], pv[:, 256:260], [kb], ["abt"])
                yield
        dma("pool", f"v{t % 2}", v_s[(koff // 128):(koff // 128) + 2, :, :].rearrange("s p n -> p s n"), vs_[:, :, :], [f"vst{t % 2}"], ["v_s"])
        yield

    def conv_stage(t, left, right):
        src, koff, is_ctx = tile_info(t)
        pr = pre[t % 4]
        kp = f"pre{t % 4}"
        if left is None:
            mset("pool", pr[:, :, 0:1], 0.0, [kp])
            yield
        else:
            cp("pool", pr[:, :, 0:1], pre[left % 4][:, :, 256:257], [f"pre{left % 4}", kp], [kp])
            yield
        if right is None:
            mset("pool", pr[:, :, 257:258], 0.0, [kp])
            yield
        else:
            cp("pool", pr[:, :, 257:258], pre[right % 4][:, :, 1:2], [f"pre{right % 4}", kp], [kp])
            yield
        for c_ in range(3):
            ts("dve", cacc[:, c_, :], pr[:, c_, 0:256], cwT[:, c_ * 3:c_ * 3 + 1], None, ALU.mult, None, [kp, "cwT"], ["cacc"])
            yield
            stt(cacc[:, c_, :], pr[:, c_, 1:257], cwT[:, c_ * 3 + 1:c_ * 3 + 2], cacc[:, c_, :], ALU.mult, ALU.add, [kp, "cacc"], ["cacc"])
            yield
            stt(cacc[:, c_, :], pr[:, c_, 2:258], cwT[:, c_ * 3 + 2:c_ * 3 + 3], cacc[:, c_, :], ALU.mult, ALU.add, [kp, "cacc"], ["cacc"])
            yield
        act(csil[:, :, :], cacc[:, :, :], AF.Silu, ["cacc"], ["csil"])
        yield
        tt("pool", csq[:, :, :], csil[:, 0:2, :], csil[:, 0:2, :], ALU.mult, ["csil"], ["csq"])
        yield
        pss = bank(7)
        mm(pss, ones, csq[:, :, :].rearrange("p a b -> p (a b)"), ["cst", "csq"], ["B7"])
        yield
        act(rinv[:, :, :].rearrange("p a b -> p (a b)"), pss, AF.Sqrt, ["B7"], ["rinvA"], bias=EPS, scale=1.0)
        yield
        recip(rinv[:, :, :].rearrange("p a b -> p (a b)"), rinv[:, :, :].rearrange("p a b -> p (a b)"), ["rinvA"], ["rinv"])
        yield
        stt(QnT[:, koff:koff + 256], csil[:, 0, :], 128.0 ** -0.5, rinv[:, 0, :], ALU.mult, ALU.mult, ["csil", "rinv"], ["QnT"])
        yield
        tt("dve", KnT[:, koff:koff + 256], csil[:, 1, :], rinv[:, 1, :], ALU.mult, ["csil", "rinv"], ["KnT"])
        yield
        pvt = bank(7)
        for s in range(2):
            tr(pvt[:, 256 + s * 128:256 + (s + 1) * 128], csil[:, 2, s * 128:(s + 1) * 128], ident, ["csil", "cst"], ["B7"])
            yield
        cp("act", Vg[:, koff // 128:koff // 128 + 2, :], pvt[:, 256:512].rearrange("p (s n) -> p s n", s=2), ["B7"], ["Vg"])
        yield

    def interleave0(*gens):
        gens = list(gens)
        if SEQ[0] == 1:
            for g in gens:
                for _ in g:
                    pass
            return
        while gens:
            for g in list(gens):
                try:
                    next(g)
                except StopIteration:
                    gens.remove(g)

    def conv_for(k):
        left = None if k in (0, 1) else k - 1
        right = None if k in (0, NT - 1) else k + 1
        return conv_stage(k, left, right)

    loadx(0)
    loadx(1)
    interleave0(front(0))
    for t in range(NT):
        if t + 2 < NT:
            loadx(t + 2)
        gl = [back(t, 0), back(t, 1)]
        if t + 1 < NT:
            gl.append(front(t + 1))
        if t - 2 >= 0:
            if SEQ[0] == 2:
                interleave0(*gl)
                gl = []
            gl.append(conv_for(t - 2))
        interleave0(*gl)
    interleave0(conv_for(NT - 2))
    interleave0(conv_for(NT - 1))

    dbg("KnT", KnT[:, :], [128, NSC * 128], "KnT")
    dbg("QnT", QnT[:, :], [128, NSC * 128], "QnT")
    dbg("Vg", Vg[:, :, :], [128, NSC, 128], "Vg")
    dbg("abt", abt[:, :, :], [128, NSC, 4], "abt")

    if stage == 1:
        return finish()
    P.barrier()
    A.off = gdn_mark

    og = A.alloc("og", [128, 64, 128], F32)
    mset("pool", og[:, :, :], 0.0, [f"og{i}" for i in range(64)])
    gg = A.alloc("gg", [128, 2, NSC], F32)
    bet = A.alloc("bet", [128, 2, NSC], F32)
    nbet = A.alloc("nbet", [128, 2, NSC], F32)
    Gc = A.alloc("Gc", [128, 2, NSC], F32)
    eG = A.alloc("eG", [128, 2, NSC], F32)
    eGl = A.alloc("eGl", [128, 2, NSC], F32)
    egl = A.alloc("egl", [128, 2, 2, NSC], F32)
    gtmp = A.alloc("gtmp", [128, 2, NSC], F32)
    nea = col(2)
    act(nea, gsc[:, 0:2], AF.Exp, ["gsc"], ["neaA"])
    ts("dve", nea, nea, -1.0, None, ALU.mult, None, ["neaA"], ["nea"])
    for d in range(2):
        act(gtmp[:, d, :], abt[:, :, d], AF.Exp, ["abt", "gsc"], ["gtmpA"], bias=gsc[:, 2 + d:3 + d], scale=1.0)
        act(gtmp[:, d, :], gtmp[:, d, :], AF.Ln, ["gtmpA"], ["gtmpB"], bias=1.0, scale=1.0)
        ts("dve", gg[:, d, :], gtmp[:, d, :], nea[:, d:d + 1], None, ALU.mult, None, ["gtmpB", "nea"], ["gg"])
        act(bet[:, d, :], abt[:, :, 2 + d], AF.Sigmoid, ["abt"], ["bet"])
        ts("dve", nbet[:, d, :], bet[:, d, :], -1.0, None, ALU.mult, None, ["bet"], ["nbet"])
        pg_ = bank(0)
        mm(pg_[:, 0:NSC], tri[d], gg[:, d, :], ["cst", "gg"], ["B0"])
        mm(pg_[:, 128:128 + NSC], blkm, gg[:, d, :], ["cst", "gg"], ["B0"])
        mm(pg_[:, 256:256 + NSC], H0, gg[:, d, :], ["cst", "gg"], ["B0"])
        mm(pg_[:, 384:384 + NSC], H1, gg[:, d, :], ["cst", "gg"], ["B0"])
        cp("dve", Gc[:, d, :], pg_[:, 0:NSC], ["B0"], ["Gc"])
        act(eG[:, d, :], pg_[:, 0:NSC], AF.Exp, ["B0"], ["eG"])
        tt("dve", gtmp[:, d, :], pg_[:, 128:128 + NSC], Gc[:, d, :], ALU.subtract, ["B0", "Gc", "gtmpB"], ["gtmpC"])
        act(eGl[:, d, :], gtmp[:, d, :], AF.Exp, ["gtmpC"], ["eGl"])
        act(egl[:, d, 0, :], pg_[:, 256:256 + NSC], AF.Exp, ["B0"], ["egl"])
        act(egl[:, d, 1, :], pg_[:, 384:384 + NSC], AF.Exp, ["B0"], ["egl"])

    dbg("gg", gg[:, :, :], [128, 2, NSC], "gg")
    dbg("Gc", Gc[:, :, :], [128, 2, NSC], "Gc")

    def pcbuf(name, dt, n=128):
        return [A.alloc(f"{name}{d}", [128, n], dt) for d in range(2)]
    gram = pcbuf("gram", F32, 256)
    Rw = pcbuf("Rw", BF16)
    kdp = [[A.alloc(f"kd{d}_{q}", [128, 128], BF16) for d in range(2)] for q in range(2)]
    qd = pcbuf("qd", BF16)
    qdTp = [[A.alloc(f"qdT{d}_{q}", [128, 128], BF16) for d in range(2)] for q in range(2)]
    dg = pcbuf("dg", F32)
    tE = pcbuf("tE", F32)
    Es = pcbuf("Es", F32)
    Ei = pcbuf("Ei", F32)
    aqkTp = [[A.alloc(f"aqkT{d}_{q}", [128, 128], BF16) for d in range(2)] for q in range(2)]
    Xb = [pcbuf("Xa", BF16), pcbuf("Xb", BF16)]
    Yb = [pcbuf("Ya", BF16), pcbuf("Yb", BF16)]
    Rb = [pcbuf("Ra", BF16), pcbuf("Rb", BF16)]
    ATb = pcbuf("AT", BF16)
    WTp = [[A.alloc(f"WT{d}_{q}", [128, 128], BF16) for d in range(2)] for q in range(2)]
    UBbp = [[A.alloc(f"UBb{d}_{q}", [128, 128], F32) for d in range(2)] for q in range(2)]
    S32 = pcbuf("S32", F32)
    Sbf = pcbuf("Sbf", BF16)
    ub = pcbuf("ub", BF16)
    for d in range(2):
        mset("pool", S32[d][:, :], 0.0, [f"S32{d}"])
        mset("pool", Sbf[d][:, :], 0.0, [f"Sbf{d}"])

    slot = [0]

    def pslot():
        s = slot[0] % 4
        slot[0] += 1
        return bank(s)[:, 0:128], f"B{s}"

    def pslot_bf():
        ap_, k_ = pslot()
        return ap_[:, 0:64].bitcast(BF16), k_

    def precompute(sc, d, par):
        WT, UBb, kd, qdT, aqkT = WTp[par], UBbp[par], kdp[par], qdTp[par], aqkTp[par]
        pq = str(par)
        lat = sc >= 2
        c0 = sc * 128
        sd = str(d)
        p1, k1 = pslot()
        mm(p1, KnT[:, c0:c0 + 128], KnT[:, c0:c0 + 128], ["KnT"], [k1])
        yield
        cp("act", gram[d][:, 0:128], p1, [k1], ["gramA" + sd])
        yield
        if lat:
            p2, k2 = pslot()
            mm(p2, KnT[:, c0:c0 + 128], QnT[:, c0:c0 + 128], ["KnT", "QnT"], [k2])
            yield
            cp("act", gram[d][:, 128:256], p2, [k2], ["gramB" + sd])
            yield
        p3, k3 = pslot_bf()
        tr(p3, KnT[:, c0:c0 + 128], identb[:, :], ["KnT", "identb"], [k3])
        yield
        act(Rw[d][:, :], p3, AF.Identity, [k3, "eG"], ["Rw" + sd], scale=eG[:, d, sc:sc + 1])
        yield
        ts("dve", kd[d][:, :], p3, eGl[:, d, sc:sc + 1], None, ALU.mult, None, [k3, "eGl"], ["kd" + sd + pq])
        yield
        if lat:
            p4, k4 = pslot_bf()
            tr(p4, QnT[:, c0:c0 + 128], identb[:, :], ["QnT", "identb"], [k4])
            yield
            act(qd[d][:, :], p4, AF.Identity, [k4, "eG"], ["qd" + sd], scale=eG[:, d, sc:sc + 1])
            yield
            p5, k5 = pslot_bf()
            tr(p5, qd[d][:, :], identb[:, :], ["qd" + sd, "identb"], [k5])
            yield
            cp("act", qdT[d][:, :], p5, [k5], ["qdT" + sd + pq])
            yield
        ts("pool", dg[d][:, :], ident, Gc[:, d, sc:sc + 1], 1.0, ALU.mult, ALU.mult, ["cst", "Gc"], ["dg" + sd])
        yield
        p6, k6 = pslot()
        mm(p6, ones, dg[d][:, :], ["cst", "dg" + sd], [k6])
        yield
        ts("dve", tE[d][:, :], p6, Gc[:, d, sc:sc + 1], 0.0, ALU.subtract, ALU.min, [k6, "Gc"], ["tEA" + sd])
        yield
        act(tE[d][:, :], tE[d][:, :], AF.Exp, ["tEA" + sd], ["tE" + sd])
        yield
        tt("pool", Es[d][:, :], tE[d][:, :], mS[d], ALU.mult, ["tE" + sd, "cst"], ["Es" + sd])
        yield
        X0 = Xb[0][d]
        stt(X0[:, :], gram[d][:, 0:128], nbet[:, d, sc:sc + 1], Es[d][:, :], ALU.mult, ALU.mult, ["gramA" + sd, "nbet", "Es" + sd], ["X0" + sd])
        yield
        if lat:
            tt("pool", Ei[d][:, :], tE[d][:, :], mI[d], ALU.mult, ["tE" + sd, "cst"], ["Ei" + sd])
            yield
            tt("dve", aqkT[d][:, :], gram[d][:, 128:256], Ei[d][:, :], ALU.mult, ["gramB" + sd, "Ei" + sd], ["aqkT" + sd + pq])
            yield
        p7, k7 = pslot_bf()
        tr(p7, X0[:, :], identb[:, :], ["X0" + sd, "identb"], [k7])
        yield
        Y0 = Yb[0][d]
        cp("act", Y0[:, :], p7, [k7], ["Y0" + sd])
        yield
        R0 = Rb[0][d]
        tt("pool", R0[:, :], X0[:, :], ident, ALU.add, ["X0" + sd, "cst"], ["R0" + sd])
        yield
        for lv in range(1, 6):
            a_, b_ = (lv - 1) % 2, lv % 2
            Xp, Yp, Rp = Xb[a_][d], Yb[a_][d], Rb[a_][d]
            Xn, Yn, Rn = Xb[b_][d], Yb[b_][d], Rb[b_][d]
            kXp, kYp, kRp = f"X{a_}{sd}", f"Y{a_}{sd}", f"R{a_}{sd}"
            kXn, kYn, kRn = f"X{b_}{sd}", f"Y{b_}{sd}", f"R{b_}{sd}"
            py, ky = pslot()
            mm(py, Xp[:, :], Yp[:, :], [kXp, kYp], [ky])
            yield
            if lv <= 4:
                px, kx_ = pslot()
                mm(px, Yp[:, :], Xp[:, :], [kXp, kYp], [kx_])
                yield
            cp("act", Yn[:, :], py, [ky], [kYn])
            yield
            if lv <= 4:
                cp("dve", Xn[:, :], px, [kx_], [kXn])
                yield
            pr_, kr_ = pslot()
            mm(pr_, Yn[:, :], Rp[:, :], [kYn, kRp], [kr_])
            yield
            if lv < 5:
                tt("dve", Rn[:, :], pr_, Rp[:, :], ALU.add, [kr_, kRp], [kRn])
                yield
            else:
                tt("dve", ATb[d][:, :], pr_, Rp[:, :], ALU.add, [kr_, kRp], ["AT" + sd])
                yield
        p8, k8 = pslot()
        mm(p8, Rw[d][:, :], ATb[d][:, :], ["Rw" + sd, "AT" + sd], [k8])
        yield
        cp("act", WT[d][:, :], p8, [k8], ["WT" + sd + pq])
        yield
        p9, k9 = pslot()
        mm(p9, ATb[d][:, :], Vg[:, sc, :], ["AT" + sd, "Vg"], [k9])
        yield
        act(UBb[d][:, :], p9, AF.Identity, [k9, "bet"], ["UBb" + sd + pq], scale=bet[:, d, sc:sc + 1])
        yield

    def step(sc, hh, d, par):
        WT, UBb, kd, qdT, aqkT = WTp[par], UBbp[par], kdp[par], qdTp[par], aqkTp[par]
        pq = str(par)
        lat = sc >= 2
        sd = str(d)
        r0, r1 = hh * 64, hh * 64 + 64
        bA = bank(4 + 2 * d)
        bO = bank(5 + 2 * d)
        pws = bA[r0:r1, 0:128]
        mm(pws, WT[d][:, r0:r1], Sbf[d][:, :], ["WT" + sd + pq, "Sbf" + sd], [f"B{4 + 2 * d}"])
        yield
        stt(ub[d][r0:r1, :], pws, nbet[r0:r1, d, sc:sc + 1], UBb[d][r0:r1, :], ALU.mult, ALU.add, [f"B{4 + 2 * d}", "nbet", "UBb" + sd + pq], ["ub" + sd])
        yield
        if lat:
            po = bO[r0:r1, 0:128]
            mm(po, qdT[d][:, r0:r1], Sbf[d][:, :], ["qdT" + sd + pq, "Sbf" + sd], [f"B{5 + 2 * d}"], start=True, stop=False)
            yield
            mm(po, aqkT[d][r0:r1, r0:r1], ub[d][r0:r1, :], ["aqkT" + sd + pq, "ub" + sd], [f"B{5 + 2 * d}"], start=False, stop=True)
            yield
            tt("dve", og[r0:r1, sc - 2, :], po, og[r0:r1, sc - 2, :], ALU.add, [f"B{5 + 2 * d}", f"og{sc - 2}"], [f"og{sc - 2}"])
            yield
        pS = bA[:, 128:256]
        mm(pS, kd[d][r0:r1, :], ub[d][r0:r1, :], ["kd" + sd + pq, "ub" + sd], [f"B{4 + 2 * d}"])
        yield
        stt(S32[d][:, :], S32[d][:, :], egl[:, d, hh, sc:sc + 1], pS, ALU.mult, ALU.add, ["S32" + sd, "egl", f"B{4 + 2 * d}"], ["S32" + sd])
        yield
        cp("act", Sbf[d][:, :], S32[d][:, :], ["S32" + sd], ["Sbf" + sd])
        yield

    fwd = list(range(NSC))
    bwd = [1, 0] + list(range(NSC - 1, 1, -1))
    def seq(*gs):
        for g in gs:
            yield from g

    def interleave(*gens):
        gens = list(gens)
        while gens:
            for g in list(gens):
                try:
                    next(g)
                except StopIteration:
                    gens.remove(g)

    interleave(precompute(fwd[0], 0, 0), precompute(bwd[0], 1, 0))
    for i in range(NSC):
        par = i % 2
        gl = [seq(step(fwd[i], 0, 0, par), step(fwd[i], 1, 0, par)), seq(step(bwd[i], 1, 1, par), step(bwd[i], 0, 1, par))]
        if i + 1 < NSC:
            gl += [precompute(fwd[i + 1], 0, 1 - par), precompute(bwd[i + 1], 1, 1 - par)]
        interleave(*gl)

    P.op("pool", lambda e: e.memset(small[:, 500:501], 0.0), reads=[f"og{i}" for i in range(64)], writes=["og_all"])
    dbg("og", og[:, :, :], [128, 64, 128], "og_all")

    ogb = A.alloc("ogb", [128, 64, 128], BF16)
    ogs = A.alloc("ogs", [128, 64], F32)
    ogr = A.alloc("ogr", [128, 64], F32)
    ogt = A.alloc("ogt", [128, 128], F32)
    sqj2 = A.alloc("sqj2", [128, 128], BF16)
    for i in range(64):
        act(sqj2[:, :], og[:, i, :], AF.Square, ["og_all"], ["sqj2", "ogs"], accum=ogs[:, i:i + 1])
    act(ogr[:, :], ogs[:, :], AF.Sqrt, ["ogs"], ["ogrA"], bias=EPS, scale=1.0 / 128)
    recip(ogr[:, :], ogr[:, :], ["ogrA"], ["ogr"])
    for i in range(64):
        stt(ogt[:, :], og[:, i, :], ogr[:, i:i + 1], gng[:, :], ALU.mult, ALU.mult, ["og_all", "ogr", "gng"], ["ogt"])
        tt("dve", ogb[:, i, :], ogt[:, :], gate_s[:, i, :], ALU.mult, ["ogt", "gate_s"], ["ogb"])
    dbg("ogb", ogb[:, :, :], [128, 64, 128], "ogb")
    if stage == 2:
        return finish()

    P.barrier()
    after_gdn = A.off
    A.off = persist_mark
    KT0 = A.alloc("KT0", [128, LC + L], BF16)
    KT1 = A.alloc("KT1", [128, LC + L], BF16)
    QT = A.alloc("QT", [128, L], BF16)
    Vv = A.alloc("Vv", [128, NSC, 130], BF16)
    assert A.off <= gdn_mark, A.off
    lim1 = A.off
    A.off = after_gdn
    wout = A.alloc("wout", [128, 2, D], BF16)
    pbuf = [A.alloc(f"pb{i}", [128, 512], BF16) for i in range(4)]
    oas = [A.alloc(f"oa{i}", [128, 128], F32) for i in range(2)]
    obs = [A.alloc(f"ob{i}", [128, 128], F32) for i in range(2)]
    omx = A.alloc("omx", [128, 128], BF16)
    omT = [A.alloc(f"omT{i}", [128, 128], BF16) for i in range(2)]
    rsst = [A.alloc(f"rsst{i}", [128, D], F32) for i in range(2)]
    att = A.alloc("att", [128, 16], F32)
    sqj3 = A.alloc("sqj3", [128, 128], BF16)
    for kc in range(4):
        dma("sp", f"ld{kc % 2}", KT0[0:64, kc * 2112:(kc + 1) * 2112], kt_s[0:64, kc * 2112:(kc + 1) * 2112], ["ktqt_s"], ["KT0a"])
        dma("sp", f"ld{2 + kc % 2}", KT1[64:128, kc * 2112:(kc + 1) * 2112], kt_s[64:128, kc * 2112:(kc + 1) * 2112], ["ktqt_s"], ["KT1a"])
        dma("sp", f"ld{4 + kc % 2}", QT[:, kc * 2048:(kc + 1) * 2048], qt_s[:, kc * 2048:(kc + 1) * 2048], ["ktqt_s"], ["QT"])
    mset("pool", KT0[64:128, :], 0.0, ["KT0b"])
    mset("pool", KT1[0:64, :], 0.0, ["KT1b"])
    for kc in range(6):
        dma("sp", f"ld{6 + kc % 2}", Vv[:, kc * 11:(kc + 1) * 11, :], v_s[kc * 11:(kc + 1) * 11, :, :].rearrange("s p n -> p s n"), ["v_s"], ["Vv"])
    for f_ in range(2):
        dma("pool", f"wo{f_}", wout[:, f_, :], wout_d[f_, :, :], (), ["wout"])

    NKT = NSC
    pcnt = [0]
    if stage == 2.91:
        dbg("KT", KT0[:, :], [128, LC + L], "KT0a")
        dbg("Vv", Vv[:, :, :], [128, NSC, 130], "Vv")
        return finish()
    qorder = [(sblk, rr, qq) for sblk in range(4) for rr in range(4) for qq in range(2)]
    SB = [0, 1, 7]

    def q0_of(ent):
        sblk_, rr_, qq_ = ent
        return rr_ * 2048 + sblk_ * 512 + qq_ * 256

    def s_mm(q0, kt_):
        sb_ = SB[kt_ % 3]
        pS_ = bank(sb_)
        mm(pS_[:, 0:256], KT0[:, kt_ * 128:(kt_ + 1) * 128], QT[:, q0:q0 + 256], ["KT0a", "KT0b", "QT"], [f"B{sb_}"])
        mm(pS_[:, 256:512], KT1[:, kt_ * 128:(kt_ + 1) * 128], QT[:, q0:q0 + 256], ["KT1a", "KT1b", "QT"], [f"B{sb_}"])

    qlist = qorder[:1] if stage in (2.92, 2.93) else qorder
    s_mm(q0_of(qlist[0]), 0)
    s_mm(q0_of(qlist[0]), 1)
    for qi_, (sblk, rr, qq) in enumerate(qlist):
        q0 = q0_of((sblk, rr, qq))
        qt_ = q0 // 256
        for kt_ in range(NKT):
            sb_ = SB[kt_ % 3]
            pi = pcnt[0] % 4
            pcnt[0] += 1
            if kt_ + 2 < NKT:
                s_mm(q0, kt_ + 2)
            act(pbuf[pi][:, :], bank(sb_), AF.Exp, [f"B{sb_}"], [f"pb{pi}"], scale=0.125)
            for sub in range(2):
                for mp in range(2):
                    acc = bank(2 + sub * 2 + mp)[:, 0:129]
                    mm(acc, pbuf[pi][:, mp * 256 + sub * 128:mp * 256 + (sub + 1) * 128], Vv[:, kt_, 0:129], [f"pb{pi}", "Vv"],
                       [f"B{2 + sub * 2 + mp}"], start=(kt_ == 0), stop=(kt_ == NKT - 1))
        if qi_ + 1 < len(qlist):
            s_mm(q0_of(qlist[qi_ + 1]), 0)
            s_mm(q0_of(qlist[qi_ + 1]), 1)
        for sub in range(0 if stage == 2.93 else 2):
            a0 = bank(2 + sub * 2)
            a1_ = bank(3 + sub * 2)
            ss_ = str(sub)
            atc = att[:, 8 * sub:8 * sub + 8]
            recip(atc[:, 0:1], a0[:, 128:129], [f"B{2 + sub * 2}"], ["att0" + ss_])
            recip(atc[:, 1:2], a1_[:, 128:129], [f"B{3 + sub * 2}"], ["att1" + ss_])
            tt("dve", atc[:, 2:3], atc[:, 1:2], nlam, ALU.mult, ["att1" + ss_, "nlam"], ["att2" + ss_])
            ts("dve", oas[sub][:, :], a0[:, 0:128], atc[:, 0:1], None, ALU.mult, None, [f"B{2 + sub * 2}", "att0" + ss_], ["oa" + ss_])
            stt(obs[sub][:, :], a1_[:, 0:128], atc[:, 2:3], oas[sub][:, :], ALU.mult, ALU.add, [f"B{3 + sub * 2}", "att2" + ss_, "oa" + ss_], ["ob" + ss_])
        for sub in range(0 if stage == 2.93 else 2):
            ti = qt_ * 2 + sub
            ss_ = str(sub)
            atc = att[:, 8 * sub:8 * sub + 8]
            oa, ob = oas[sub], obs[sub]
            act(sqj3[:, :], ob[:, :], AF.Square, ["ob" + ss_], ["sqj3", "att3" + ss_], accum=atc[:, 3:4])
            act(atc[:, 4:5], atc[:, 3:4], AF.Sqrt, ["att3" + ss_], ["att4" + ss_], bias=EPS, scale=1.0 / 128)
            recip(atc[:, 5:6], atc[:, 4:5], ["att4" + ss_], ["att5" + ss_])
            ts("dve", oa[:, :], ob[:, :], atc[:, 5:6], 1.0 - LAM_INIT, ALU.mult, ALU.mult, ["ob" + ss_, "att5" + ss_], ["oa" + ss_])
            tt("dve", omx[:, :], oa[:, :], sgg[:, :], ALU.mult, ["oa" + ss_, "sgg"], ["omx"])
            pt_ = bank(6)
            ptb = pt_[:, 0:128].bitcast(BF16)
            tr(ptb[:, 0:128], omx[:, :], identb[:, :], ["omx", "identb"], ["B6"])
            tr(ptb[:, 128:256], ogb[:, ti, :], identb[:, :], ["ogb", "identb"], ["B6"])
            cp("act", omT[0][:, :], ptb[:, 0:128], ["B6"], ["omT0"])
            cp("act", omT[1][:, :], ptb[:, 128:256], ["B6"], ["omT1"])
            rb = rsst[ti % 2]
            for nh in range(2):
                po_ = bank(6)
                mm(po_, omT[0][:, :], wout[:, 0, nh * 512:(nh + 1) * 512], ["omT0", "wout"], ["B6"], start=True, stop=False)
                mm(po_, omT[1][:, :], wout[:, 1, nh * 512:(nh + 1) * 512], ["omT1", "wout"], ["B6"], start=False, stop=True)
                if nh == 0:
                    cp("act", rb[:, 0:512], po_, ["B6"], [f"rsst{ti % 2}"])
                else:
                    cp("dve", rb[:, 512:1024], po_, ["B6"], [f"rsst{ti % 2}"])
            rrow = rr * 512 + qq * 256 + sub * 128
            dma("pool", f"rs{ti % 2}", rs_in[sblk].ap()[rrow:rrow + 128, :], rb[:, :], [f"rsst{ti % 2}"], [f"rs_in{sblk}"])
        if rr == 3 and qq == 1 and stage >= 3:
            P.dma("pool", f"ccrs{sblk}", lambda e, sblk=sblk: e.collective_compute(
                "ReduceScatter", ALU.add, replica_groups=[[0, 1, 2, 3], [4, 5, 6, 7]],
                ins=[rs_in[sblk].ap().opt()], outs=[rs_out[sblk].ap().opt()]), reads=[f"rs_in{sblk}"], writes=[f"rs_out{sblk}"], inc=1)

    if 2.9 <= stage < 3:
        return finish()
    if stage == 3:
        return finish()
    P.barrier()
    A.off = persist_mark
    h2T = A.alloc("h2T", [128, 8, 2048], BF16)
    aff = A.alloc("aff", [128, 16, 16], F32)
    gw = A.alloc("gw", [128, 16, 16], F32)
    gt1bc = A.alloc("gt1bc", [128, D], F32)
    gt2bc = A.alloc("gt2bc", [128, D], F32)
    fgbc = A.alloc("fgbc", [128, D], F32)
    wr = A.alloc("wr", [128, 8, 16], BF16)
    sh2T = col(8)
    sc2T = col(8)
    a2 = col(8)
    n2 = A.alloc("n2", [128, 16], F32)
    ex = A.alloc("ex", [128, 16], F32)
    bs = A.alloc("bs", [64, 8], F32)
    taud = A.alloc("taud", [16, 16], F32)
    taubc = A.alloc("taubc", [128, 16], F32)
    xot = [A.alloc(f"xot{i}", [128, D], F32) for i in range(2)]
    rst = [A.alloc(f"rst{i}", [128, D], F32) for i in range(2)]
    xnw = [A.alloc(f"xnw{i}", [128, D], F32) for i in range(2)]
    sqj4 = A.alloc("sqj4", [128, D], BF16)
    m3_mark = A.off
    modrow = A.alloc("modrow", [1, 4096], F32)
    bm2 = A.alloc("bm2", [1, 4096], F32)
    p3_mark = A.off
    wm2 = A.alloc("wm2", [128, 8, 4096], BF16)
    for kc in range(8):
        dma("pool", f"wm{kc}", wm2[:, kc, :], wmod[kc * 128:(kc + 1) * 128, 2048:6144], (), [f"wm2_{kc}"])
    dma("sp", "sm0", bm2[:, :], bm2_d, (), ["bm2"])
    dma("sp", "sm1", fgbc[:, :], fgbc_d, (), ["fgbc"])
    dma("pool", "wab", wr[:, :, :].rearrange("p a b -> p (a b)"), wr_d, (), ["wr"])
    for cb in range(8):
        pm_ = bank(cb % 2)[0:1, :]
        for kc in range(8):
            mm(pm_, scb[:, kc, 0:1], wm2[:, kc, cb * 512:(cb + 1) * 512], ["scb", f"wm2_{kc}"], [f"B{cb % 2}"], start=(kc == 0), stop=(kc == 7))
        tt("dve", modrow[0:1, cb * 512:(cb + 1) * 512], pm_, bm2[0:1, cb * 512:(cb + 1) * 512], ALU.add, [f"B{cb % 2}", "bm2"], ["modrow"])
    for nh in range(2):
        pb_ = bank(2)
        mm(pb_, ones[0:1, :], modrow[0:1, nh * 512:(nh + 1) * 512], ["cst", "modrow"], ["B2"])
        cp("act", gt1bc[:, nh * 512:(nh + 1) * 512], pb_, ["B2"], ["gt1bc"])
        mm(pb_, ones[0:1, :], modrow[0:1, 3072 + nh * 512:3072 + (nh + 1) * 512], ["cst", "modrow"], ["B2"])
        cp("act", gt2bc[:, nh * 512:(nh + 1) * 512], pb_, ["B2"], ["gt2bc"])
    pc_ = bank(3)
    for kc in range(8):
        mm(pc_[:, kc:kc + 1], modrow[0:1, 1024 + kc * 128:1024 + (kc + 1) * 128], ones[0:1, 0:1], ["modrow", "cst"], ["B3"])
        mm(pc_[:, 8 + kc:9 + kc], modrow[0:1, 2048 + kc * 128:2048 + (kc + 1) * 128], ones[0:1, 0:1], ["modrow", "cst"], ["B3"])
    cp("dve", sh2T, pc_[:, 0:8], ["B3"], ["sh2T"])
    cp("dve", sc2T, pc_[:, 8:16], ["B3"], ["sc2T"])
    stt(a2, sc2T, 1.0, g2T, ALU.add, ALU.mult, ["sc2T", "g2T"], ["a2"])
    P.barrier()
    A.off = p3_mark
    xn2 = [A.alloc(f"xn2_{i}", [128, D], F32) for i in range(2)]
    exs = [ex, A.alloc("exb", [128, 16], F32)]
    affT = A.alloc("affT", [16, 2048], F32)
    affall = A.alloc("affall", [64, 2048], F32)
    cmpj = A.alloc("cmpj", [64, 2048], BF16)
    def p2_front(i):
        b_ = i % 2
        c0 = b_ * 3
        dma("sp", f"xo{b_}", xot[b_][:, :], xo[i * 128:(i + 1) * 128, :], (), [f"xot{b_}"])
        yield
        dma("sp", f"rsl{b_}", rst[b_][:, :], rs_out[i // 4].ap()[(i % 4) * 128:(i % 4 + 1) * 128, :], [f"rs_out{i // 4}"], [f"rst{b_}"])
        yield
        tt("pool", rst[b_][:, :], rst[b_][:, :], gt1bc[:, :], ALU.mult, [f"rst{b_}", "gt1bc"], [f"rst{b_}"])
        yield
        tt("dve", xnw[b_][:, :], rst[b_][:, :], xot[b_][:, :], ALU.add, [f"rst{b_}", f"xot{b_}"], [f"xnw{b_}"])
        yield
        dma("pool", f"xns{b_}", xnew_s[i * 128:(i + 1) * 128, :], xnw[b_][:, :], [f"xnw{b_}"], ["xnew_s"])
        yield
        act(sqj4[:, :], xnw[b_][:, :], AF.Square, [f"xnw{b_}"], ["sqj4", f"n2a{b_}"], accum=n2[:, c0:c0 + 1])
        yield
        act(n2[:, c0 + 1:c0 + 2], n2[:, c0:c0 + 1], AF.Sqrt, [f"n2a{b_}"], [f"n2b{b_}"], bias=EPS, scale=1.0 / D)
        yield
        recip(n2[:, c0 + 2:c0 + 3], n2[:, c0 + 1:c0 + 2], [f"n2b{b_}"], [f"n2c{b_}"])
        yield
        ts("pool", xn2[b_][:, :], xnw[b_][:, :], n2[:, c0 + 2:c0 + 3], 1.0, ALU.mult, ALU.mult, [f"xnw{b_}", f"n2c{b_}"], [f"xn2{b_}"])
        yield

    def p2_back(i):
        b_ = i % 2
        c0 = 6 + b_ * 3
        for kc in range(8):
            pb2 = kc % 2
            pT = bank(pb2)[:, 0:128]
            tr(pT, xn2[b_][:, kc * 128:(kc + 1) * 128], ident, [f"xn2{b_}", "cst"], [f"B{pb2}"])
            yield
            if kc % 2 == 0:
                act(h2T[:, kc, i * 128:(i + 1) * 128], pT, AF.Identity, [f"B{pb2}", "a2", "sh2T"], [f"h2T_{kc}"], bias=sh2T[:, kc:kc + 1], scale=a2[:, kc:kc + 1])
            else:
                ts("dve", h2T[:, kc, i * 128:(i + 1) * 128], pT, a2[:, kc:kc + 1], sh2T[:, kc:kc + 1], ALU.mult, ALU.add, [f"B{pb2}", "a2", "sh2T"], [f"h2T_{kc}"])
            yield
        pl = bank(2)[:, 0:16]
        for kc in range(8):
            mm(pl, h2T[:, kc, i * 128:(i + 1) * 128], wr[:, kc, :], [f"h2T_{kc}", "wr"], ["B2"], start=(kc == 0), stop=(kc == 7))
        yield
        P.op("dve", lambda e, pl=pl, c0=c0: e.tensor_reduce(out=n2[:, c0:c0 + 1], in_=pl, axis=AX.X, op=ALU.max, negate=True), reads=["B2"], writes=[f"n2d{b_}"])
        yield
        act(exs[b_][:, :], pl, AF.Exp, ["B2", f"n2d{b_}"], [f"ex{b_}", f"n2e{b_}"], bias=n2[:, c0:c0 + 1], scale=1.0, accum=n2[:, c0 + 1:c0 + 2])
        yield
        recip(n2[:, c0 + 2:c0 + 3], n2[:, c0 + 1:c0 + 2], [f"n2e{b_}"], [f"n2f{b_}"])
        yield
        ts("dve", aff[:, i, :], exs[b_][:, :], n2[:, c0 + 2:c0 + 3], None, ALU.mult, None, [f"ex{b_}", f"n2f{b_}"], ["aff"])
        yield
        pa_ = bank(3)[0:16, 0:128]
        tr(pa_, aff[:, i, :], ident, ["aff", "cst"], ["B3"])
        yield
        cp("act", affT[:, i * 128:(i + 1) * 128], pa_, ["B3"], ["affT"])
        yield

    interleave(p2_front(0))
    for i in range(16):
        gl = [p2_back(i)]
        if i + 1 < 16:
            gl.append(p2_front(i + 1))
        interleave(*gl)
    dma("pool", "agi", ag_in.ap()[:, :], affT[:, :], ["affT"], ["ag_in"])
    P.dma("pool", "ccag", lambda e: e.collective_compute(
        "AllGather", ALU.bypass, replica_groups=[[0, 1, 2, 3], [4, 5, 6, 7]],
        ins=[ag_in.ap().opt()], outs=[ag_out.ap().opt()]), reads=["ag_in"], writes=["ag_out"], inc=1)
    dma("sp", "ago", affall[:, :], ag_out.ap()[:, :], ["ag_out"], ["affall"])
    mset("pool", bs[:, 0:1], 0.0, ["lo"])
    G64 = C(12)
    for it in range(24):
        hw = 2.0 ** -(it + 1)
        ts("dve", bs[:, 1:2], bs[:, 0:1], hw, None, ALU.add, None, ["lo"], ["mid"])
        ts("dve", cmpj[:, :], affall[:, :], bs[:, 1:2], 0.0, ALU.is_ge, ALU.add, ["affall", "mid"], ["cmpj", "cnt"], accum=bs[:, 2:3])
        pc2 = bank(4)[0:64, 0:1]
        mm(pc2, G64[0:64, 0:64], bs[:, 2:3], ["cst", "cnt"], ["B4"])
        ts("dve", bs[:, 3:4], pc2, 1023.5, hw, ALU.is_ge, ALU.mult, ["B4"], ["gd"])
        tt("dve", bs[:, 0:1], bs[:, 0:1], bs[:, 3:4], ALU.add, ["lo", "gd"], ["lo"])
    ts("dve", taud[:, :], ident[0:16, 0:16], bs[0:16, 0:1], None, ALU.mult, None, ["cst", "lo"], ["taud"])
    ptau = bank(5)[:, 0:16]
    mm(ptau, ones[0:16, :], taud[:, :], ["cst", "taud"], ["B5"])
    cp("dve", taubc[:, :], ptau, ["B5"], ["taubc"])
    for i in range(16):
        tt("dve", gw[:, i, :], aff[:, i, :], taubc[:, :], ALU.is_ge, ["aff", "taubc"], ["gwA"])
        tt("dve", gw[:, i, :], gw[:, i, :], aff[:, i, :], ALU.mult, ["gwA", "aff"], ["gw"])
    dbg("xnew", xnew_s[:, :], [2048, D], "xnew_s")
    dbg("taubc", taubc[:, :], [128, 16], "taubc")
    dbg("aff", aff[:, :, :], [128, 16, 16], "aff")
    dbg("gw", gw[:, :, :], [128, 16, 16], "gw")

    if stage == 4:
        return finish()
    P.barrier()
    A.off = m3_mark
    yacc = A.alloc("yacc", [128, 16, D], F32)
    mset("pool", yacc[:, :, :], 0.0, [f"yacc{a}_{b}" for a in range(16) for b in range(2)])
    wgs = [A.alloc(f"wgs{i}", [128, 8, 512], BF16) for i in range(2)]
    wus = [A.alloc(f"wus{i}", [128, 8, 512], BF16) for i in range(2)]
    wds = [A.alloc(f"wds{i}", [128, 4, D], BF16) for i in range(2)]
    sg = [A.alloc(f"sg{i}", [128, 512], F32) for i in range(1)]
    hid = [A.alloc(f"hid{i}", [128, 512], BF16) for i in range(8)]
    gcnt = [0]

    def gu(e_, hf, TB, ws):
        for fc in range(4):
            gp = gcnt[0] % 2
            gcnt[0] += 1
            bG, bU = gp * 2, gp * 2 + 1
            pG, pU = bank(bG), bank(bU)
            for kc in range(8):
                mm(pG, wgs[ws][:, kc, fc * 128:(fc + 1) * 128], h2T[:, kc, TB * 512:(TB + 1) * 512], [f"wgs{ws}", f"h2T_{kc}"], [f"B{bG}"],
                   start=(kc == 0), stop=(kc == 7))
            for kc in range(8):
                mm(pU, wus[ws][:, kc, fc * 128:(fc + 1) * 128], h2T[:, kc, TB * 512:(TB + 1) * 512], [f"wus{ws}", f"h2T_{kc}"], [f"B{bU}"],
                   start=(kc == 0), stop=(kc == 7))
            act(sg[0][:, :], pG, AF.Silu, [f"B{bG}"], ["sg0"])
            hi = (TB % 2) * 4 + fc
            tt("dve", hid[hi][:, :], sg[0][:, :], pU, ALU.mult, ["sg0", f"B{bU}"], [f"hid{hi}"])

    def down(e_, hf, TB, ws):
        for half in range(2):
            for sub in range(2):
                for nh in range(2):
                    bY = 4 + sub * 2 + nh
                    py_ = bank(bY)
                    c0 = half * 256 + sub * 128
                    for fc in range(4):
                        hi = (TB % 2) * 4 + fc
                        mm(py_, hid[hi][:, c0:c0 + 128], wds[ws][:, fc, nh * 512:(nh + 1) * 512], [f"hid{hi}", f"wds{ws}"],
                           [f"B{bY}"], start=(fc == 0), stop=(fc == 3))
                    ti = TB * 4 + half * 2 + sub
                    stt(yacc[:, ti, nh * 512:(nh + 1) * 512], py_, gw[:, ti, e_:e_ + 1], yacc[:, ti, nh * 512:(nh + 1) * 512], ALU.mult, ALU.add,
                        [f"B{bY}", "gw", f"yacc{ti}_{nh}"], [f"yacc{ti}_{nh}"])

    prev = None
    for e_ in range(16):
        for hf in range(2):
            ws = (e_ * 2 + hf) % 2
            for kk in range(2):
                dma("pool", f"wg{ws}{kk}", wgs[ws][:, kk * 4:(kk + 1) * 4, :],
                    wg_d[e_, kk * 512:(kk + 1) * 512, hf * 512:(hf + 1) * 512].rearrange("(kc p) f -> p kc f", p=128), (), [f"wgs{ws}"])
                dma("pool", f"wu{ws}{kk}", wus[ws][:, kk * 4:(kk + 1) * 4, :],
                    wu_d[e_, kk * 512:(kk + 1) * 512, hf * 512:(hf + 1) * 512].rearrange("(kc p) f -> p kc f", p=128), (), [f"wus{ws}"])
                dma("pool", f"wd{ws}{kk}", wds[ws][:, kk * 2:(kk + 1) * 2, :],
                    wd_d[e_, hf * 512 + kk * 256:hf * 512 + (kk + 1) * 256, :].rearrange("(fc p) n -> p fc n", p=128), (), [f"wds{ws}"])
            for TB in range(4):
                gu(e_, hf, TB, ws)
                if prev is not None:
                    down(*prev)
                prev = (e_, hf, TB, ws)
    down(*prev)


    for i in range(16):
        b_ = i % 2
        dma("sp", f"xo{b_}", xot[b_][:, :], xnew_s[i * 128:(i + 1) * 128, :], ["xnew_s"], [f"xot{b_}"])
        tt("pool", rst[b_][:, :], yacc[:, i, :], gt2bc[:, :], ALU.mult, [f"yacc{i}_0", f"yacc{i}_1", "gt2bc"], [f"rst{b_}"])
        tt("dve", xnw[b_][:, :], rst[b_][:, :], xot[b_][:, :], ALU.add, [f"rst{b_}", f"xot{b_}"], [f"xnw{b_}"])
        act(sqj4[:, :], xnw[b_][:, :], AF.Square, [f"xnw{b_}"], ["sqj4", "n2a"], accum=n2[:, 0:1])
        act(n2[:, 1:2], n2[:, 0:1], AF.Sqrt, ["n2a"], ["n2b"], bias=EPS, scale=1.0 / D)
        recip(n2[:, 2:3], n2[:, 1:2], ["n2b"], ["n2c"])
        stt(rst[b_][:, :], xnw[b_][:, :], n2[:, 2:3], fgbc[:, :], ALU.mult, ALU.mult, [f"xnw{b_}", "n2c", "fgbc"], [f"rst{b_}"])
        dma("pool", f"out{b_}", out_d[i * 128:(i + 1) * 128, :], rst[b_][:, :], [f"rst{b_}"], [f"out{b_}"])
    return finish()


_CACHE = {}


def kernel(x, c, ctx, c_ctx, w_mod, b_mod, norm1_g, w_in, conv_w, a_log, dt_bias, gdn_norm_g,
           lam_q1, lam_k1, lam_q2, lam_k2, da_subln_g, w_out, norm2_g,
           w_router, w_gate, w_up, w_down, final_g):
    f32 = np.float32
    A_ = lambda a: np.ascontiguousarray(np.asarray(a, dtype=f32))
    x, c, ctx, c_ctx = A_(x), A_(c), A_(ctx), A_(c_ctx)
    w_mod, b_mod, w_in, conv_w = A_(w_mod)[0], A_(b_mod)[0], A_(w_in)[0], A_(conv_w)[0]
    a_log, dt_bias = A_(a_log)[0], A_(dt_bias)[0]
    w_out, w_router = A_(w_out)[0], A_(w_router)[0]
    w_gate, w_up, w_down = A_(w_gate)[0], A_(w_up)[0], A_(w_down)[0]
    norm1_g, norm2_g, final_g = A_(norm1_g)[0], A_(norm2_g)[0], A_(final_g)
    gdn_norm_g, da_subln_g = A_(gdn_norm_g)[0], A_(da_subln_g)[0]
    lamcat = np.concatenate([A_(lam_q1)[0], A_(lam_k1)[0], A_(lam_q2)[0], A_(lam_k2)[0]])

    if MAPS_ONLY[0]:
        nc = None
    else:
        key = (tuple(DEBUG), STAGE[0])
        if key not in _CACHE:
            _CACHE[key] = build(debug=key[0], stage=key[1])
        nc, dbg_outs = _CACHE[key]

    consts = make_consts()
    rope = make_rope()

    def colT(v, n):
        return np.ascontiguousarray(v.reshape(n, 128).T)

    in_maps = []
    for core in range(8):
        b, h = core // 4, core % 4
        j = h
        cT = np.zeros((128, 8, 2), f32)
        cT[:, :, 0] = colT(c[b], 8)
        cT[:, :, 1] = colT(c_ctx, 8)
        cols = np.concatenate([
            np.arange(h * 128, h * 128 + 128),
            512 + np.arange(h * 128, h * 128 + 128),
            1536 + np.arange(h * 128, h * 128 + 128),
            1536 + 512 + np.arange(h * 128, h * 128 + 128),
            1536 + 1024 + np.arange(h * 128, h * 128 + 128),
            1024 + np.arange(h * 128, h * 128 + 128),
            3072 + np.arange(h * 128, h * 128 + 128),
        ])
        abcols = 3584 + np.array([0 * 8 + 0 * 4 + h, 0 * 8 + 1 * 4 + h, 1 * 8 + 0 * 4 + h, 1 * 8 + 1 * 4 + h])
        cw = np.zeros((128, 3, 3), f32)
        for cc in range(3):
            for tap in range(3):
                cw[:, cc, tap] = conv_w[tap, cc * 512 + h * 128:cc * 512 + h * 128 + 128]
        gsc = np.tile(np.array([a_log[0, h], a_log[1, h], dt_bias[0, h], dt_bias[1, h]], f32)[None, :], (128, 1))
        m = {
            "xb": x[b], "ctxb": ctx[b], "xo": np.ascontiguousarray(x[b, j * 2048:(j + 1) * 2048]),
            "cT": cT.reshape(128, 16), "wmod": w_mod,
            "bm1T": colT(b_mod[0:2048], 16), "bm2": np.ascontiguousarray(b_mod[2048:].reshape(1, 4096)),
            "g1T": colT(norm1_g, 8), "g2T": colT(norm2_g, 8),
            "fgbc": np.ascontiguousarray(np.tile(final_g[None, :], (128, 1))),
            "wslab": np.ascontiguousarray(w_in[:, cols]), "wab": np.ascontiguousarray(w_in[:, abcols]),
            "cwT": cw.reshape(128, 9), "gsc": np.ascontiguousarray(gsc),
            "gng": np.ascontiguousarray(np.tile(gdn_norm_g[None, :], (128, 1))),
            "sgg": np.ascontiguousarray(np.tile(da_subln_g[None, :], (128, 1))),
            "lamv": np.ascontiguousarray(np.tile(lamcat[None, :], (128, 1))),
            "wout": np.ascontiguousarray(np.stack([w_out[h * 128:(h + 1) * 128], w_out[512 + h * 128:512 + (h + 1) * 128]], 0)),
            "rope": rope, "consts": consts,
            "wr": np.ascontiguousarray(w_router.reshape(8, 128, 16).transpose(1, 0, 2).reshape(128, 128)),
            "wg": w_gate if STAGE[0] > 4 else w_gate[0:1, 0:8, 0:8].copy(),
            "wu": w_up if STAGE[0] > 4 else w_up[0:1, 0:8, 0:8].copy(),
            "wd": w_down if STAGE[0] > 4 else w_down[0:1, 0:8, 0:8].copy(),
        }
        in_maps.append(m)
    if MAPS_ONLY[0]:
        return in_maps
    res = run_bass_kernel_spmd(nc, in_maps, core_ids=list(range(8)), **RUN_KW)
    out = np.zeros((2, L, D), f32)
    LAST["res"] = res
    for core in range(8):
        b, j = core // 4, core % 4
        out[b, j * 2048:(j + 1) * 2048] = res.results[core]["out"]
    return out
```

```python
import math
import numpy as np
import concourse.bass as bass
import concourse.mybir as mybir
from concourse.bass_utils import run_bass_kernel_spmd

F32 = mybir.dt.float32
BF16 = mybir.dt.bfloat16
AF = mybir.ActivationFunctionType
ALU = mybir.AluOpType
AX = mybir.AxisListType

ENGS = ("pe", "act", "dve", "pool", "sp")
EPOCH = 30000
EPS = 1e-6
L = 8192
LC = 256
D = 1024
NSC = 66
LAM_INIT = 0.8 - 0.6 * math.exp(-0.3 * 0)

STAGE = [99]
MAPS_ONLY = [False]
SEQ = [False]
RUN_KW = {}
DEBUG = []
LAST = {}


class Prog:
    def __init__(self, nc, sync_same_engine=True):
        self.nc = nc
        self.ops = {e: [] for e in ENGS}
        self.count = {e: 0 for e in ENGS}
        self.sems = {}
        self.chan_count = {}
        self.chan_inc = {}
        self.waited = {e: {} for e in ENGS}
        self.bufs = {}
        self.sync_same = sync_same_engine

    def sem(self, name):
        if name not in self.sems:
            ctx = self.nc.semaphore(name)
            self.sems[name] = ctx.__enter__()
        return self.sems[name]

    def _need(self, eng, tok, waits):
        if tok is None:
            return
        sname, val, teng = tok
        if teng == eng and (eng == "pe" or not self.sync_same):
            return
        if val <= self.waited[eng].get(sname, 0):
            return
        self.waited[eng][sname] = val
        waits.append((sname, val))

    def _deps(self, eng, reads, writes):
        waits = []
        for k in reads:
            b = self.bufs.get(k)
            if b is not None:
                self._need(eng, b["w"], waits)
                if k[0] == "B":
                    for t in b["r"]:
                        if t[2] != eng:
                            self._need(eng, t, waits)
        for k in writes:
            b = self.bufs.get(k)
            if b is not None:
                self._need(eng, b["w"], waits)
                for t in b["r"]:
                    self._need(eng, t, waits)
        return waits

    def _commit(self, tok, reads, writes):
        for k in reads:
            b = self.bufs.setdefault(k, {"w": None, "r": []})
            b["r"].append(tok)
            if len(b["r"]) > 64:
                b["r"] = b["r"][-48:]
        for k in writes:
            self.bufs[k] = {"w": tok, "r": []}

    def op(self, eng, fn, reads=(), writes=()):
        waits = self._deps(eng, reads, writes)
        idx = self.count[eng]
        self.count[eng] += 1
        ep, k = divmod(idx, EPOCH)
        sname = f"p_{eng}_{ep}"
        self.sem(sname)
        tok = (sname, k + 1, eng)
        self.ops[eng].append((waits, fn, (sname, 1)))
        self._commit(tok, reads, writes)
        return tok

    def dma(self, eng, chan, fn, reads=(), writes=(), inc=16):
        waits = self._deps(eng, reads, writes)
        n = self.chan_count.get(chan, 0)
        sname = f"d_{chan}"
        self.sem(sname)
        self.chan_inc[chan] = inc
        if n > 0:
            self._need(eng, (sname, inc * n, "dma"), waits)
        self.chan_count[chan] = n + 1
        tok = (sname, inc * (n + 1), "dma")
        self.ops[eng].append((waits, fn, (sname, inc)))
        self._commit(tok, reads, writes)
        return tok

    def barrier(self):
        toks = []
        for e in ENGS:
            n = self.count[e]
            if n:
                ep, k = divmod(n - 1, EPOCH)
                toks.append((f"p_{e}_{ep}", k + 1, e))
        for c, n in self.chan_count.items():
            if c.startswith("ccrs"):
                continue
            toks.append((f"d_{c}", self.chan_inc[c] * n, "dma"))
        for e in ENGS:
            waits = []
            for t in toks:
                if t[2] == e and t[2] != "dma":
                    pass
                sname, val, _ = t
                if val > self.waited[e].get(sname, 0):
                    self.waited[e][sname] = val
                    waits.append((sname, val))
            self.ops[e].append((waits, None, None))

    def emit(self):
        nc = self.nc
        sems = self.sems
        ops = self.ops
        with nc.Block() as block:
            def run(e, engobj):
                for waits, fn, inc in ops[e]:
                    for sname, val in waits:
                        engobj.wait_ge(sems[sname], val)
                    if fn is not None:
                        fn(engobj).then_inc(sems[inc[0]], inc[1])

            @block.tensor
            def _(eng):
                run("pe", eng)

            @block.scalar
            def _(eng):
                run("act", eng)

            @block.vector
            def _(eng):
                run("dve", eng)

            @block.gpsimd
            def _(eng):
                run("pool", eng)

            @block.sync
            def _(eng):
                run("sp", eng)


def _isz(dt):
    return 2 if dt == BF16 else 4


class Arena:
    def __init__(self, nc, limit=229376 - 1024):
        self.nc = nc
        self.off = 16384 + 1024
        self.n = 0
        self.limit = limit

    def alloc(self, name, shape, dt):
        sz = _isz(dt)
        for s in shape[1:]:
            sz *= s
        sz = (sz + 63) // 64 * 64
        t = self.nc.alloc_sbuf_tensor_at(f"{name}_{self.n}", list(shape), dt, offset=self.off)
        self.off += sz
        self.n += 1
        assert self.off <= self.limit, (name, self.off)
        return t


NCONST = 13


def make_consts():
    idx = np.arange(128)
    blk = (idx[:, None] // 64) == (idx[None, :] // 64)
    k = idx[:, None]
    m = idx[None, :]
    c = np.zeros((NCONST, 128, 128), np.float32)
    c[0] = np.eye(128)
    c[1] = 1.0
    c[2] = blk & (k <= m)
    c[3] = blk & (k >= m)
    c[4] = blk
    c[5] = (k < 64) & (m >= 0)
    c[6] = (k >= 64) & (m >= 0)
    c[7] = blk & (m > k)
    c[8] = blk & (m >= k)
    c[9] = blk & (m < k)
    c[10] = blk & (m <= k)
    pm = np.zeros((128, 128), np.float32)
    for mm_ in range(128):
        i = mm_ % 64
        r = i % 32
        partner = mm_ + 16 if r < 16 else mm_ - 16
        pm[partner, mm_] = 1.0
    c[11] = pm
    g = np.zeros((128, 128), np.float32)
    g[:64, :64] = (idx[:64, None] % 16) == (idx[None, :64] % 16)
    c[12] = g
    return np.ascontiguousarray(c.transpose(1, 0, 2).reshape(128, NCONST * 128))


def make_rope():
    f32 = np.float32
    t = np.arange(L)
    rows = (t // 64).astype(f32)
    cols = (t % 64).astype(f32)
    inv_freq = np.power(f32(10000.0), -np.arange(0, 32, 2, dtype=f32) / f32(32)).astype(f32)
    ang_row = (rows[:, None] * inv_freq[None, :]).astype(f32)
    ang_col = (cols[:, None] * inv_freq[None, :]).astype(f32)
    cosT = np.zeros((128, L), f32)
    sinT = np.zeros((128, L), f32)
    for p in range(128):
        i = p % 64
        ang = ang_row if i < 32 else ang_col
        r = i % 32
        f = r % 16
        sign = -1.0 if r < 16 else 1.0
        cosT[p] = np.cos(ang[:, f]).astype(f32)
        sinT[p] = (sign * np.sin(ang[:, f])).astype(f32)
    return np.stack([cosT, sinT], 0)


def build(debug=(), stage=99):
    nc = bass.Bass("TRN2", target_bir_lowering=False)
    P = Prog(nc)

    def din(name, shape, dt=F32):
        return nc.dram_tensor(name, list(shape), dt, kind="ExternalInput").ap()

    xb = din("xb", [L, D])
    ctxb = din("ctxb", [LC, D])
    xo = din("xo", [2048, D])
    cT_d = din("cT", [128, 16])
    wmod = din("wmod", [D, 6 * D])
    bm1T_d = din("bm1T", [128, 16])
    bm2_d = din("bm2", [1, 4096])
    g1T_d = din("g1T", [128, 8])
    g2T_d = din("g2T", [128, 8])
    fgbc_d = din("fgbc", [128, D])
    wslab_d = din("wslab", [D, 896])
    wab_d = din("wab", [D, 4])
    cwT_d = din("cwT", [128, 9])
    gsc_d = din("gsc", [128, 4])
    gng_d = din("gng", [128, 128])
    sgg_d = din("sgg", [128, 128])
    lamv_d = din("lamv", [128, 256])
    wout_d = din("wout", [2, 128, D])
    rope_d = din("rope", [2, 128, L])
    consts_d = din("consts", [128, NCONST * 128])
    wr_d = din("wr", [128, 128])
    wshape = [16, D, D] if stage > 4 else [1, 8, 8]
    wg_d = din("wg", wshape)
    wu_d = din("wu", wshape)
    wd_d = din("wd", wshape)
    out_d = nc.dram_tensor("out", [2048, D], F32, kind="ExternalOutput").ap()

    kt_s = nc.dram_tensor("kt_s", [128, LC + L], BF16).ap()
    qt_s = nc.dram_tensor("qt_s", [128, L], BF16).ap()
    v_s = nc.dram_tensor("v_s", [NSC, 128, 130], BF16).ap()
    rs_in = [nc.dram_tensor(f"rs_in{i}", [2048, D], F32) for i in range(4)]
    rs_out = [nc.dram_tensor(f"rs_out{i}", [512, D], F32) for i in range(4)]
    ag_in = nc.dram_tensor("ag_in", [16, 2048], F32)
    ag_out = nc.dram_tensor("ag_out", [64, 2048], F32)
    xnew_s = nc.dram_tensor("xnew_s", [2048, D], F32).ap()

    dbg_outs = {}

    def finish():
        waits = []
        for k in ["out0", "out1"] + ["dbg_" + n for n in dbg_outs]:
            b = P.bufs.get(k)
            if b is not None:
                P._need("pool", b["w"], waits)
        P.ops["pool"].append((waits, None, None))
        P.emit()
        return nc, dbg_outs

    def dbg(name, src_ap, shape, key):
        if name in debug:
            o = nc.dram_tensor("dbg_" + name, list(shape), src_ap.tensor.dtype, kind="ExternalOutput").ap()
            dbg_outs[name] = o
            P.dma("pool", "dbg_" + name, lambda e: e.dma_start(out=o, in_=src_ap), reads=[key], writes=["dbg_" + name])

    psum = nc.alloc_psum_tensor("ps", [128, 8, 512], F32)

    def bank(i):
        return psum[:, i, :]

    def mm(out, lhsT, rhs, r, w, start=True, stop=True):
        P.op("pe", lambda e: e.matmul(out, lhsT=lhsT, rhs=rhs, start=start, stop=stop), reads=r, writes=w)

    def tr(out, in_, idt, r, w):
        P.op("pe", lambda e: e.transpose(out, in_, idt), reads=r, writes=w)

    def act(out, in_, func, r, w, bias=None, scale=None, accum=None):
        kw = {}
        if bias is not None:
            kw["bias"] = bias
        if scale is not None:
            kw["scale"] = scale
        if accum is not None:
            kw["accum_out"] = accum
        P.op("act", lambda e: e.activation(out=out, in_=in_, func=func, **kw), reads=r, writes=w)

    def ts(eng, out, in0, s1, s2, op0, op1, r, w, accum=None):
        if op1 is None:
            P.op(eng, lambda e: e.tensor_scalar(out=out, in0=in0, scalar1=s1, scalar2=None, op0=op0), reads=r, writes=w)
        elif accum is None:
            P.op(eng, lambda e: e.tensor_scalar(out=out, in0=in0, scalar1=s1, scalar2=s2, op0=op0, op1=op1), reads=r, writes=w)
        else:
            P.op(eng, lambda e: e.tensor_scalar(out=out, in0=in0, scalar1=s1, scalar2=s2, op0=op0, op1=op1, accum_out=accum),
                 reads=r, writes=w)

    def tt(eng, out, in0, in1, op, r, w):
        P.op(eng, lambda e: e.tensor_tensor(out=out, in0=in0, in1=in1, op=op), reads=r, writes=w)

    def stt(out, in0, scalar, in1, op0, op1, r, w):
        P.op("dve", lambda e: e.scalar_tensor_tensor(out=out, in0=in0, scalar=scalar, in1=in1, op0=op0, op1=op1), reads=r, writes=w)

    def cp(eng, out, in_, r, w):
        if eng == "act":
            P.op("act", lambda e: e.copy(out=out, in_=in_), reads=r, writes=w)
        else:
            P.op(eng, lambda e: e.tensor_copy(out=out, in_=in_), reads=r, writes=w)

    def recip(out, in_, r, w):
        P.op("dve", lambda e: e.reciprocal(out=out, in_=in_), reads=r, writes=w)

    def mset(eng, ap, val, w):
        P.op(eng, lambda e: e.memset(ap, val), reads=(), writes=w)

    def dma(eng, chan, out, in_, r, w):
        P.dma(eng, chan, lambda e: e.dma_start(out=out, in_=in_), reads=r, writes=w)

    A = Arena(nc)
    cst = A.alloc("cst", [128, NCONST, 128], F32)
    dma("sp", "cst", cst[:, :, :].rearrange("p a b -> p (a b)"), consts_d, (), ["cst"])

    def C(i):
        return cst[:, i, :]
    ident, ones, triF, triB, blkm, H0, H1 = C(0), C(1), C(2), C(3), C(4), C(5), C(6)
    mS = [C(7), C(9)]
    mI = [C(8), C(10)]
    tri = [triF, triB]
    identb = A.alloc("identb", [128, 128], BF16)
    pmb = A.alloc("pmb", [128, 128], BF16)
    cp("dve", identb[:, :], ident, ["cst"], ["identb"])
    cp("dve", pmb[:, :], C(11), ["cst"], ["pmb"])

    small = A.alloc("small", [128, 512], F32)
    _sc = [0]

    def col(n=1):
        c0 = _sc[0]
        _sc[0] += n
        assert _sc[0] <= 512
        return small[:, c0:c0 + n]

    cT = col(16)
    bm1T = col(16)
    g1T = col(8)
    g2T = col(8)
    cwT = col(9)
    gsc = col(4)
    dma("sp", "sm0", cT, cT_d, (), ["cT"])
    dma("sp", "sm1", bm1T, bm1T_d, (), ["bm1T"])
    dma("sp", "sm2", g1T, g1T_d, (), ["g1T"])
    dma("sp", "sm3", g2T, g2T_d, (), ["g2T"])
    dma("sp", "sm4", cwT, cwT_d, (), ["cwT"])
    dma("sp", "sm5", gsc, gsc_d, (), ["gsc"])
    gng = A.alloc("gng", [128, 128], F32)
    sgg = A.alloc("sgg", [128, 128], F32)
    dma("sp", "sm6", gng[:, :], gng_d, (), ["gng"])
    dma("sp", "sm7", sgg[:, :], sgg_d, (), ["sgg"])
    lamv = A.alloc("lamv", [128, 256], F32)
    dma("sp", "sm8", lamv[:, :], lamv_d, (), ["lamv"])

    lamt = col(8)
    lsc = A.alloc("lsc", [128, 64], F32)
    P.op("dve", lambda e: e.tensor_tensor(out=lsc[:, :], in0=lamv[:, 0:64], in1=lamv[:, 64:128], op=ALU.mult), reads=["lamv"], writes=["lsc"])
    P.op("dve", lambda e: e.tensor_reduce(out=lamt[:, 0:1], in_=lsc[:, :], axis=AX.X, op=ALU.add), reads=["lsc"], writes=["lam0"])
    P.op("dve", lambda e: e.tensor_tensor(out=lsc[:, :], in0=lamv[:, 128:192], in1=lamv[:, 192:256], op=ALU.mult), reads=["lamv", "lam0"], writes=["lsc"])
    P.op("dve", lambda e: e.tensor_reduce(out=lamt[:, 1:2], in_=lsc[:, :], axis=AX.X, op=ALU.add), reads=["lsc"], writes=["lam1"])
    act(lamt[:, 2:4], lamt[:, 0:2], AF.Exp, ["lam0", "lam1"], ["lam2"])
    tt("dve", lamt[:, 4:5], lamt[:, 2:3], lamt[:, 3:4], ALU.subtract, ["lam2"], ["lam3"])
    ts("dve", lamt[:, 5:6], lamt[:, 4:5], -1.0, -LAM_INIT, ALU.mult, ALU.add, ["lam3"], ["nlam"])
    nlam = lamt[:, 5:6]

    scb = A.alloc("scb", [128, 8, 2], BF16)
    persist_mark = A.off

    KnT = A.alloc("KnT", [128, NSC * 128], BF16)
    QnT = A.alloc("QnT", [128, NSC * 128], BF16)
    Vg = A.alloc("Vg", [128, NSC, 128], BF16)
    gate_s = A.alloc("gate_s", [128, 64, 128], BF16)
    abt = A.alloc("abt", [128, NSC, 4], F32)
    gdn_mark = A.off

    wslab = A.alloc("wslab", [128, 8, 896], BF16)
    wab = A.alloc("wab", [128, 8, 4], BF16)
    for kc in range(8):
        dma("pool", f"wsl{kc % 4}", wslab[:, kc, :], wslab_d[kc * 128:(kc + 1) * 128, :], (), ["wslab"])
    dma("pool", "wab", wab[:, :, :], wab_d.rearrange("(kc p) n -> p kc n", p=128), (), ["wab"])

    m1 = A.alloc("m1", [128, 16, 2], F32)
    a1 = col(8)
    a1c = col(8)
    ipw_mark = A.off
    wm1 = A.alloc("wm1", [128, 8, 2048], BF16)
    for kc in range(8):
        dma("pool", f"wm{kc}", wm1[:, kc, :], wmod[kc * 128:(kc + 1) * 128, 0:2048], (), [f"wm1_{kc}"])
    act(scb[:, :, :].rearrange("p a b -> p (a b)"), cT, AF.Silu, ["cT"], ["scb"])
    pmod = bank(0)[:, 0:32]
    for c_ in range(16):
        for kc in range(8):
            mm(pmod[:, c_ * 2:(c_ + 1) * 2], wm1[:, kc, c_ * 128:(c_ + 1) * 128], scb[:, kc, :], [f"wm1_{kc}", "scb"], ["B0"],
               start=(kc == 0), stop=(kc == 7))
    pmod3 = pmod.rearrange("p (c v) -> p c v", v=2)
    for v in range(2):
        tt("dve", m1[:, :, v], pmod3[:, :, v], bm1T, ALU.add, ["B0", "bm1T"], ["m1"])
    stt(a1, m1[:, 8:16, 0], 1.0, g1T, ALU.add, ALU.mult, ["m1", "g1T"], ["a1"])
    stt(a1c, m1[:, 8:16, 1], 1.0, g1T, ALU.add, ALU.mult, ["m1", "g1T"], ["a1c"])
    if stage == 0:
        return finish()
    P.barrier()
    A.off = ipw_mark

    NT = 33
    xt = [A.alloc(f"xt{i}", [128, 2, D], F32) for i in range(3)]
    hT = [A.alloc(f"hT{i}", [128, 8, 256], BF16) for i in range(2)]
    pre = [A.alloc(f"pre{i}", [128, 3, 258], F32) for i in range(4)]
    rtab = [A.alloc(f"rtab{i}", [128, 2, 256], F32) for i in range(3)]
    sqj = A.alloc("sqj", [128, D], BF16)
    nrm = A.alloc("nrm", [128, 8], F32)
    qb = [A.alloc(f"qb{i}", [128, 256], BF16) for i in range(2)]
    rt1 = [A.alloc(f"rt1_{i}", [128, 256], F32) for i in range(2)]
    rt2 = [A.alloc(f"rt2_{i}", [128, 256], F32) for i in range(2)]
    qkst = [A.alloc(f"qkst{i}", [128, 256], BF16) for i in range(4)]
    vst = [A.alloc(f"vst{i}", [128, 2, 130], BF16) for i in range(2)]
    cacc = A.alloc("cacc", [128, 3, 256], F32)
    csil = A.alloc("csil", [128, 3, 256], F32)
    csq = A.alloc("csq", [128, 2, 256], F32)
    rinv = A.alloc("rinv", [128, 2, 256], F32)
    for i in range(2):
        mset("pool", vst[i][:, :, 128:130], 1.0, [f"vst{i}"])

    def tile_info(t):
        if t == 0:
            return ctxb, 0, True
        return xb[(t - 1) * 256:t * 256, :], LC + (t - 1) * 256, False

    fm_cnt = [0]

    def loadx(t):
        src, koff, is_ctx = tile_info(t)
        dma("sp", f"x{t % 3}", xt[t % 3][:, :, :], src.rearrange("(s p) d -> p s d", p=128), (), [f"xt{t % 3}"])
        if not is_ctx:
            dma("sp", f"rt{t % 3}", rtab[t % 3][:, :, :], rope_d[:, :, (t - 1) * 256:t * 256].rearrange("a p n -> p a n"), (), [f"rtab{t % 3}"])

    def front(t):
        src, koff, is_ctx = tile_info(t)
        xs = xt[t % 3]
        hs = hT[t % 2]
        kx, kh = f"xt{t % 3}", f"hT{t % 2}"
        sh = m1[:, 0:8, 1] if is_ctx else m1[:, 0:8, 0]
        aa = a1c if is_ctx else a1
        for s in range(2):
            act(sqj[:, :], xs[:, s, :], AF.Square, [kx], ["sqj", f"nrm{s}"], accum=nrm[:, s:s + 1])
            yield
        act(nrm[:, 2:4], nrm[:, 0:2], AF.Sqrt, ["nrm0", "nrm1"], ["nrmB"], bias=EPS, scale=1.0 / D)
        yield
        recip(nrm[:, 4:6], nrm[:, 2:4], ["nrmB"], ["nrmC"])
        yield
        for s in range(2):
            ts("pool", xs[:, s, :], xs[:, s, :], nrm[:, 4 + s:5 + s], 1.0, ALU.mult, ALU.mult, [kx, "nrmC"], [kx])
            yield
        for kc in range(8):
            pb = kc % 2
            pT = bank(pb)[:, 0:256]
            for s in range(2):
                tr(pT[:, s * 128:(s + 1) * 128], xs[:, s, kc * 128:(kc + 1) * 128], ident, [kx, "cst"], [f"B{pb}"])
                yield
            if kc % 2 == 0:
                act(hs[:, kc, :], pT, AF.Identity, [f"B{pb}", "a1", "a1c", "m1"], [f"{kh}_{kc}"], bias=sh[:, kc:kc + 1], scale=aa[:, kc:kc + 1])
                yield
            else:
                ts("dve", hs[:, kc, :], pT, aa[:, kc:kc + 1], sh[:, kc:kc + 1], ALU.mult, ALU.add, [f"B{pb}", "a1", "a1c", "m1"], [f"{kh}_{kc}"])
                yield

    def back(t, part):
        src, koff, is_ctx = tile_info(t)
        hs = hT[t % 2]
        kh = f"hT{t % 2}"
        pr = pre[t % 4]
        kp = f"pre{t % 4}"
        for blk_ in (range(5) if part == 0 else ()):
            if is_ctx and blk_ == 0:
                continue
            fb = 2 + (fm_cnt[0] % 2)
            fm_cnt[0] += 1
            pf = bank(fb)[:, 0:256]
            for kc in range(8):
                mm(pf, wslab[:, kc, blk_ * 128:(blk_ + 1) * 128], hs[:, kc, :], ["wslab", f"{kh}_{kc}"], [f"B{fb}"], start=(kc == 0), stop=(kc == 7))
                yield
            if blk_ < 2:
                st = qkst[(t % 2) * 2 + blk_]
                kst = f"qkst{(t % 2) * 2 + blk_}"
                dst = (qt_s[:, (t - 1) * 256:t * 256] if blk_ == 0 else kt_s[:, koff:koff + 256]) if not is_ctx else kt_s[:, 0:256]
                if is_ctx:
                    cp("act", st[:, :], pf, [f"B{fb}"], [kst])
                    yield
                else:
                    q_b = qb[blk_]
                    rs_ = rtab[t % 3]
                    cp("act", q_b[:, :], pf, [f"B{fb}"], [f"qb{blk_}"])
                    yield
                    pp = bank(6)[:, blk_ * 256:(blk_ + 1) * 256]
                    mm(pp, pmb[:, :], q_b[:, :], ["pmb", f"qb{blk_}"], ["B6"])
                    yield
                    tt("dve", rt1[blk_][:, :], pf, rs_[:, 0, :], ALU.mult, [f"B{fb}", f"rtab{t % 3}"], [f"rt1_{blk_}"])
                    yield
                    tt("dve", rt2[blk_][:, :], pp, rs_[:, 1, :], ALU.mult, ["B6", f"rtab{t % 3}"], [f"rt2_{blk_}"])
                    yield
                    tt("pool", st[:, :], rt1[blk_][:, :], rt2[blk_][:, :], ALU.add, [f"rt1_{blk_}", f"rt2_{blk_}"], [kst])
                    yield
                if stage != 0.325:
                    dma("pool", f"qk{(t % 2) * 2 + blk_}", dst, st[:, :], [kst], ["ktqt_s"])
                    yield
            else:
                c_ = blk_ - 2
                if c_ % 2 == 0:
                    cp("act", pr[:, c_, 1:257], pf, [f"B{fb}"], [kp])
                    yield
                else:
                    cp("dve", pr[:, c_, 1:257], pf, [f"B{fb}"], [kp])
                    yield
        if part == 0:
            return
        vs_ = vst[t % 2]
        for s in range(2):
            sc = (koff // 128) + s
            pv = bank(4 + s)
            kb = f"B{4 + s}"
            for kc in range(8):
                mm(pv[:, 0:128], hs[:, kc, s * 128:(s + 1) * 128], wslab[:, kc, 640:768], [f"{kh}_{kc}", "wslab"], [kb], start=(kc == 0), stop=(kc == 7))
                yield
            if not is_ctx:
                for kc in range(8):
                    mm(pv[:, 128:256], hs[:, kc, s * 128:(s + 1) * 128], wslab[:, kc, 768:896], [f"{kh}_{kc}", "wslab"], [kb], start=(kc == 0), stop=(kc == 7))
                    yield
            if stage != 0.331:
                for kc in range(8):
                    mm(pv[:, 256:260], hs[:, kc, s * 128:(s + 1) * 128], wab[:, kc, :], [f"{kh}_{kc}", "wab"], [kb], start=(kc == 0), stop=(kc == 7))
                    yield
            if stage != 0.333:
                cp("act", vs_[:, s, 0:128], pv[:, 0:128], [kb], [f"vst{t % 2}"])
                yield
            if not is_ctx:
                act(gate_s[:, sc - 2, :], pv[:, 128:256], AF.Silu, [kb], ["gate_s"])
                yield
            if stage not in (0.331, 0.332):
                cp("dve", abt[:, sc, :], pv[:, 256:260], [kb], ["abt"])
                yield
        dma("pool", f"v{t % 2}", v_s[(koff // 128):(koff // 128) + 2, :, :].rearrange("s p n -> p s n"), vs_[:, :, :], [f"vst{t % 2}"], ["v_s"])
        yield

    def conv_stage(t, left, right):
        src, koff, is_ctx = tile_info(t)
        pr = pre[t % 4]
        kp = f"pre{t % 4}"
        if left is None:
            mset("pool", pr[:, :, 0:1], 0.0, [kp])
            yield
        else:
            cp("pool", pr[:, :, 0:1], pre[left % 4][:, :, 256:257], [f"pre{left % 4}", kp], [kp])
            yield
        if right is None:
            mset("pool", pr[:, :, 257:258], 0.0, [kp])
            yield
        else:
            cp("pool", pr[:, :, 257:258], pre[right % 4][:, :, 1:2], [f"pre{right % 4}", kp], [kp])
            yield
        for c_ in range(3):
            ts("dve", cacc[:, c_, :], pr[:, c_, 0:256], cwT[:, c_ * 3:c_ * 3 + 1], None, ALU.mult, None, [kp, "cwT"], ["cacc"])
            yield
            stt(cacc[:, c_, :], pr[:, c_, 1:257], cwT[:, c_ * 3 + 1:c_ * 3 + 2], cacc[:, c_, :], ALU.mult, ALU.add, [kp, "cacc"], ["cacc"])
            yield
            stt(cacc[:, c_, :], pr[:, c_, 2:258], cwT[:, c_ * 3 + 2:c_ * 3 + 3], cacc[:, c_, :], ALU.mult, ALU.add, [kp, "cacc"], ["cacc"])
            yield
        act(csil[:, :, :], cacc[:, :, :], AF.Silu, ["cacc"], ["csil"])
        yield
        tt("pool", csq[:, :, :], csil[:, 0:2, :], csil[:, 0:2, :], ALU.mult, ["csil"], ["csq"])
        yield
        pss = bank(7)
        mm(pss, ones, csq[:, :, :].rearrange("p a b -> p (a b)"), ["cst", "csq"], ["B7"])
        yield
        act(rinv[:, :, :].rearrange("p a b -> p (a b)"), pss, AF.Sqrt, ["B7"], ["rinvA"], bias=EPS, scale=1.0)
        yield
        recip(rinv[:, :, :].rearrange("p a b -> p (a b)"), rinv[:, :, :].rearrange("p a b -> p (a b)"), ["rinvA"], ["rinv"])
        yield
        stt(QnT[:, koff:koff + 256], csil[:, 0, :], 128.0 ** -0.5, rinv[:, 0, :], ALU.mult, ALU.mult, ["csil", "rinv"], ["QnT"])
        yield
        tt("dve", KnT[:, koff:koff + 256], csil[:, 1, :], rinv[:, 1, :], ALU.mult, ["csil", "rinv"], ["KnT"])
        yield
        pvt = bank(7)
        for s in range(2):
            tr(pvt[:, 256 + s * 128:256 + (s + 1) * 128], csil[:, 2, s * 128:(s + 1) * 128], ident, ["csil", "cst"], ["B7"])
            yield
        cp("act", Vg[:, koff // 128:koff // 128 + 2, :], pvt[:, 256:512].rearrange("p (s n) -> p s n", s=2), ["B7"], ["Vg"])
        yield

    def interleave0(*gens):
        gens = list(gens)
        if SEQ[0] == 1:
            for g in gens:
                for _ in g:
                    pass
            return
        while gens:
            for g in list(gens):
                try:
                    next(g)
                except StopIteration:
                    gens.remove(g)

    def conv_for(k):
        left = None if k in (0, 1) else k - 1
        right = None if k in (0, NT - 1) else k + 1
        return conv_stage(k, left, right)

    loadx(0)
    loadx(1)
    interleave0(front(0))
    for t in range(NT):
        if t + 2 < NT:
            loadx(t + 2)
        gl = [back(t, 0), back(t, 1)]
        if t + 1 < NT:
            gl.append(front(t + 1))
        if t - 2 >= 0:
            if SEQ[0] == 2:
                interleave0(*gl)
                gl = []
            gl.append(conv_for(t - 2))
        interleave0(*gl)
    interleave0(conv_for(NT - 2))
    interleave0(conv_for(NT - 1))

    dbg("KnT", KnT[:, :], [128, NSC * 128], "KnT")
    dbg("QnT", QnT[:, :], [128, NSC * 128], "QnT")
    dbg("Vg", Vg[:, :, :], [128, NSC, 128], "Vg")
    dbg("abt", abt[:, :, :], [128, NSC, 4], "abt")

    if stage == 1:
        return finish()
    P.barrier()
    A.off = gdn_mark

    og = A.alloc("og", [128, 64, 128], F32)
    mset("pool", og[:, :, :], 0.0, [f"og{i}" for i in range(64)])
    gg = A.alloc("gg", [128, 2, NSC], F32)
    bet = A.alloc("bet", [128, 2, NSC], F32)
    nbet = A.alloc("nbet", [128, 2, NSC], F32)
    Gc = A.alloc("Gc", [128, 2, NSC], F32)
    eG = A.alloc("eG", [128, 2, NSC], F32)
    eGl = A.alloc("eGl", [128, 2, NSC], F32)
    egl = A.alloc("egl", [128, 2, 2, NSC], F32)
    gtmp = A.alloc("gtmp", [128, 2, NSC], F32)
    nea = col(2)
    act(nea, gsc[:, 0:2], AF.Exp, ["gsc"], ["neaA"])
    ts("dve", nea, nea, -1.0, None, ALU.mult, None, ["neaA"], ["nea"])
    for d in range(2):
        act(gtmp[:, d, :], abt[:, :, d], AF.Exp, ["abt", "gsc"], ["gtmpA"], bias=gsc[:, 2 + d:3 + d], scale=1.0)
        act(gtmp[:, d, :], gtmp[:, d, :], AF.Ln, ["gtmpA"], ["gtmpB"], bias=1.0, scale=1.0)
        ts("dve", gg[:, d, :], gtmp[:, d, :], nea[:, d:d + 1], None, ALU.mult, None, ["gtmpB", "nea"], ["gg"])
        act(bet[:, d, :], abt[:, :, 2 + d], AF.Sigmoid, ["abt"], ["bet"])
        ts("dve", nbet[:, d, :], bet[:, d, :], -1.0, None, ALU.mult, None, ["bet"], ["nbet"])
        pg_ = bank(0)
        mm(pg_[:, 0:NSC], tri[d], gg[:, d, :], ["cst", "gg"], ["B0"])
        mm(pg_[:, 128:128 + NSC], blkm, gg[:, d, :], ["cst", "gg"], ["B0"])
        mm(pg_[:, 256:256 + NSC], H0, gg[:, d, :], ["cst", "gg"], ["B0"])
        mm(pg_[:, 384:384 + NSC], H1, gg[:, d, :], ["cst", "gg"], ["B0"])
        cp("dve", Gc[:, d, :], pg_[:, 0:NSC], ["B0"], ["Gc"])
        act(eG[:, d, :], pg_[:, 0:NSC], AF.Exp, ["B0"], ["eG"])
        tt("dve", gtmp[:, d, :], pg_[:, 128:128 + NSC], Gc[:, d, :], ALU.subtract, ["B0", "Gc", "gtmpB"], ["gtmpC"])
        act(eGl[:, d, :], gtmp[:, d, :], AF.Exp, ["gtmpC"], ["eGl"])
        act(egl[:, d, 0, :], pg_[:, 256:256 + NSC], AF.Exp, ["B0"], ["egl"])
        act(egl[:, d, 1, :], pg_[:, 384:384 + NSC], AF.Exp, ["B0"], ["egl"])

    dbg("gg", gg[:, :, :], [128, 2, NSC], "gg")
    dbg("Gc", Gc[:, :, :], [128, 2, NSC], "Gc")

    def pcbuf(name, dt, n=128):
        return [A.alloc(f"{name}{d}", [128, n], dt) for d in range(2)]
    gram = pcbuf("gram", F32, 256)
    Rw = pcbuf("Rw", BF16)
    kdp = [[A.alloc(f"kd{d}_{q}", [128, 128], BF16) for d in range(2)] for q in range(2)]
    qd = pcbuf("qd", BF16)
    qdTp = [[A.alloc(f"qdT{d}_{q}", [128, 128], BF16) for d in range(2)] for q in range(2)]
    dg = pcbuf("dg", F32)
    tE = pcbuf("tE", F32)
    Es = pcbuf("Es", F32)
    Ei = pcbuf("Ei", F32)
    aqkTp = [[A.alloc(f"aqkT{d}_{q}", [128, 128], BF16) for d in range(2)] for q in range(2)]
    Xb = [pcbuf("Xa", BF16), pcbuf("Xb", BF16)]
    Yb = [pcbuf("Ya", BF16), pcbuf("Yb", BF16)]
    Rb = [pcbuf("Ra", BF16), pcbuf("Rb", BF16)]
    ATb = pcbuf("AT", BF16)
    WTp = [[A.alloc(f"WT{d}_{q}", [128, 128], BF16) for d in range(2)] for q in range(2)]
    UBbp = [[A.alloc(f"UBb{d}_{q}", [128, 128], F32) for d in range(2)] for q in range(2)]
    S32 = pcbuf("S32", F32)
    Sbf = pcbuf("Sbf", BF16)
    ub = pcbuf("ub", BF16)
    for d in range(2):
        mset("pool", S32[d][:, :], 0.0, [f"S32{d}"])
        mset("pool", Sbf[d][:, :], 0.0, [f"Sbf{d}"])

    slot = [0]

    def pslot():
        s = slot[0] % 4
        slot[0] += 1
        return bank(s)[:, 0:128], f"B{s}"

    def pslot_bf():
        ap_, k_ = pslot()
        return ap_[:, 0:64].bitcast(BF16), k_

    def precompute(sc, d, par):
        WT, UBb, kd, qdT, aqkT = WTp[par], UBbp[par], kdp[par], qdTp[par], aqkTp[par]
        pq = str(par)
        lat = sc >= 2
        c0 = sc * 128
        sd = str(d)
        p1, k1 = pslot()
        mm(p1, KnT[:, c0:c0 + 128], KnT[:, c0:c0 + 128], ["KnT"], [k1])
        yield
        cp("act", gram[d][:, 0:128], p1, [k1], ["gramA" + sd])
        yield
        if lat:
            p2, k2 = pslot()
            mm(p2, KnT[:, c0:c0 + 128], QnT[:, c0:c0 + 128], ["KnT", "QnT"], [k2])
            yield
            cp("act", gram[d][:, 128:256], p2, [k2], ["gramB" + sd])
            yield
        p3, k3 = pslot_bf()
        tr(p3, KnT[:, c0:c0 + 128], identb[:, :], ["KnT", "identb"], [k3])
        yield
        act(Rw[d][:, :], p3, AF.Identity, [k3, "eG"], ["Rw" + sd], scale=eG[:, d, sc:sc + 1])
        yield
        ts("dve", kd[d][:, :], p3, eGl[:, d, sc:sc + 1], None, ALU.mult, None, [k3, "eGl"], ["kd" + sd + pq])
        yield
        if lat:
            p4, k4 = pslot_bf()
            tr(p4, QnT[:, c0:c0 + 128], identb[:, :], ["QnT", "identb"], [k4])
            yield
            act(qd[d][:, :], p4, AF.Identity, [k4, "eG"], ["qd" + sd], scale=eG[:, d, sc:sc + 1])
            yield
            p5, k5 = pslot_bf()
            tr(p5, qd[d][:, :], identb[:, :], ["qd" + sd, "identb"], [k5])
            yield
            cp("act", qdT[d][:, :], p5, [k5], ["qdT" + sd + pq])
            yield
        ts("pool", dg[d][:, :], ident, Gc[:, d, sc:sc + 1], 1.0, ALU.mult, ALU.mult, ["cst", "Gc"], ["dg" + sd])
        yield
        p6, k6 = pslot()
        mm(p6, ones, dg[d][:, :], ["cst", "dg" + sd], [k6])
        yield
        ts("dve", tE[d][:, :], p6, Gc[:, d, sc:sc + 1], 0.0, ALU.subtract, ALU.min, [k6, "Gc"], ["tEA" + sd])
        yield
        act(tE[d][:, :], tE[d][:, :], AF.Exp, ["tEA" + sd], ["tE" + sd])
        yield
        tt("pool", Es[d][:, :], tE[d][:, :], mS[d], ALU.mult, ["tE" + sd, "cst"], ["Es" + sd])
        yield
        X0 = Xb[0][d]
        stt(X0[:, :], gram[d][:, 0:128], nbet[:, d, sc:sc + 1], Es[d][:, :], ALU.mult, ALU.mult, ["gramA" + sd, "nbet", "Es" + sd], ["X0" + sd])
        yield
        if lat:
            tt("pool", Ei[d][:, :], tE[d][:, :], mI[d], ALU.mult, ["tE" + sd, "cst"], ["Ei" + sd])
            yield
            tt("dve", aqkT[d][:, :], gram[d][:, 128:256], Ei[d][:, :], ALU.mult, ["gramB" + sd, "Ei" + sd], ["aqkT" + sd + pq])
            yield
        p7, k7 = pslot_bf()
        tr(p7, X0[:, :], identb[:, :], ["X0" + sd, "identb"], [k7])
        yield
        Y0 = Yb[0][d]
        cp("act", Y0[:, :], p7, [k7], ["Y0" + sd])
        yield
        R0 = Rb[0][d]
        tt("pool", R0[:, :], X0[:, :], ident, ALU.add, ["X0" + sd, "cst"], ["R0" + sd])
        yield
        for lv in range(1, 6):
            a_, b_ = (lv - 1) % 2, lv % 2
            Xp, Yp, Rp = Xb[a_][d], Yb[a_][d], Rb[a_][d]
            Xn, Yn, Rn = Xb[b_][d], Yb[b_][d], Rb[b_][d]
            kXp, kYp, kRp = f"X{a_}{sd}", f"Y{a_}{sd}", f"R{a_}{sd}"
            kXn, kYn, kRn = f"X{b_}{sd}", f"Y{b_}{sd}", f"R{b_}{sd}"
            py, ky = pslot()
            mm(py, Xp[:, :], Yp[:, :], [kXp, kYp], [ky])
            yield
            if lv <= 4:
                px, kx_ = pslot()
                mm(px, Yp[:, :], Xp[:, :], [kXp, kYp], [kx_])
                yield
            cp("act", Yn[:, :], py, [ky], [kYn])
            yield
            if lv <= 4:
                cp("act" if lv % 2 == 0 else "dve", Xn[:, :], px, [kx_], [kXn])
                yield
            pr_, kr_ = pslot()
            mm(pr_, Yn[:, :], Rp[:, :], [kYn, kRp], [kr_])
            yield
            if lv < 5:
                tt("dve", Rn[:, :], pr_, Rp[:, :], ALU.add, [kr_, kRp], [kRn])
                yield
            else:
                tt("dve", ATb[d][:, :], pr_, Rp[:, :], ALU.add, [kr_, kRp], ["AT" + sd])
                yield
        p8, k8 = pslot()
        mm(p8, Rw[d][:, :], ATb[d][:, :], ["Rw" + sd, "AT" + sd], [k8])
        yield
        cp("act", WT[d][:, :], p8, [k8], ["WT" + sd + pq])
        yield
        p9, k9 = pslot()
        mm(p9, ATb[d][:, :], Vg[:, sc, :], ["AT" + sd, "Vg"], [k9])
        yield
        act(UBb[d][:, :], p9, AF.Identity, [k9, "bet"], ["UBb" + sd + pq], scale=bet[:, d, sc:sc + 1])
        yield

    def step(sc, hh, d, par):
        WT, UBb, kd, qdT, aqkT = WTp[par], UBbp[par], kdp[par], qdTp[par], aqkTp[par]
        pq = str(par)
        lat = sc >= 2
        sd = str(d)
        r0, r1 = hh * 64, hh * 64 + 64
        bA = bank(4 + 2 * d)
        bO = bank(5 + 2 * d)
        pws = bA[r0:r1, 0:128]
        mm(pws, WT[d][:, r0:r1], Sbf[d][:, :], ["WT" + sd + pq, "Sbf" + sd], [f"B{4 + 2 * d}"])
        yield
        stt(ub[d][r0:r1, :], pws, nbet[r0:r1, d, sc:sc + 1], UBb[d][r0:r1, :], ALU.mult, ALU.add, [f"B{4 + 2 * d}", "nbet", "UBb" + sd + pq], ["ub" + sd])
        yield
        if lat:
            po = bO[r0:r1, 0:128]
            mm(po, qdT[d][:, r0:r1], Sbf[d][:, :], ["qdT" + sd + pq, "Sbf" + sd], [f"B{5 + 2 * d}"], start=True, stop=False)
            yield
            mm(po, aqkT[d][r0:r1, r0:r1], ub[d][r0:r1, :], ["aqkT" + sd + pq, "ub" + sd], [f"B{5 + 2 * d}"], start=False, stop=True)
            yield
            tt("dve", og[r0:r1, sc - 2, :], po, og[r0:r1, sc - 2, :], ALU.add, [f"B{5 + 2 * d}", f"og{sc - 2}"], [f"og{sc - 2}"])
            yield
        pS = bA[:, 128:256]
        mm(pS, kd[d][r0:r1, :], ub[d][r0:r1, :], ["kd" + sd + pq, "ub" + sd], [f"B{4 + 2 * d}"])
        yield
        stt(S32[d][:, :], S32[d][:, :], egl[:, d, hh, sc:sc + 1], pS, ALU.mult, ALU.add, ["S32" + sd, "egl", f"B{4 + 2 * d}"], ["S32" + sd])
        yield
        cp("act", Sbf[d][:, :], S32[d][:, :], ["S32" + sd], ["Sbf" + sd])
        yield

    fwd = list(range(NSC))
    bwd = [1, 0] + list(range(NSC - 1, 1, -1))
    def seq(*gs):
        for g in gs:
            yield from g

    def interleave(*gens):
        gens = list(gens)
        while gens:
            for g in list(gens):
                try:
                    next(g)
                except StopIteration:
                    gens.remove(g)

    interleave(precompute(fwd[0], 0, 0), precompute(bwd[0], 1, 0))
    for i in range(NSC):
        par = i % 2
        gl = [seq(step(fwd[i], 0, 0, par), step(fwd[i], 1, 0, par)), seq(step(bwd[i], 1, 1, par), step(bwd[i], 0, 1, par))]
        if i + 1 < NSC:
            gl += [precompute(fwd[i + 1], 0, 1 - par), precompute(bwd[i + 1], 1, 1 - par)]
        interleave(*gl)

    P.op("pool", lambda e: e.memset(small[:, 500:501], 0.0), reads=[f"og{i}" for i in range(64)], writes=["og_all"])
    dbg("og", og[:, :, :], [128, 64, 128], "og_all")

    ogb = A.alloc("ogb", [128, 64, 128], BF16)
    ogs = A.alloc("ogs", [128, 64], F32)
    ogr = A.alloc("ogr", [128, 64], F32)
    ogt = A.alloc("ogt", [128, 128], F32)
    sqj2 = A.alloc("sqj2", [128, 128], BF16)
    for i in range(64):
        act(sqj2[:, :], og[:, i, :], AF.Square, ["og_all"], ["sqj2", "ogs"], accum=ogs[:, i:i + 1])
    act(ogr[:, :], ogs[:, :], AF.Sqrt, ["ogs"], ["ogrA"], bias=EPS, scale=1.0 / 128)
    recip(ogr[:, :], ogr[:, :], ["ogrA"], ["ogr"])
    for i in range(64):
        stt(ogt[:, :], og[:, i, :], ogr[:, i:i + 1], gng[:, :], ALU.mult, ALU.mult, ["og_all", "ogr", "gng"], ["ogt"])
        tt("dve", ogb[:, i, :], ogt[:, :], gate_s[:, i, :], ALU.mult, ["ogt", "gate_s"], ["ogb"])
    dbg("ogb", ogb[:, :, :], [128, 64, 128], "ogb")
    if stage == 2:
        return finish()

    P.barrier()
    after_gdn = A.off
    A.off = persist_mark
    KT0 = A.alloc("KT0", [128, LC + L], BF16)
    KT1 = A.alloc("KT1", [128, LC + L], BF16)
    QT = A.alloc("QT", [128, L], BF16)
    Vv = A.alloc("Vv", [128, NSC, 130], BF16)
    assert A.off <= gdn_mark, A.off
    lim1 = A.off
    A.off = after_gdn
    wout = A.alloc("wout", [128, 2, D], BF16)
    pbuf = [A.alloc(f"pb{i}", [128, 512], BF16) for i in range(4)]
    oas = [A.alloc(f"oa{i}", [128, 128], F32) for i in range(2)]
    obs = [A.alloc(f"ob{i}", [128, 128], F32) for i in range(2)]
    omx = A.alloc("omx", [128, 128], BF16)
    omT = [A.alloc(f"omT{i}", [128, 128], BF16) for i in range(2)]
    rsst = [A.alloc(f"rsst{i}", [128, D], F32) for i in range(2)]
    att = A.alloc("att", [128, 16], F32)
    sqj3 = A.alloc("sqj3", [128, 128], BF16)
    for kc in range(4):
        dma("sp", f"ld{kc % 2}", KT0[0:64, kc * 2112:(kc + 1) * 2112], kt_s[0:64, kc * 2112:(kc + 1) * 2112], ["ktqt_s"], ["KT0a"])
        dma("sp", f"ld{2 + kc % 2}", KT1[64:128, kc * 2112:(kc + 1) * 2112], kt_s[64:128, kc * 2112:(kc + 1) * 2112], ["ktqt_s"], ["KT1a"])
        dma("sp", f"ld{4 + kc % 2}", QT[:, kc * 2048:(kc + 1) * 2048], qt_s[:, kc * 2048:(kc + 1) * 2048], ["ktqt_s"], ["QT"])
    mset("pool", KT0[64:128, :], 0.0, ["KT0b"])
    mset("pool", KT1[0:64, :], 0.0, ["KT1b"])
    for kc in range(6):
        dma("sp", f"ld{6 + kc % 2}", Vv[:, kc * 11:(kc + 1) * 11, :], v_s[kc * 11:(kc + 1) * 11, :, :].rearrange("s p n -> p s n"), ["v_s"], ["Vv"])
    for f_ in range(2):
        dma("pool", f"wo{f_}", wout[:, f_, :], wout_d[f_, :, :], (), ["wout"])

    NKT = NSC
    pcnt = [0]
    if stage == 2.91:
        dbg("KT", KT0[:, :], [128, LC + L], "KT0a")
        dbg("Vv", Vv[:, :, :], [128, NSC, 130], "Vv")
        return finish()
    qorder = [(sblk, rr, qq) for sblk in range(4) for rr in range(4) for qq in range(2)]
    SB = [0, 1, 7]

    def q0_of(ent):
        sblk_, rr_, qq_ = ent
        return rr_ * 2048 + sblk_ * 512 + qq_ * 256

    def s_mm(q0, kt_):
        sb_ = SB[kt_ % 3]
        pS_ = bank(sb_)
        mm(pS_[:, 0:256], KT0[:, kt_ * 128:(kt_ + 1) * 128], QT[:, q0:q0 + 256], ["KT0a", "KT0b", "QT"], [f"B{sb_}"])
        mm(pS_[:, 256:512], KT1[:, kt_ * 128:(kt_ + 1) * 128], QT[:, q0:q0 + 256], ["KT1a", "KT1b", "QT"], [f"B{sb_}"])

    qlist = qorder[:1] if stage in (2.92, 2.93) else qorder
    s_mm(q0_of(qlist[0]), 0)
    s_mm(q0_of(qlist[0]), 1)
    for qi_, (sblk, rr, qq) in enumerate(qlist):
        q0 = q0_of((sblk, rr, qq))
        qt_ = q0 // 256
        for kt_ in range(NKT):
            sb_ = SB[kt_ % 3]
            pi = pcnt[0] % 4
            pcnt[0] += 1
            if kt_ + 2 < NKT:
                s_mm(q0, kt_ + 2)
            act(pbuf[pi][:, :], bank(sb_), AF.Exp, [f"B{sb_}"], [f"pb{pi}"], scale=0.125)
            for sub in range(2):
                for mp in range(2):
                    acc = bank(2 + sub * 2 + mp)[:, 0:129]
                    mm(acc, pbuf[pi][:, mp * 256 + sub * 128:mp * 256 + (sub + 1) * 128], Vv[:, kt_, 0:129], [f"pb{pi}", "Vv"],
                       [f"B{2 + sub * 2 + mp}"], start=(kt_ == 0), stop=(kt_ == NKT - 1))
        if qi_ + 1 < len(qlist):
            s_mm(q0_of(qlist[qi_ + 1]), 0)
            s_mm(q0_of(qlist[qi_ + 1]), 1)
        for sub in range(0 if stage == 2.93 else 2):
            a0 = bank(2 + sub * 2)
            a1_ = bank(3 + sub * 2)
            ss_ = str(sub)
            atc = att[:, 8 * sub:8 * sub + 8]
            recip(atc[:, 0:1], a0[:, 128:129], [f"B{2 + sub * 2}"], ["att0" + ss_])
            recip(atc[:, 1:2], a1_[:, 128:129], [f"B{3 + sub * 2}"], ["att1" + ss_])
            tt("dve", atc[:, 2:3], atc[:, 1:2], nlam, ALU.mult, ["att1" + ss_, "nlam"], ["att2" + ss_])
            ts("dve", oas[sub][:, :], a0[:, 0:128], atc[:, 0:1], None, ALU.mult, None, [f"B{2 + sub * 2}", "att0" + ss_], ["oa" + ss_])
            stt(obs[sub][:, :], a1_[:, 0:128], atc[:, 2:3], oas[sub][:, :], ALU.mult, ALU.add, [f"B{3 + sub * 2}", "att2" + ss_, "oa" + ss_], ["ob" + ss_])
        for sub in range(0 if stage == 2.93 else 2):
            ti = qt_ * 2 + sub
            ss_ = str(sub)
            atc = att[:, 8 * sub:8 * sub + 8]
            oa, ob = oas[sub], obs[sub]
            act(sqj3[:, :], ob[:, :], AF.Square, ["ob" + ss_], ["sqj3", "att3" + ss_], accum=atc[:, 3:4])
            act(atc[:, 4:5], atc[:, 3:4], AF.Sqrt, ["att3" + ss_], ["att4" + ss_], bias=EPS, scale=1.0 / 128)
            recip(atc[:, 5:6], atc[:, 4:5], ["att4" + ss_], ["att5" + ss_])
            ts("dve", oa[:, :], ob[:, :], atc[:, 5:6], 1.0 - LAM_INIT, ALU.mult, ALU.mult, ["ob" + ss_, "att5" + ss_], ["oa" + ss_])
            tt("dve", omx[:, :], oa[:, :], sgg[:, :], ALU.mult, ["oa" + ss_, "sgg"], ["omx"])
            pt_ = bank(6)
            ptb = pt_[:, 0:128].bitcast(BF16)
            tr(ptb[:, 0:128], omx[:, :], identb[:, :], ["omx", "identb"], ["B6"])
            tr(ptb[:, 128:256], ogb[:, ti, :], identb[:, :], ["ogb", "identb"], ["B6"])
            cp("act", omT[0][:, :], ptb[:, 0:128], ["B6"], ["omT0"])
            cp("act", omT[1][:, :], ptb[:, 128:256], ["B6"], ["omT1"])
            rb = rsst[ti % 2]
            for nh in range(2):
                po_ = bank(6)
                mm(po_, omT[0][:, :], wout[:, 0, nh * 512:(nh + 1) * 512], ["omT0", "wout"], ["B6"], start=True, stop=False)
                mm(po_, omT[1][:, :], wout[:, 1, nh * 512:(nh + 1) * 512], ["omT1", "wout"], ["B6"], start=False, stop=True)
                if nh == 0:
                    cp("act", rb[:, 0:512], po_, ["B6"], [f"rsst{ti % 2}"])
                else:
                    cp("dve", rb[:, 512:1024], po_, ["B6"], [f"rsst{ti % 2}"])
            rrow = rr * 512 + qq * 256 + sub * 128
            dma("pool", f"rs{ti % 2}", rs_in[sblk].ap()[rrow:rrow + 128, :], rb[:, :], [f"rsst{ti % 2}"], [f"rs_in{sblk}"])
        if rr == 3 and qq == 1 and stage >= 3:
            P.dma("pool", f"ccrs{sblk}", lambda e, sblk=sblk: e.collective_compute(
                "ReduceScatter", ALU.add, replica_groups=[[0, 1, 2, 3], [4, 5, 6, 7]],
                ins=[rs_in[sblk].ap().opt()], outs=[rs_out[sblk].ap().opt()]), reads=[f"rs_in{sblk}"], writes=[f"rs_out{sblk}"], inc=1)

    if 2.9 <= stage < 3:
        return finish()
    if stage == 3:
        return finish()
    P.barrier()
    A.off = persist_mark
    h2T = A.alloc("h2T", [128, 8, 2048], BF16)
    aff = A.alloc("aff", [128, 16, 16], F32)
    gw = A.alloc("gw", [128, 16, 16], F32)
    gt1bc = A.alloc("gt1bc", [128, D], F32)
    gt2bc = A.alloc("gt2bc", [128, D], F32)
    fgbc = A.alloc("fgbc", [128, D], F32)
    wr = A.alloc("wr", [128, 8, 16], BF16)
    sh2T = col(8)
    sc2T = col(8)
    a2 = col(8)
    n2 = A.alloc("n2", [128, 16], F32)
    ex = A.alloc("ex", [128, 16], F32)
    bs = A.alloc("bs", [64, 8], F32)
    taud = A.alloc("taud", [16, 16], F32)
    taubc = A.alloc("taubc", [128, 16], F32)
    xot = [A.alloc(f"xot{i}", [128, D], F32) for i in range(2)]
    rst = [A.alloc(f"rst{i}", [128, D], F32) for i in range(2)]
    xnw = [A.alloc(f"xnw{i}", [128, D], F32) for i in range(2)]
    sqj4 = A.alloc("sqj4", [128, D], BF16)
    m3_mark = A.off
    modrow = A.alloc("modrow", [1, 4096], F32)
    bm2 = A.alloc("bm2", [1, 4096], F32)
    p3_mark = A.off
    wm2 = A.alloc("wm2", [128, 8, 4096], BF16)
    for kc in range(8):
        dma("pool", f"wm{kc}", wm2[:, kc, :], wmod[kc * 128:(kc + 1) * 128, 2048:6144], (), [f"wm2_{kc}"])
    dma("sp", "sm0", bm2[:, :], bm2_d, (), ["bm2"])
    dma("sp", "sm1", fgbc[:, :], fgbc_d, (), ["fgbc"])
    dma("pool", "wab", wr[:, :, :].rearrange("p a b -> p (a b)"), wr_d, (), ["wr"])
    for cb in range(8):
        pm_ = bank(cb % 2)[0:1, :]
        for kc in range(8):
            mm(pm_, scb[:, kc, 0:1], wm2[:, kc, cb * 512:(cb + 1) * 512], ["scb", f"wm2_{kc}"], [f"B{cb % 2}"], start=(kc == 0), stop=(kc == 7))
        tt("dve", modrow[0:1, cb * 512:(cb + 1) * 512], pm_, bm2[0:1, cb * 512:(cb + 1) * 512], ALU.add, [f"B{cb % 2}", "bm2"], ["modrow"])
    for nh in range(2):
        pb_ = bank(2)
        mm(pb_, ones[0:1, :], modrow[0:1, nh * 512:(nh + 1) * 512], ["cst", "modrow"], ["B2"])
        cp("act", gt1bc[:, nh * 512:(nh + 1) * 512], pb_, ["B2"], ["gt1bc"])
        mm(pb_, ones[0:1, :], modrow[0:1, 3072 + nh * 512:3072 + (nh + 1) * 512], ["cst", "modrow"], ["B2"])
        cp("act", gt2bc[:, nh * 512:(nh + 1) * 512], pb_, ["B2"], ["gt2bc"])
    pc_ = bank(3)
    for kc in range(8):
        mm(pc_[:, kc:kc + 1], modrow[0:1, 1024 + kc * 128:1024 + (kc + 1) * 128], ones[0:1, 0:1], ["modrow", "cst"], ["B3"])
        mm(pc_[:, 8 + kc:9 + kc], modrow[0:1, 2048 + kc * 128:2048 + (kc + 1) * 128], ones[0:1, 0:1], ["modrow", "cst"], ["B3"])
    cp("dve", sh2T, pc_[:, 0:8], ["B3"], ["sh2T"])
    cp("dve", sc2T, pc_[:, 8:16], ["B3"], ["sc2T"])
    stt(a2, sc2T, 1.0, g2T, ALU.add, ALU.mult, ["sc2T", "g2T"], ["a2"])
    P.barrier()
    A.off = p3_mark
    xn2 = [A.alloc(f"xn2_{i}", [128, D], F32) for i in range(2)]
    exs = [ex, A.alloc("exb", [128, 16], F32)]
    affT = A.alloc("affT", [16, 2048], F32)
    affall = A.alloc("affall", [64, 2048], F32)
    cmpj = A.alloc("cmpj", [64, 2048], BF16)
    def p2_front(i):
        b_ = i % 2
        c0 = b_ * 3
        dma("sp", f"xo{b_}", xot[b_][:, :], xo[i * 128:(i + 1) * 128, :], (), [f"xot{b_}"])
        yield
        dma("sp", f"rsl{b_}", rst[b_][:, :], rs_out[i // 4].ap()[(i % 4) * 128:(i % 4 + 1) * 128, :], [f"rs_out{i // 4}"], [f"rst{b_}"])
        yield
        tt("pool", rst[b_][:, :], rst[b_][:, :], gt1bc[:, :], ALU.mult, [f"rst{b_}", "gt1bc"], [f"rst{b_}"])
        yield
        tt("dve", xnw[b_][:, :], rst[b_][:, :], xot[b_][:, :], ALU.add, [f"rst{b_}", f"xot{b_}"], [f"xnw{b_}"])
        yield
        dma("pool", f"xns{b_}", xnew_s[i * 128:(i + 1) * 128, :], xnw[b_][:, :], [f"xnw{b_}"], ["xnew_s"])
        yield
        act(sqj4[:, :], xnw[b_][:, :], AF.Square, [f"xnw{b_}"], ["sqj4", f"n2a{b_}"], accum=n2[:, c0:c0 + 1])
        yield
        act(n2[:, c0 + 1:c0 + 2], n2[:, c0:c0 + 1], AF.Sqrt, [f"n2a{b_}"], [f"n2b{b_}"], bias=EPS, scale=1.0 / D)
        yield
        recip(n2[:, c0 + 2:c0 + 3], n2[:, c0 + 1:c0 + 2], [f"n2b{b_}"], [f"n2c{b_}"])
        yield
        ts("pool", xn2[b_][:, :], xnw[b_][:, :], n2[:, c0 + 2:c0 + 3], 1.0, ALU.mult, ALU.mult, [f"xnw{b_}", f"n2c{b_}"], [f"xn2{b_}"])
        yield

    def p2_back(i):
        b_ = i % 2
        c0 = 6 + b_ * 3
        for kc in range(8):
            pb2 = kc % 2
            pT = bank(pb2)[:, 0:128]
            tr(pT, xn2[b_][:, kc * 128:(kc + 1) * 128], ident, [f"xn2{b_}", "cst"], [f"B{pb2}"])
            yield
            if kc % 2 == 0:
                act(h2T[:, kc, i * 128:(i + 1) * 128], pT, AF.Identity, [f"B{pb2}", "a2", "sh2T"], [f"h2T_{kc}"], bias=sh2T[:, kc:kc + 1], scale=a2[:, kc:kc + 1])
            else:
                ts("dve", h2T[:, kc, i * 128:(i + 1) * 128], pT, a2[:, kc:kc + 1], sh2T[:, kc:kc + 1], ALU.mult, ALU.add, [f"B{pb2}", "a2", "sh2T"], [f"h2T_{kc}"])
            yield
        pl = bank(2)[:, 0:16]
        for kc in range(8):
            mm(pl, h2T[:, kc, i * 128:(i + 1) * 128], wr[:, kc, :], [f"h2T_{kc}", "wr"], ["B2"], start=(kc == 0), stop=(kc == 7))
        yield
        P.op("dve", lambda e, pl=pl, c0=c0: e.tensor_reduce(out=n2[:, c0:c0 + 1], in_=pl, axis=AX.X, op=ALU.max, negate=True), reads=["B2"], writes=[f"n2d{b_}"])
        yield
        act(exs[b_][:, :], pl, AF.Exp, ["B2", f"n2d{b_}"], [f"ex{b_}", f"n2e{b_}"], bias=n2[:, c0:c0 + 1], scale=1.0, accum=n2[:, c0 + 1:c0 + 2])
        yield
        recip(n2[:, c0 + 2:c0 + 3], n2[:, c0 + 1:c0 + 2], [f"n2e{b_}"], [f"n2f{b_}"])
        yield
        ts("dve", aff[:, i, :], exs[b_][:, :], n2[:, c0 + 2:c0 + 3], None, ALU.mult, None, [f"ex{b_}", f"n2f{b_}"], ["aff"])
        yield
        pa_ = bank(3)[0:16, 0:128]
        tr(pa_, aff[:, i, :], ident, ["aff", "cst"], ["B3"])
        yield
        cp("act", affT[:, i * 128:(i + 1) * 128], pa_, ["B3"], ["affT"])
        yield

    interleave(p2_front(0))
    for i in range(16):
        gl = [p2_back(i)]
        if i + 1 < 16:
            gl.append(p2_front(i + 1))
        interleave(*gl)
    dma("pool", "agi", ag_in.ap()[:, :], affT[:, :], ["affT"], ["ag_in"])
    P.dma("pool", "ccag", lambda e: e.collective_compute(
        "AllGather", ALU.bypass, replica_groups=[[0, 1, 2, 3], [4, 5, 6, 7]],
        ins=[ag_in.ap().opt()], outs=[ag_out.ap().opt()]), reads=["ag_in"], writes=["ag_out"], inc=1)
    dma("sp", "ago", affall[:, :], ag_out.ap()[:, :], ["ag_out"], ["affall"])
    mset("pool", bs[:, 0:1], 0.0, ["lo"])
    G64 = C(12)
    for it in range(24):
        hw = 2.0 ** -(it + 1)
        ts("dve", bs[:, 1:2], bs[:, 0:1], hw, None, ALU.add, None, ["lo"], ["mid"])
        ts("dve", cmpj[:, :], affall[:, :], bs[:, 1:2], 0.0, ALU.is_ge, ALU.add, ["affall", "mid"], ["cmpj", "cnt"], accum=bs[:, 2:3])
        pc2 = bank(4)[0:64, 0:1]
        mm(pc2, G64[0:64, 0:64], bs[:, 2:3], ["cst", "cnt"], ["B4"])
        ts("dve", bs[:, 3:4], pc2, 1023.5, hw, ALU.is_ge, ALU.mult, ["B4"], ["gd"])
        tt("dve", bs[:, 0:1], bs[:, 0:1], bs[:, 3:4], ALU.add, ["lo", "gd"], ["lo"])
    ts("dve", taud[:, :], ident[0:16, 0:16], bs[0:16, 0:1], None, ALU.mult, None, ["cst", "lo"], ["taud"])
    ptau = bank(5)[:, 0:16]
    mm(ptau, ones[0:16, :], taud[:, :], ["cst", "taud"], ["B5"])
    cp("dve", taubc[:, :], ptau, ["B5"], ["taubc"])
    for i in range(16):
        tt("dve", gw[:, i, :], aff[:, i, :], taubc[:, :], ALU.is_ge, ["aff", "taubc"], ["gwA"])
        tt("dve", gw[:, i, :], gw[:, i, :], aff[:, i, :], ALU.mult, ["gwA", "aff"], ["gw"])
    dbg("xnew", xnew_s[:, :], [2048, D], "xnew_s")
    dbg("taubc", taubc[:, :], [128, 16], "taubc")
    dbg("aff", aff[:, :, :], [128, 16, 16], "aff")
    dbg("gw", gw[:, :, :], [128, 16, 16], "gw")

    if stage == 4:
        return finish()
    P.barrier()
    A.off = m3_mark
    yacc = A.alloc("yacc", [128, 16, D], F32)
    mset("pool", yacc[:, :, :], 0.0, [f"yacc{a}_{b}" for a in range(16) for b in range(2)])
    wgs = [A.alloc(f"wgs{i}", [128, 8, 512], BF16) for i in range(2)]
    wus = [A.alloc(f"wus{i}", [128, 8, 512], BF16) for i in range(2)]
    wds = [A.alloc(f"wds{i}", [128, 4, D], BF16) for i in range(2)]
    sg = [A.alloc(f"sg{i}", [128, 512], F32) for i in range(1)]
    hid = [A.alloc(f"hid{i}", [128, 512], BF16) for i in range(8)]
    gcnt = [0]

    def gu(e_, hf, TB, ws):
        for fc in range(4):
            gp = gcnt[0] % 2
            gcnt[0] += 1
            bG, bU = gp * 2, gp * 2 + 1
            pG, pU = bank(bG), bank(bU)
            for kc in range(8):
                mm(pG, wgs[ws][:, kc, fc * 128:(fc + 1) * 128], h2T[:, kc, TB * 512:(TB + 1) * 512], [f"wgs{ws}", f"h2T_{kc}"], [f"B{bG}"],
                   start=(kc == 0), stop=(kc == 7))
            for kc in range(8):
                mm(pU, wus[ws][:, kc, fc * 128:(fc + 1) * 128], h2T[:, kc, TB * 512:(TB + 1) * 512], [f"wus{ws}", f"h2T_{kc}"], [f"B{bU}"],
                   start=(kc == 0), stop=(kc == 7))
            act(sg[0][:, :], pG, AF.Silu, [f"B{bG}"], ["sg0"])
            hi = (TB % 2) * 4 + fc
            tt("dve", hid[hi][:, :], sg[0][:, :], pU, ALU.mult, ["sg0", f"B{bU}"], [f"hid{hi}"])

    def down(e_, hf, TB, ws):
        for half in range(2):
            for sub in range(2):
                for nh in range(2):
                    bY = 4 + sub * 2 + nh
                    py_ = bank(bY)
                    c0 = half * 256 + sub * 128
                    for fc in range(4):
                        hi = (TB % 2) * 4 + fc
                        mm(py_, hid[hi][:, c0:c0 + 128], wds[ws][:, fc, nh * 512:(nh + 1) * 512], [f"hid{hi}", f"wds{ws}"],
                           [f"B{bY}"], start=(fc == 0), stop=(fc == 3))
                    ti = TB * 4 + half * 2 + sub
                    stt(yacc[:, ti, nh * 512:(nh + 1) * 512], py_, gw[:, ti, e_:e_ + 1], yacc[:, ti, nh * 512:(nh + 1) * 512], ALU.mult, ALU.add,
                        [f"B{bY}", "gw", f"yacc{ti}_{nh}"], [f"yacc{ti}_{nh}"])

    prev = None
    for e_ in range(16):
        for hf in range(2):
            ws = (e_ * 2 + hf) % 2
            for kk in range(2):
                dma("pool", f"wg{ws}{kk}", wgs[ws][:, kk * 4:(kk + 1) * 4, :],
                    wg_d[e_, kk * 512:(kk + 1) * 512, hf * 512:(hf + 1) * 512].rearrange("(kc p) f -> p kc f", p=128), (), [f"wgs{ws}"])
                dma("pool", f"wu{ws}{kk}", wus[ws][:, kk * 4:(kk + 1) * 4, :],
                    wu_d[e_, kk * 512:(kk + 1) * 512, hf * 512:(hf + 1) * 512].rearrange("(kc p) f -> p kc f", p=128), (), [f"wus{ws}"])
                dma("pool", f"wd{ws}{kk}", wds[ws][:, kk * 2:(kk + 1) * 2, :],
                    wd_d[e_, hf * 512 + kk * 256:hf * 512 + (kk + 1) * 256, :].rearrange("(fc p) n -> p fc n", p=128), (), [f"wds{ws}"])
            for TB in range(4):
                gu(e_, hf, TB, ws)
                if prev is not None:
                    down(*prev)
                prev = (e_, hf, TB, ws)
    down(*prev)


    for i in range(16):
        b_ = i % 2
        dma("sp", f"xo{b_}", xot[b_][:, :], xnew_s[i * 128:(i + 1) * 128, :], ["xnew_s"], [f"xot{b_}"])
        tt("pool", rst[b_][:, :], yacc[:, i, :], gt2bc[:, :], ALU.mult, [f"yacc{i}_0", f"yacc{i}_1", "gt2bc"], [f"rst{b_}"])
        tt("dve", xnw[b_][:, :], rst[b_][:, :], xot[b_][:, :], ALU.add, [f"rst{b_}", f"xot{b_}"], [f"xnw{b_}"])
        act(sqj4[:, :], xnw[b_][:, :], AF.Square, [f"xnw{b_}"], ["sqj4", "n2a"], accum=n2[:, 0:1])
        act(n2[:, 1:2], n2[:, 0:1], AF.Sqrt, ["n2a"], ["n2b"], bias=EPS, scale=1.0 / D)
        recip(n2[:, 2:3], n2[:, 1:2], ["n2b"], ["n2c"])
        stt(rst[b_][:, :], xnw[b_][:, :], n2[:, 2:3], fgbc[:, :], ALU.mult, ALU.mult, [f"xnw{b_}", "n2c", "fgbc"], [f"rst{b_}"])
        dma("pool", f"out{b_}", out_d[i * 128:(i + 1) * 128, :], rst[b_][:, :], [f"rst{b_}"], [f"out{b_}"])
    return finish()


_CACHE = {}


def kernel(x, c, ctx, c_ctx, w_mod, b_mod, norm1_g, w_in, conv_w, a_log, dt_bias, gdn_norm_g,
           lam_q1, lam_k1, lam_q2, lam_k2, da_subln_g, w_out, norm2_g,
           w_router, w_gate, w_up, w_down, final_g):
    f32 = np.float32
    A_ = lambda a: np.ascontiguousarray(np.asarray(a, dtype=f32))
    x, c, ctx, c_ctx = A_(x), A_(c), A_(ctx), A_(c_ctx)
    w_mod, b_mod, w_in, conv_w = A_(w_mod)[0], A_(b_mod)[0], A_(w_in)[0], A_(conv_w)[0]
    a_log, dt_bias = A_(a_log)[0], A_(dt_bias)[0]
    w_out, w_router = A_(w_out)[0], A_(w_router)[0]
    w_gate, w_up, w_down = A_(w_gate)[0], A_(w_up)[0], A_(w_down)[0]
    norm1_g, norm2_g, final_g = A_(norm1_g)[0], A_(norm2_g)[0], A_(final_g)
    gdn_norm_g, da_subln_g = A_(gdn_norm_g)[0], A_(da_subln_g)[0]
    lamcat = np.concatenate([A_(lam_q1)[0], A_(lam_k1)[0], A_(lam_q2)[0], A_(lam_k2)[0]])

    if MAPS_ONLY[0]:
        nc = None
    else:
        key = (tuple(DEBUG), STAGE[0])
        if key not in _CACHE:
            _CACHE[key] = build(debug=key[0], stage=key[1])
        nc, dbg_outs = _CACHE[key]

    consts = make_consts()
    rope = make_rope()

    def colT(v, n):
        return np.ascontiguousarray(v.reshape(n, 128).T)

    in_maps = []
    for core in range(8):
        b, h = core // 4, core % 4
        j = h
        cT = np.zeros((128, 8, 2), f32)
        cT[:, :, 0] = colT(c[b], 8)
        cT[:, :, 1] = colT(c_ctx, 8)
        cols = np.concatenate([
            np.arange(h * 128, h * 128 + 128),
            512 + np.arange(h * 128, h * 128 + 128),
            1536 + np.arange(h * 128, h * 128 + 128),
            1536 + 512 + np.arange(h * 128, h * 128 + 128),
            1536 + 1024 + np.arange(h * 128, h * 128 + 128),
            1024 + np.arange(h * 128, h * 128 + 128),
            3072 + np.arange(h * 128, h * 128 + 128),
        ])
        abcols = 3584 + np.array([0 * 8 + 0 * 4 + h, 0 * 8 + 1 * 4 + h, 1 * 8 + 0 * 4 + h, 1 * 8 + 1 * 4 + h])
        cw = np.zeros((128, 3, 3), f32)
        for cc in range(3):
            for tap in range(3):
                cw[:, cc, tap] = conv_w[tap, cc * 512 + h * 128:cc * 512 + h * 128 + 128]
        gsc = np.tile(np.array([a_log[0, h], a_log[1, h], dt_bias[0, h], dt_bias[1, h]], f32)[None, :], (128, 1))
        m = {
            "xb": x[b], "ctxb": ctx[b], "xo": np.ascontiguousarray(x[b, j * 2048:(j + 1) * 2048]),
            "cT": cT.reshape(128, 16), "wmod": w_mod,
            "bm1T": colT(b_mod[0:2048], 16), "bm2": np.ascontiguousarray(b_mod[2048:].reshape(1, 4096)),
            "g1T": colT(norm1_g, 8), "g2T": colT(norm2_g, 8),
            "fgbc": np.ascontiguousarray(np.tile(final_g[None, :], (128, 1))),
            "wslab": np.ascontiguousarray(w_in[:, cols]), "wab": np.ascontiguousarray(w_in[:, abcols]),
            "cwT": cw.reshape(128, 9), "gsc": np.ascontiguousarray(gsc),
            "gng": np.ascontiguousarray(np.tile(gdn_norm_g[None, :], (128, 1))),
            "sgg": np.ascontiguousarray(np.tile(da_subln_g[None, :], (128, 1))),
            "lamv": np.ascontiguousarray(np.tile(lamcat[None, :], (128, 1))),
            "wout": np.ascontiguousarray(np.stack([w_out[h * 128:(h + 1) * 128], w_out[512 + h * 128:512 + (h + 1) * 128]], 0)),
            "rope": rope, "consts": consts,
            "wr": np.ascontiguousarray(w_router.reshape(8, 128, 16).transpose(1, 0, 2).reshape(128, 128)),
            "wg": w_gate if STAGE[0] > 4 else w_gate[0:1, 0:8, 0:8].copy(),
            "wu": w_up if STAGE[0] > 4 else w_up[0:1, 0:8, 0:8].copy(),
            "wd": w_down if STAGE[0] > 4 else w_down[0:1, 0:8, 0:8].copy(),
        }
        in_maps.append(m)
    if MAPS_ONLY[0]:
        return in_maps
    res = run_bass_kernel_spmd(nc, in_maps, core_ids=list(range(8)), **RUN_KW)
    out = np.zeros((2, L, D), f32)
    LAST["res"] = res
    for core in range(8):
        b, j = core // 4, core % 4
        out[b, j * 2048:(j + 1) * 2048] = res.results[core]["out"]
    return out
```
